# Optimizing a Trainium2 kernel written in Bass

```python
import jax, jax.numpy as jnp
from jax import lax
import numpy as np

D_MODEL = 1024
BATCH = 4
SEQ = 8192
DEPTH = 2

N_MIXERS = 2
ATTN_HEADS = 8
HEAD_DIM = D_MODEL // ATTN_HEADS
MOBA_BLOCK = 256
MOBA_TOPK = 3
Q_CHUNK = 16
CONV_WIDTH = 3
FFN_HIDDEN = ((-(-8 * D_MODEL // 3) + 255) // 256) * 256
RMS_EPS = 1e-6
N_ATTN_LAYERS = (DEPTH + N_MIXERS - 1) // N_MIXERS
N_CONV_LAYERS = DEPTH // N_MIXERS

kernel_name = "moba_shortconv_hybrid_trunk"


def rms_norm(x, g):
    xf = x.astype(jnp.float32)
    y = xf * lax.rsqrt(jnp.mean(xf * xf, axis=-1, keepdims=True) + RMS_EPS)
    return (y * g.astype(jnp.float32)).astype(x.dtype)


def moba_attention(h, w_qkv, w_o):
    B, S, _ = h.shape
    H, Dh, BS = ATTN_HEADS, HEAD_DIM, MOBA_BLOCK
    nb = -(-S // BS)
    pad = nb * BS - S
    qkv = jnp.einsum('bsd,de->bse', h, w_qkv).reshape(B, S, 3, H, Dh)
    q = jnp.transpose(qkv[:, :, 0], (0, 2, 1, 3))
    k = jnp.pad(jnp.transpose(qkv[:, :, 1], (0, 2, 1, 3)), ((0, 0), (0, 0), (0, pad), (0, 0)))
    v = jnp.pad(jnp.transpose(qkv[:, :, 2], (0, 2, 1, 3)), ((0, 0), (0, 0), (0, pad), (0, 0)))
    kb = k.reshape(B, H, nb, BS, Dh)
    vb = v.reshape(B, H, nb, BS, Dh)
    k_mean = jnp.mean(kb.astype(jnp.float32), axis=3)
    topk = min(MOBA_TOPK, nb)
    n_chunks = S // Q_CHUNK
    q_chunks = jnp.moveaxis(q.reshape(B, H, n_chunks, Q_CHUNK, Dh), 2, 0)
    starts = jnp.arange(n_chunks, dtype=jnp.int32) * Q_CHUNK
    b_ix = jnp.arange(B)[:, None, None, None]
    h_ix = jnp.arange(H)[None, :, None, None]
    blk_ids = jnp.arange(nb, dtype=jnp.int32)
    offs = jnp.arange(BS, dtype=jnp.int32)
    scale = Dh ** -0.5
    neg = jnp.finfo(jnp.float32).min

    def one_chunk(args):
        qc, start = args
        t = start + jnp.arange(Q_CHUNK, dtype=jnp.int32)
        own = t // BS
        gate = jnp.einsum('bhqd,bhnd->bhqn', qc.astype(jnp.float32), k_mean)
        past = blk_ids[None, :] < own[:, None]
        gate = jnp.where(past, gate, neg)
        _, sel = lax.top_k(gate, topk)
        own_b = jnp.broadcast_to(own[None, None, :, None], (B, H, Q_CHUNK, 1))
        idx = jnp.concatenate([sel, own_b], axis=-1)
        valid_blk = jnp.concatenate(
            [sel < own[None, None, :, None], jnp.ones((B, H, Q_CHUNK, 1), dtype=bool)], axis=-1)
        kpos = idx[..., None] * BS + offs
        mask = valid_blk[..., None] & (kpos <= t[None, None, :, None, None])
        kg = kb[b_ix, h_ix, idx]
        vg = vb[b_ix, h_ix, idx]
        s = jnp.einsum('bhqd,bhqnkd->bhqnk', qc, kg).astype(jnp.float32) * scale
        s = jnp.where(mask, s, neg)
        p = jax.nn.softmax(s.reshape(B, H, Q_CHUNK, -1), axis=-1).reshape(s.shape)
        return jnp.einsum('bhqnk,bhqnkd->bhqd', p.astype(vg.dtype), vg)

    out = lax.map(one_chunk, (q_chunks, starts))
    out = jnp.moveaxis(out, 0, 2).reshape(B, H, S, Dh)
    out = jnp.transpose(out, (0, 2, 1, 3)).reshape(B, S, H * Dh)
    return jnp.einsum('bse,ed->bsd', out, w_o)


def short_conv_mixer(h, w_in, conv_w, w_out):
    S = h.shape[1]
    bcx = jnp.einsum('bsd,de->bse', h, w_in)
    b_gate, c_gate, xt = jnp.split(bcx, 3, axis=-1)
    u = c_gate * xt
    up = jnp.pad(u, ((0, 0), (CONV_WIDTH - 1, 0), (0, 0)))
    conv = conv_w[0] * up[:, 0:S]
    for j in range(1, CONV_WIDTH):
        conv = conv + conv_w[j] * up[:, j:j + S]
    return jnp.einsum('bsd,de->bse', b_gate * conv, w_out)


def swiglu(h, w_in, w_out):
    g, u = jnp.split(jnp.einsum('bsd,df->bsf', h, w_in), 2, axis=-1)
    return jnp.einsum('bsf,fd->bsd', jax.nn.silu(g) * u, w_out)


def setup_inputs(seed: int = 0) -> dict:
    key = jax.random.key(seed)
    ks = jax.random.split(key, 12)
    D, F = D_MODEL, FFN_HIDDEN
    na, nc = N_ATTN_LAYERS, N_CONV_LAYERS
    f32 = jnp.float32

    def w(k, shape, fan_in):
        return jax.random.normal(k, shape, f32) * (fan_in ** -0.5)

    def gain(k, shape):
        return 1.0 + 0.05 * jax.random.normal(k, shape, f32)

    return {
        "x": jax.random.normal(ks[0], (BATCH, SEQ, D), f32),
        "attn_norm": gain(ks[1], (na, D)),
        "attn_w_qkv": w(ks[2], (na, D, 3 * ATTN_HEADS * HEAD_DIM), D),
        "attn_w_o": w(ks[3], (na, ATTN_HEADS * HEAD_DIM, D), ATTN_HEADS * HEAD_DIM),
        "conv_norm": gain(ks[4], (nc, D)),
        "conv_w_in": w(ks[5], (nc, D, 3 * D), D),
        "conv_w": w(ks[6], (nc, CONV_WIDTH, D), CONV_WIDTH),
        "conv_w_out": w(ks[7], (nc, D, D), D),
        "ffn_norm": gain(ks[8], (DEPTH, D)),
        "ffn_w_in": w(ks[9], (DEPTH, D, 2 * F), D),
        "ffn_w_out": w(ks[10], (DEPTH, F, D), F),
        "final_norm": gain(ks[11], (D,)),
    }


def reference(x, attn_norm, attn_w_qkv, attn_w_o, conv_norm, conv_w_in, conv_w, conv_w_out,
              ffn_norm, ffn_w_in, ffn_w_out, final_norm):
    for i in range(DEPTH):
        j = i // N_MIXERS
        if i % N_MIXERS == 0:
            x = x + moba_attention(rms_norm(x, attn_norm[j]), attn_w_qkv[j], attn_w_o[j])
        else:
            x = x + short_conv_mixer(rms_norm(x, conv_norm[j]), conv_w_in[j], conv_w[j], conv_w_out[j])
        x = x + swiglu(rms_norm(x, ffn_norm[i]), ffn_w_in[i], ffn_w_out[i])
    return rms_norm(x, final_norm)
```

```python
import contextlib
import numpy as np
import ml_dtypes
import concourse.bass as bass
import concourse.mybir as mybir
from concourse.bass_utils import run_bass_kernel_spmd

F32 = mybir.dt.float32
BF16 = mybir.dt.bfloat16
AF = mybir.ActivationFunctionType
ALU = mybir.AluOpType
AX = mybir.AxisListType

SEM_LIMIT = 30000


class Buf:
    __slots__ = ("name", "last_w", "readers", "dma_readers")

    def __init__(self, name):
        self.name = name
        self.last_w = None
        self.readers = {}
        self.dma_readers = []


class Op:
    __slots__ = ("eng", "fn", "waits", "signal", "sem", "val", "is_dma", "skey", "idx")

    def __init__(self, eng, fn, is_dma, skey):
        self.eng = eng
        self.fn = fn
        self.waits = []
        self.signal = False
        self.sem = None
        self.val = 0
        self.is_dma = is_dma
        self.skey = skey


class Prog:
    ENGS = ("pe", "act", "dve", "pool", "sp")

    def __init__(self, nc):
        self.nc = nc
        self.ops = {e: [] for e in self.ENGS}
        self.stack = contextlib.ExitStack()
        self.nbuf = 0
        self.sb_base = (nc.sbuf_base + 63) // 64 * 64
        self.sb_ptr = self.sb_base
        self.sb_top = nc.sbuf_top
        self.sb_n = 0
        self.sb_peak = 0
        self.last_comp = {}
        self.dma_since = {}

    def sbuf(self, name, shape, dtype):
        esz = 4 if dtype == F32 else 2
        size = esz
        for d in shape[1:]:
            size *= d
        size = (size + 63) // 64 * 64
        off = self.sb_ptr
        self.sb_ptr += size
        assert self.sb_ptr <= self.sb_top, (name, self.sb_ptr, self.sb_top)
        self.sb_peak = max(self.sb_peak, self.sb_ptr)
        self.sb_n += 1
        return self.nc.alloc_sbuf_tensor_at(f"{name}{self.sb_n}", list(shape), dtype, offset=off)

    def mark(self):
        return self.sb_ptr

    def release(self, mark):
        self.sb_ptr = mark

    def barrier(self):
        deps = list(self.last_comp.values()) + list(self.dma_since.values())
        for e in self.ENGS:
            op = Op(e, lambda eng: None, False, None)
            for w in deps:
                if w.eng == e and not w.is_dma:
                    continue
                w.signal = True
                op.waits.append(w)
            self.ops[e].append(op)
        self.dma_since = {}

    def psum(self, name, shape, dtype):
        return self.stack.enter_context(self.nc.psum_tensor(name, list(shape), dtype))

    def buf(self, name=None):
        self.nbuf += 1
        return Buf(name or f"b{self.nbuf}")

    def wait_only(self, eng, reads):
        op = Op(eng, lambda e: None, False, None)
        for b in reads:
            w = b.last_w
            if w is not None and w not in op.waits:
                w.signal = True
                op.waits.append(w)
        self.ops[eng].append(op)
        return op

    def emit(self, eng, fn, reads=(), writes=(), dma=False, skey=None):
        op = Op(eng, fn, dma, skey)
        waits = []
        for b in reads:
            if b.last_w is not None:
                waits.append(b.last_w)
        for b in writes:
            w = b.last_w
            if w is not None and (w.eng != eng or w.is_dma or dma):
                waits.append(w)
            for e, r in b.readers.items():
                if e != eng or dma:
                    waits.append(r)
            for r in b.dma_readers:
                waits.append(r)
        seen = set()
        for w in waits:
            if id(w) in seen or w is op:
                continue
            seen.add(id(w))
            w.signal = True
            op.waits.append(w)
        for b in reads:
            if dma:
                b.dma_readers.append(op)
            else:
                b.readers[eng] = op
        for b in writes:
            b.last_w = op
            b.readers = {}
            b.dma_readers = []
        if dma:
            op.signal = True
            self.dma_since[skey] = op
        else:
            self.last_comp[eng] = op
        self.ops[eng].append(op)
        return op

    def dma(self, out, in_, reads=(), writes=(), skey=None, eng="sp", **kw):
        assert skey is not None
        return self.emit(eng, lambda e: e.dma_start(out=out, in_=in_, **kw),
                         reads=reads, writes=writes, dma=True, skey=skey)

    def mm(self, out, lhsT, rhs, start, stop, reads=(), writes=()):
        return self.emit("pe", lambda e: e.matmul(out, lhsT, rhs, start=start, stop=stop),
                         reads=reads, writes=writes)

    def tr(self, out, in_, ident, reads=(), writes=()):
        return self.emit("pe", lambda e: e.transpose(out, in_, ident), reads=reads, writes=writes)

    def finalize(self):
        nc = self.nc
        sem_stack = self.stack
        nsem = [0]

        def new_sem(tag):
            nsem[0] += 1
            return sem_stack.enter_context(nc.semaphore(f"s_{tag}_{nsem[0]}"))

        for e in self.ENGS:
            cur = None
            cnt = 0
            for op in self.ops[e]:
                if op.is_dma or not op.signal:
                    continue
                if cur is None or cnt >= SEM_LIMIT:
                    cur = new_sem(e)
                    cnt = 0
                cnt += 1
                op.sem, op.val = cur, cnt
        keysem = {}
        for e in self.ENGS:
            for op in self.ops[e]:
                if not op.is_dma:
                    continue
                if op.skey not in keysem or keysem[op.skey][1] >= SEM_LIMIT:
                    keysem[op.skey] = [new_sem("d"), 0]
                ks = keysem[op.skey]
                ks[1] += 16
                op.sem, op.val = ks[0], ks[1]
        self.n_sems = nsem[0]

        with nc.Block() as block:
            def runner(ename):
                def run(eng):
                    seen = {}
                    for op in self.ops[ename]:
                        need = {}
                        for w in op.waits:
                            k = id(w.sem)
                            if seen.get(k, 0) >= w.val:
                                continue
                            if k not in need or need[k][1] < w.val:
                                need[k] = (w.sem, w.val)
                        for k, (s, v) in need.items():
                            eng.wait_ge(s, v)
                            seen[k] = v
                        ins = op.fn(eng)
                        if ins is None:
                            assert not op.signal
                            continue
                        if op.signal and op.sem is not None:
                            ins.then_inc(op.sem, 16 if op.is_dma else 1)
                return run

            block.tensor(runner("pe"))
            block.scalar(runner("act"))
            block.vector(runner("dve"))
            block.gpsimd(runner("pool"))
            block.sync(runner("sp"))
        self.stack.close()


D = 1024
H = 8
F = 2816
NFC = 22
EPS = 1e-6
QSCALE = 128.0 ** -0.5
NEGBIG = -30000.0


def build_program(NP, NO, debug=False, stop_after=None, conv_in_b=True):
    nc = bass.Bass("TRN2", target_bir_lowering=False)
    NB = NP + NO
    LT = NB * 256
    NT = LT // 128
    NG = LT // 512
    NQT = 2 * NO + 1
    GQ0 = NP // 2 - 1
    NQG = NG - GQ0
    QW = NQG * 512
    NAO = NQT * 128
    OUT_T = NO * 256

    def din(name, shape):
        return nc.dram_tensor(name, list(shape), F32, kind="ExternalInput").ap()

    xl = din("xl", [LT, D])
    wqkv = din("wqkv", [D, 3 * D])
    wo = din("wo", [D, D])
    cwin = din("cwin", [D, 3 * D])
    cw = din("cw", [3, D])
    cwout = din("cwout", [D, D])
    fwin = din("fwin", [2, D, 2 * F])
    fwout = din("fwout", [2, F, D])
    n_attn = din("n_attn", [D])
    n_conv = din("n_conv", [D])
    n_ffn = din("n_ffn", [2, D])
    n_fin = din("n_fin", [D])
    pastneg = din("pastneg", [128, NQT * NB])
    pastind = din("pastind", [128, NQT * NB])
    flag = din("flag", [128, 1])
    out = nc.dram_tensor("out", [OUT_T, D], F32, kind="ExternalOutput").ap()
    sk = "ExternalOutput" if debug else "Internal"
    KTs = nc.dram_tensor("KTs", [H, 128, LT], BF16, kind=sk).ap()
    Vs = nc.dram_tensor("Vs", [H, 128, NT, 130], BF16, kind=sk).ap()
    QTs = nc.dram_tensor("QTs", [H, 128, QW], BF16, kind=sk).ap()
    AOs = nc.dram_tensor("AOs", [H, 128, NAO], BF16, kind=sk).ap()
    NU = 46
    WU = nc.dram_tensor("WU", [NU, 128, 4096], BF16, kind="Internal").ap()
    U_WO = [0, 1]
    U_FIN = [[2 + l * 17 + u for u in range(11)] for l in range(2)]
    U_FOUT = [[2 + l * 17 + 11 + k for k in range(6)] for l in range(2)]
    U_CIN = [36 + j for j in range(8)]
    U_COUT = [44, 45]

    P = Prog(nc)
    psb = [P.psum(f"psb{i}", [128, 512], F32) for i in range(6)]
    b_ps = [P.buf(f"ps{i}") for i in range(6)]
    pT = [P.psum(f"pT{i}", [128, 8, 128], BF16) for i in range(2)]
    b_pT = [P.buf(f"pT{i}") for i in range(2)]
    psrr = [0]

    def nbank():
        i = psrr[0] % 6
        psrr[0] += 1
        return i

    identf = P.sbuf("identf", [128, 128], F32)
    ident = P.sbuf("ident", [128, 128], BF16)
    tri = P.sbuf("tri", [128, 128], BF16)
    gcols = P.sbuf("gcols", [128, 4, 8], F32)
    kms = P.sbuf("kms", [128, H, NB], F32)
    kmb = P.sbuf("kmb", [128, H, NB], BF16)
    flagt = P.sbuf("flagt", [128, 1], F32)
    b_idf, b_id, b_tri, b_g, b_kms, b_kmb, b_flag = [P.buf() for _ in range(7)]
    P.emit("pool", lambda e: e.memset(identf[:], 0.0), writes=[b_idf])
    P.emit("pool", lambda e: e.affine_select(out=identf[:], in_=identf[:], pattern=[[-1, 128]],
                                             compare_op=ALU.not_equal, fill=1.0, base=0, channel_multiplier=1),
           reads=[b_idf], writes=[b_idf])
    P.emit("dve", lambda e: e.tensor_copy(out=ident[:], in_=identf[:]), reads=[b_idf], writes=[b_id])
    P.emit("pool", lambda e: e.memset(identf[:], 1.0), reads=[b_idf], writes=[b_idf])
    P.emit("pool", lambda e: e.affine_select(out=identf[:], in_=identf[:], pattern=[[1, 128]],
                                             compare_op=ALU.is_ge, fill=0.0, base=0, channel_multiplier=-1),
           reads=[b_idf], writes=[b_idf])
    P.emit("dve", lambda e: e.tensor_copy(out=tri[:], in_=identf[:]), reads=[b_idf], writes=[b_tri])
    for i, src in enumerate([n_attn, n_conv, n_ffn[0], n_ffn[1]]):
        P.dma(gcols[:, i, :], src.rearrange("(kc p) -> p kc", p=128), writes=[b_g], skey=f"g{i}",
              allow_slow_non_contiguous=True)
    P.dma(flagt[:], flag, writes=[b_flag], skey="flag")
    P.emit("pool", lambda e: e.memset(kms[:], 0.0), writes=[b_kms])

    b_WU = [P.buf(f"WU{u}") for u in range(NU)]
    rr = [0]

    def make_conv_jobs(stg, b_stg, img, b_img):
        jobs = []
        cnt = [0]

        def conv_ops(slot, views, gidx, eng_list):
            for (iv, sv, kc) in views:
                eng = eng_list[rr[0] % len(eng_list)]
                rr[0] += 1
                rd = [b for b in b_stg[slot]] + ([b_g] if kc is not None else [])
                if kc is None:
                    if eng == "act":
                        P.emit("act", lambda e, iv=iv, sv=sv: e.activation(out=iv, in_=sv, func=AF.Copy),
                               reads=rd, writes=[b_img[slot]])
                    else:
                        P.emit(eng, lambda e, iv=iv, sv=sv: e.tensor_copy(out=iv, in_=sv), reads=rd, writes=[b_img[slot]])
                else:
                    gs = gcols[:, gidx, kc:kc + 1]
                    if eng == "act":
                        P.emit("act", lambda e, iv=iv, sv=sv, gs=gs: e.activation(out=iv, in_=sv, func=AF.Copy, scale=gs),
                               reads=rd, writes=[b_img[slot]])
                    else:
                        P.emit(eng, lambda e, iv=iv, sv=sv, gs=gs: e.tensor_scalar(out=iv, in0=sv, scalar1=gs, scalar2=None, op0=ALU.mult),
                               reads=rd, writes=[b_img[slot]])

        def job(uidx, loads, views, gidx):
            def run(eng_list):
                slot = cnt[0] % 2
                cnt[0] += 1
                for i, (sv, src) in enumerate(loads):
                    P.dma(sv(stg[slot]), src, writes=[b_stg[slot][i]], skey=f"stg{slot}_{i}")
                conv_ops(slot, [(iv(img[slot]), sv(stg[slot]), kc) for (iv, sv, kc) in views], gidx, eng_list)
                P.dma(WU[uidx], img[slot][:], reads=[b_img[slot]], writes=[b_WU[uidx]], skey=f"img{slot}", eng="pool")
            return run

        def v3(a, b):
            return lambda t: t[:, 0:a * b].rearrange("p (a b) -> p a b", a=a)

        for srcw, units in ((wo, U_WO), (cwout, U_COUT)):
            for n in range(2):
                src = srcw.rearrange("(h p) c -> p h c", p=128)[:, :, n * 512:(n + 1) * 512]
                jobs.append(job(units[n], [(v3(8, 512), src)],
                                [(lambda t: t[:, :], lambda t: t[:, :], None)], None))
        for l in range(2):
            for u in range(11):
                loads = []
                for gu in range(2):
                    for c in range(2):
                        col0 = gu * F + (2 * u + c) * 128
                        src = fwin[l].rearrange("(kc p) n -> p kc n", p=128)[:, :, col0:col0 + 128]
                        o = (gu * 2 + c) * 1024
                        loads.append((lambda t, o=o: t[:, o:o + 1024].rearrange("p (a b) -> p a b", a=8), src))
                views = []
                for kc in range(8):
                    vw = lambda t, kc=kc: t[:, :].rearrange("p (g k f) -> p g k f", g=4, k=8)[:, :, kc, :]
                    views.append((vw, vw, kc))
                jobs.append(job(U_FIN[l][u], loads, views, 2 + l))
        for l in range(2):
            for half in range(2):
                for k in range(3):
                    f0 = 8 * k
                    nfc = min(8, NFC - f0)
                    src = fwout[l].rearrange("(fc p) c -> p fc c", p=128)[:, f0:f0 + nfc, half * 512:(half + 1) * 512]
                    jobs.append(job(U_FOUT[l][half * 3 + k], [(v3(nfc, 512), src)],
                                    [(lambda t, n=nfc: t[:, 0:n * 512], lambda t, n=nfc: t[:, 0:n * 512], None)], None))
        for j in range(8):
            loads = []
            for t3 in range(3):
                col0 = t3 * D + j * 128
                src = cwin.rearrange("(kc p) n -> p kc n", p=128)[:, :, col0:col0 + 128]
                o = t3 * 1024
                loads.append((lambda t, o=o: t[:, o:o + 1024].rearrange("p (a b) -> p a b", a=8), src))
            views = []
            for kc in range(8):
                vw = lambda t, kc=kc: t[:, 0:3072].rearrange("p (g k f) -> p g k f", g=3, k=8)[:, :, kc, :]
                views.append((vw, vw, kc))
            jobs.append(job(U_CIN[j], loads, views, 1))
        return jobs

    m_persist = P.mark()

    Wsb = P.sbuf("Wsb", [128, 8, 3 * D], BF16)
    b_W = [P.buf(f"W{kc}") for kc in range(8)]
    stgA = [P.sbuf(f"stgA{i}", [128, 3 * D], F32) for i in range(2)]
    b_stgA = [P.buf() for _ in range(2)]
    for kc in range(8):
        sl = kc % 2
        P.dma(stgA[sl][:], wqkv[kc * 128:(kc + 1) * 128, :], writes=[b_stgA[sl]], skey=f"stgA{sl}")
        for part in range(3):
            eng = ["dve", "act", "pool"][part]
            ov = Wsb[:, kc, part * D:(part + 1) * D]
            iv = stgA[sl][:, part * D:(part + 1) * D]
            gs = gcols[:, 0, kc:kc + 1]
            if eng == "act":
                P.emit("act", lambda e, ov=ov, iv=iv, gs=gs: e.activation(out=ov, in_=iv, func=AF.Copy, scale=gs),
                       reads=[b_stgA[sl], b_g], writes=[b_W[kc]])
            else:
                P.emit(eng, lambda e, ov=ov, iv=iv, gs=gs: e.tensor_scalar(out=ov, in0=iv, scalar1=gs, scalar2=None, op0=ALU.mult),
                       reads=[b_stgA[sl], b_g], writes=[b_W[kc]])

    xs = [P.sbuf(f"xs{i}", [128, 4, D], F32) for i in range(2)]
    b_xs = [P.buf() for _ in range(2)]
    junk = P.sbuf("junk", [128, D], BF16)
    b_junk = P.buf()
    msA = P.sbuf("msA", [128, NT], F32)
    sdA = P.sbuf("sdA", [128, NT], F32)
    rsA = P.sbuf("rsA", [128, NT], F32)
    b_msA = [P.buf() for _ in range(NG)]
    xn = [P.sbuf(f"xn{i}", [128, D], BF16) for i in range(2)]
    b_xn = [P.buf() for _ in range(2)]
    xnT = [P.sbuf(f"xnT{i}", [128, 8, 512], BF16) for i in range(2)]
    b_xnT = [P.buf() for _ in range(2)]
    Kst = [P.sbuf(f"Kst{i}", [128, H, 512], BF16) for i in range(2)]
    Qst = [P.sbuf(f"Qst{i}", [128, H, 512], BF16) for i in range(2)]
    Vst = [P.sbuf(f"Vst{i}", [128, H, 4, 130], BF16) for i in range(2)]
    b_Kst = [P.buf() for _ in range(2)]
    b_Qst = [P.buf() for _ in range(2)]
    b_Vst = [P.buf() for _ in range(2)]
    b_KT = [P.buf() for _ in range(NG)]
    b_V = [P.buf() for _ in range(NG)]
    b_Q = [P.buf() for _ in range(NG)]
    P.emit("pool", lambda e: e.memset(msA[:], 0.0), writes=b_msA)
    for i in range(2):
        P.emit("pool", lambda e, i=i: e.memset(Vst[i][:, :, :, 128:129], 1.0), writes=[b_Vst[i]])
        P.emit("pool", lambda e, i=i: e.memset(Vst[i][:, :, :, 129:130], 0.0), writes=[b_Vst[i]])

    def rms_stats(xtile_fn, nt, ms, sd, rs, c0, b_x, b_ms):
        for s in range(nt):
            P.emit("act", lambda e, s=s: e.activation(out=junk[:], in_=xtile_fn(s), func=AF.Square, scale=1.0 / 32,
                                                      accum_out=ms[:, c0 + s:c0 + s + 1]),
                   reads=[b_x], writes=[b_junk, b_ms])
        P.emit("dve", lambda e: e.tensor_scalar(out=sd[:, c0:c0 + nt], in0=ms[:, c0:c0 + nt], scalar1=EPS, scalar2=None, op0=ALU.add),
               reads=[b_ms], writes=[b_ms])
        P.emit("act", lambda e: e.activation(out=sd[:, c0:c0 + nt], in_=sd[:, c0:c0 + nt], func=AF.Sqrt),
               reads=[b_ms], writes=[b_ms])
        P.emit("dve", lambda e: e.reciprocal(out=rs[:, c0:c0 + nt], in_=sd[:, c0:c0 + nt]), reads=[b_ms], writes=[b_ms])

    def norm_T(xtile_fn, nt, rs, c0, b_x, b_ms, xnT_t, b_xnT_t):
        for s in range(nt):
            i = s % 2
            P.emit("act", lambda e, s=s, i=i: e.activation(out=xn[i][:], in_=xtile_fn(s), func=AF.Copy,
                                                           scale=rs[:, c0 + s:c0 + s + 1]),
                   reads=[b_x, b_ms], writes=[b_xn[i]])
            for kc in range(8):
                P.tr(pT[i][:, kc, :], xn[i][:, kc * 128:(kc + 1) * 128], ident[:], reads=[b_xn[i], b_id], writes=[b_pT[i]])
            P.emit("dve", lambda e, s=s, i=i: e.tensor_copy(out=xnT_t[:, :, s * 128:(s + 1) * 128], in_=pT[i][:]),
                   reads=[b_pT[i]], writes=[b_xnT_t])

    def A_front(g):
        sl = g % 2
        P.dma(xs[sl][:], xl[g * 512:(g + 1) * 512, :].rearrange("(s p) d -> p s d", p=128), writes=[b_xs[sl]], skey=f"xs{sl}")
        rms_stats(lambda s: xs[sl][:, s, :], 4, msA, sdA, rsA, g * 4, b_xs[sl], b_msA[g])
        norm_T(lambda s: xs[sl][:, s, :], 4, rsA, g * 4, b_xs[sl], b_msA[g], xnT[sl], b_xnT[sl])

    evr = [0]

    def A_back(g):
        sl = g % 2
        xt_ = xnT[sl]
        for h in range(H):
            bi = nbank()
            for kc in range(8):
                P.mm(psb[bi][:, :], Wsb[:, kc, D + h * 128:D + (h + 1) * 128], xt_[:, kc, :], kc == 0, kc == 7,
                     reads=[b_W[kc], b_xnT[sl]], writes=[b_ps[bi]])
            for blk in range(2):
                P.emit("act", lambda e, h=h, bi=bi, blk=blk: e.activation(
                    out=Kst[sl][:, h, blk * 256:(blk + 1) * 256], in_=psb[bi][:, blk * 256:(blk + 1) * 256], func=AF.Copy,
                    accum_out=kms[:, h, 2 * g + blk:2 * g + blk + 1]),
                    reads=[b_ps[bi]], writes=[b_Kst[sl], b_kms])
        P.dma(KTs.rearrange("h d t -> d h t")[:, :, g * 512:(g + 1) * 512], Kst[sl][:], reads=[b_Kst[sl]], writes=[b_KT[g]],
              skey=f"Kst{sl}", eng="pool")
        if g >= GQ0:
            for h in range(H):
                bi = nbank()
                for kc in range(8):
                    P.mm(psb[bi][:, :], Wsb[:, kc, h * 128:(h + 1) * 128], xt_[:, kc, :], kc == 0, kc == 7,
                         reads=[b_W[kc], b_xnT[sl]], writes=[b_ps[bi]])
                P.emit("dve", lambda e, h=h, bi=bi: e.tensor_scalar(out=Qst[sl][:, h, :], in0=psb[bi][:, :], scalar1=QSCALE,
                                                                   scalar2=None, op0=ALU.mult),
                       reads=[b_ps[bi]], writes=[b_Qst[sl]])
            gq = g - GQ0
            P.dma(QTs.rearrange("h d t -> d h t")[:, :, gq * 512:(gq + 1) * 512], Qst[sl][:], reads=[b_Qst[sl]], writes=[b_Q[g]],
                  skey=f"Qst{sl}", eng="pool")
        for s in range(4):
            for half in range(2):
                bi = nbank()
                for kc in range(8):
                    P.mm(psb[bi][:, :], xt_[:, kc, s * 128:(s + 1) * 128], Wsb[:, kc, 2 * D + half * 512:2 * D + (half + 1) * 512],
                         kc == 0, kc == 7, reads=[b_W[kc], b_xnT[sl]], writes=[b_ps[bi]])
                ov = Vst[sl][:, half * 4:(half + 1) * 4, s, 0:128]
                iv = psb[bi][:, :].rearrange("p (h d) -> p h d", h=4)
                evr[0] += 1
                if evr[0] % 2 == 0:
                    P.emit("act", lambda e, ov=ov, iv=iv: e.activation(out=ov, in_=iv, func=AF.Copy),
                           reads=[b_ps[bi]], writes=[b_Vst[sl]])
                else:
                    P.emit("dve", lambda e, ov=ov, iv=iv: e.tensor_copy(out=ov, in_=iv), reads=[b_ps[bi]], writes=[b_Vst[sl]])
        P.dma(Vs.rearrange("h p t c -> p h t c")[:, :, g * 4:(g + 1) * 4, :], Vst[sl][:], reads=[b_Vst[sl]], writes=[b_V[g]],
              skey=f"Vst{sl}", eng="pool")

    A_front(0)
    for g in range(NG):
        if g + 1 < NG:
            A_front(g + 1)
        A_back(g)
    P.emit("dve", lambda e: e.tensor_copy(out=kmb[:], in_=kms[:]), reads=[b_kms], writes=[b_kmb])
    P.barrier()
    P.release(m_persist)
    if stop_after == "A":
        P.finalize()
        return nc

    KT = [P.sbuf(f"KT{i}", [128, LT], BF16) for i in range(2)]
    Vb = [P.sbuf(f"Vb{i}", [128, NT, 130], BF16) for i in range(2)]
    QT = [P.sbuf(f"QT{i}", [128, QW], BF16) for i in range(2)]
    AOst = [P.sbuf(f"AOst{i}", [128, NAO], BF16) for i in range(2)]
    b_KTb = [P.buf() for _ in range(2)]
    b_Vb = [P.buf() for _ in range(2)]
    b_QTb = [P.buf() for _ in range(2)]
    b_AOst = [P.buf() for _ in range(2)]
    b_AO = [P.buf() for _ in range(H)]
    pneg = P.sbuf("pneg", [128, NQT * NB], F32)
    pind = P.sbuf("pind", [128, NQT * NB], F32)
    b_pm = P.buf()
    P.dma(pneg[:], pastneg, writes=[b_pm], skey="pneg")
    P.dma(pind[:], pastind, writes=[b_pm], skey="pind")
    gm = P.sbuf("gm", [128, NQT, NB], F32)
    top8 = P.sbuf("top8", [128, NQT, 8], F32)
    sel = [P.sbuf(f"sel{i}", [128, NQT, NB], F32) for i in range(2)]
    b_gm = P.buf()
    b_top8 = P.buf()
    b_sel = [P.buf() for _ in range(2)]
    PT = [P.sbuf(f"PT{i}", [128, 512], BF16) for i in range(3)]
    b_PT = [P.buf() for _ in range(3)]
    acc = [P.sbuf(f"acc{i}", [128, 2, 130], F32) for i in range(2)]
    b_acc = [P.buf() for _ in range(2)]
    rec = [P.sbuf(f"rec{i}", [128, 2], F32) for i in range(2)]
    obf = [P.sbuf(f"obf{i}", [128, 2, 128], BF16) for i in range(2)]
    b_obf = [P.buf() for _ in range(2)]
    stgB = [P.sbuf(f"stgB{i}", [128, 4096], F32) for i in range(2)]
    imgB = [P.sbuf(f"imgB{i}", [128, 4096], BF16) for i in range(2)]
    b_stgB = [[P.buf() for _ in range(4)] for _ in range(2)]
    b_imgB = [P.buf() for _ in range(2)]
    conv_jobs = make_conv_jobs(stgB, b_stgB, imgB, b_imgB)

    def B_load(h):
        hb = h % 2
        P.dma(KT[hb][:], KTs[h], reads=b_KT, writes=[b_KTb[hb]], skey=f"KT{hb}")
        P.dma(Vb[hb][:], Vs[h], reads=b_V, writes=[b_Vb[hb]], skey=f"Vb{hb}")
        P.dma(QT[hb][:], QTs[h], reads=[b for b in b_Q[GQ0:]], writes=[b_QTb[hb]], skey=f"QT{hb}")

    def B_gate(h):
        hb = h % 2
        j0 = 0
        while j0 < NQT:
            j1 = min(NQT, j0 + 512 // NB)
            for j in range(j0, j1):
                P.mm(psb[5][:, (j - j0) * NB:(j - j0 + 1) * NB], QT[hb][:, (j + 3) * 128:(j + 4) * 128], kmb[:, h, :], True, True,
                     reads=[b_QTb[hb], b_kmb], writes=[b_ps[5]])
            P.emit("dve", lambda e, j0=j0, j1=j1: e.tensor_tensor(
                out=gm[:, j0:j1, :], in0=psb[5][:, 0:(j1 - j0) * NB].rearrange("p (j n) -> p j n", n=NB),
                in1=pneg[:, j0 * NB:j1 * NB].rearrange("p (j n) -> p j n", n=NB), op=ALU.add),
                reads=[b_ps[5], b_pm], writes=[b_gm])
            j0 = j1
        for j in range(NQT):
            P.emit("dve", lambda e, j=j: e.max(out=top8[:, j, :], in_=gm[:, j, :]), reads=[b_gm], writes=[b_top8])
        for j in range(NQT):
            P.emit("dve", lambda e, j=j: e.scalar_tensor_tensor(
                out=sel[hb][:, j, :], in0=gm[:, j, :], scalar=top8[:, j, 2:3], in1=pind[:, j * NB:(j + 1) * NB],
                op0=ALU.is_ge, op1=ALU.mult), reads=[b_gm, b_top8, b_pm], writes=[b_sel[hb]])

    items = []
    for h in range(H):
        for m in range(NO + 1):
            ob = NP - 1 + m
            items.append((h, m, ob, True))
            for n in range(ob):
                items.append((h, m, n, False))
    nitems = len(items)
    ncj = len(conv_jobs)
    cj_every = max(1, (nitems - 40) // ncj)
    cj_next = [0]
    scount = [0]
    ocount = [0]
    item_state = {}

    def qinfo(m):
        if m == 0:
            return [0], 3 * 128, 128
        return [2 * m - 1, 2 * m], (2 * m + 2) * 128, 256

    def item_S(idx):
        h, m, n, own = items[idx]
        hb = h % 2
        jt, qc, W = qinfo(m)
        sb = scount[0] % 3
        scount[0] += 1
        item_state[idx] = sb
        for kt in range(2):
            P.mm(psb[sb][:, kt * W:(kt + 1) * W], KT[hb][:, (2 * n + kt) * 128:(2 * n + kt + 1) * 128], QT[hb][:, qc:qc + W],
                 True, True, reads=[b_KTb[hb], b_QTb[hb]], writes=[b_ps[sb]])
        P.emit("act", lambda e, sb=sb, W=W: e.activation(out=PT[sb][:, 0:2 * W], in_=psb[sb][:, 0:2 * W], func=AF.Exp),
               reads=[b_ps[sb]], writes=[b_PT[sb]])
        if own:
            if m == 0:
                P.emit("pool", lambda e, sb=sb, W=W: e.tensor_tensor(out=PT[sb][:, W:W + 128], in0=PT[sb][:, W:W + 128], in1=tri[:], op=ALU.mult),
                       reads=[b_PT[sb], b_tri], writes=[b_PT[sb]])
            else:
                P.emit("pool", lambda e, sb=sb: e.tensor_tensor(out=PT[sb][:, 0:128], in0=PT[sb][:, 0:128], in1=tri[:], op=ALU.mult),
                       reads=[b_PT[sb], b_tri], writes=[b_PT[sb]])
                P.emit("pool", lambda e, sb=sb, W=W: e.tensor_tensor(out=PT[sb][:, W + 128:W + 256], in0=PT[sb][:, W + 128:W + 256], in1=tri[:], op=ALU.mult),
                       reads=[b_PT[sb], b_tri], writes=[b_PT[sb]])

    def item_PV(idx):
        h, m, n, own = items[idx]
        hb = h % 2
        jt, qc, W = qinfo(m)
        sb = item_state.pop(idx)
        ob_ = 3 + (ocount[0] % 2)
        ocount[0] += 1
        ab = m % 2
        pso = psb[ob_][:, :].rearrange("p (q c) -> p q c", q=2)
        for qt in range(len(jt)):
            kts = [0, 1]
            if own and m > 0 and qt == 0:
                kts = [0]
            for kt in kts:
                P.mm(pso[:, qt, 0:130], PT[sb][:, kt * W + qt * 128:kt * W + (qt + 1) * 128], Vb[hb][:, 2 * n + kt, :],
                     kt == kts[0], kt == kts[-1], reads=[b_PT[sb], b_Vb[hb]], writes=[b_ps[ob_]])
        for qt in range(len(jt)):
            if own:
                P.emit("dve", lambda e, qt=qt, pso=pso, ab=ab: e.tensor_copy(out=acc[ab][:, qt, :], in_=pso[:, qt, 0:130]),
                       reads=[b_ps[ob_]], writes=[b_acc[ab]])
            else:
                j = jt[qt]
                P.emit("dve", lambda e, qt=qt, pso=pso, ab=ab, j=j, n=n, hb=hb: e.scalar_tensor_tensor(
                    out=acc[ab][:, qt, :], in0=pso[:, qt, 0:130], scalar=sel[hb][:, j, n:n + 1], in1=acc[ab][:, qt, :],
                    op0=ALU.mult, op1=ALU.add), reads=[b_ps[ob_], b_sel[hb], b_acc[ab]], writes=[b_acc[ab]])
        last = (idx + 1 == nitems) or items[idx + 1][3]
        if last:
            nq = len(jt)
            P.emit("dve", lambda e, ab=ab, nq=nq: e.reciprocal(out=rec[ab][:, 0:nq], in_=acc[ab][:, 0:nq, 128]),
                   reads=[b_acc[ab]], writes=[b_acc[ab]])
            for qt in range(nq):
                P.emit("dve", lambda e, ab=ab, qt=qt: e.tensor_scalar(out=obf[ab][:, qt, :], in0=acc[ab][:, qt, 0:128],
                                                                      scalar1=rec[ab][:, qt:qt + 1], scalar2=None, op0=ALU.mult),
                       reads=[b_acc[ab]], writes=[b_obf[ab]])
            for qt in range(nq):
                P.tr(pT[ab][:, qt, :], obf[ab][:, qt, :], ident[:], reads=[b_obf[ab], b_id], writes=[b_pT[ab]])
            c0 = jt[0] * 128
            P.emit("act", lambda e, ab=ab, nq=nq, c0=c0, hb=hb: e.activation(
                out=AOst[hb][:, c0:c0 + nq * 128], in_=pT[ab][:, 0:nq, :], func=AF.Copy),
                reads=[b_pT[ab]], writes=[b_AOst[hb]])
            if m == NO:
                P.dma(AOs[h], AOst[hb][:], reads=[b_AOst[hb]], writes=[b_AO[h]], skey=f"AOst{hb}", eng="pool")

    B_load(0)
    B_gate(0)
    LOOK = 2
    for idx in range(nitems + LOOK):
        if idx < nitems:
            h, m, n, own = items[idx]
            if own and m == NO and h + 1 < H:
                B_gate(h + 1)
            item_S(idx)
        if idx - LOOK >= 0:
            item_PV(idx - LOOK)
            h2, m2, n2, own2 = items[idx - LOOK]
            if own2 and m2 == 0 and h2 + 1 < H:
                B_load(h2 + 1)
        if idx % cj_every == cj_every - 1 and cj_next[0] < ncj:
            conv_jobs[cj_next[0]](["pool"])
            cj_next[0] += 1
    while cj_next[0] < ncj:
        conv_jobs[cj_next[0]](["pool", "dve", "act"])
        cj_next[0] += 1
    P.barrier()
    P.release(m_persist)
    if stop_after == "B":
        P.finalize()
        return nc

    NS = 5
    wslot = [P.sbuf(f"wslot{i}", [128, 4096], BF16) for i in range(NS)]
    b_wslot = [P.buf() for _ in range(NS)]
    xr = [P.sbuf(f"xr{i}", [128, 4, D], F32) for i in range(2)]
    b_xr = [P.buf() for _ in range(2)]
    AOt = [P.sbuf(f"AOt{i}", [128, H, 512], BF16) for i in range(2)]
    b_AOt = [P.buf() for _ in range(2)]
    xnTc = P.sbuf("xnTc", [128, 8, 512], BF16)
    b_xnTc = P.buf()
    hT = P.sbuf("hT", [128, NFC, 512], BF16)
    b_hT = [P.buf() for _ in range(NFC)]
    sgS = [P.sbuf(f"sgS{i}", [128, 512], F32) for i in range(2)]
    b_sgS = [P.buf() for _ in range(2)]
    cS = [P.sbuf(f"cS{i}", [128, 512], F32) for i in range(2)]
    b_cS = [P.buf() for _ in range(2)]
    uT = P.sbuf("uT", [128, 8, 516], F32)
    b_uT = [P.buf() for _ in range(8)]
    t1 = [P.sbuf(f"t1_{i}", [128, 512], F32) for i in range(2)]
    t2 = [P.sbuf(f"t2_{i}", [128, 512], F32) for i in range(2)]
    b_t1 = [P.buf() for _ in range(2)]
    b_t2 = [P.buf() for _ in range(2)]
    zT = P.sbuf("zT", [128, 8, 512], BF16)
    b_zT = [P.buf() for _ in range(8)]
    cwt = P.sbuf("cwt", [128, 8, 3], F32)
    b_cwt = P.buf()
    gfin = P.sbuf("gfin", [128, D], F32)
    b_gfin = P.buf()
    ot = [P.sbuf(f"ot{i}", [128, D], F32) for i in range(2)]
    b_ot = [P.buf() for _ in range(2)]
    NMS = 5 * 4 * (NO // 2 + 1) + 8
    msC = P.sbuf("msC", [128, NMS], F32)
    sdC = P.sbuf("sdC", [128, NMS], F32)
    rsC = P.sbuf("rsC", [128, NMS], F32)
    msc = [0]
    b_out = []
    for k in range(3):
        P.dma(cwt[:, :, k], cw[k].rearrange("(j p) -> p j", p=128), writes=[b_cwt], skey=f"cwt{k}", allow_slow_non_contiguous=True)
    P.dma(gfin[:], n_fin.partition_broadcast(128), writes=[b_gfin], skey="gfin")
    b_msall = P.buf()
    P.emit("pool", lambda e: e.memset(msC[:], 0.0), writes=[b_msall])

    groups = [("halo", 2 * NP - 1, 1, 0)] + [("own", 2 * NP + 4 * g, 4, 1 + 4 * g) for g in range(NO // 2)]
    seq = []
    for (kind, tt0, nt, j0) in groups:
        seq += U_WO + U_FIN[0] + U_FOUT[0] + U_CIN
        if kind == "own":
            seq += U_COUT + U_FIN[1] + U_FOUT[1]
    wst = {"issued": 0, "cur": 0}

    def w_ensure(upto):
        while wst["issued"] <= min(upto, len(seq) - 1):
            i = wst["issued"]
            P.dma(wslot[i % NS][:], WU[seq[i]], reads=[b_WU[seq[i]]], writes=[b_wslot[i % NS]], skey=f"ws{i % NS}")
            wst["issued"] += 1

    def w_get(expect):
        i = wst["cur"]
        assert seq[i] == expect, (i, seq[i], expect)
        w_ensure(i + NS - 1)
        wst["cur"] += 1
        return wslot[i % NS], b_wslot[i % NS]

    def C_load(gi):
        kind, tt0, nt, j0 = groups[gi]
        sl = gi % 2
        T = nt * 128
        P.dma(xr[sl][:, 0:nt, :], xl[tt0 * 128:tt0 * 128 + T, :].rearrange("(s p) d -> p s d", p=128), writes=[b_xr[sl]], skey=f"xr{sl}")
        P.dma(AOt[sl][:, :, 0:T], AOs.rearrange("h d t -> d h t")[:, :, j0 * 128:j0 * 128 + T], reads=b_AO, writes=[b_AOt[sl]],
              skey=f"AOt{sl}")

    def proj_tokmajor(lhs_fn, lhs_reads, nk, units, sl, nt):
        for half in range(2):
            wt, b_wt = w_get(units[half])
            for s in range(nt):
                bi = nbank()
                for k in range(nk):
                    P.mm(psb[bi][:, :], lhs_fn(k, s), wt[:, k * 512:(k + 1) * 512], k == 0, k == nk - 1,
                         reads=lhs_reads + [b_wt], writes=[b_ps[bi]])
                xv = xr[sl][:, s, half * 512:(half + 1) * 512]
                P.emit("dve", lambda e, xv=xv, bi=bi: e.tensor_tensor(out=xv, in0=psb[bi][:, :], in1=xv, op=ALU.add),
                       reads=[b_ps[bi], b_xr[sl]], writes=[b_xr[sl]])

    def do_norm(sl, nt):
        c0 = msc[0]
        msc[0] += nt
        b_ms = P.buf()
        b_ms.last_w = b_msall.last_w
        rms_stats(lambda s: xr[sl][:, s, :], nt, msC, sdC, rsC, c0, b_xr[sl], b_ms)
        return c0, b_ms

    def ffn(l, sl, nt):
        T = nt * 128
        c0, b_ms = do_norm(sl, nt)
        norm_T(lambda s: xr[sl][:, s, :], nt, rsC, c0, b_xr[sl], b_ms, xnTc, b_xnTc)
        for u in range(11):
            wt, b_wt = w_get(U_FIN[l][u])
            wv = wt[:, :].rearrange("p (g c k f) -> p g c k f", g=2, c=2, k=8)
            for c in range(2):
                fc = 2 * u + c
                bg = nbank()
                for kc in range(8):
                    P.mm(psb[bg][:, 0:T], wv[:, 0, c, kc, :], xnTc[:, kc, 0:T], kc == 0, kc == 7, reads=[b_wt, b_xnTc], writes=[b_ps[bg]])
                bu = nbank()
                for kc in range(8):
                    P.mm(psb[bu][:, 0:T], wv[:, 1, c, kc, :], xnTc[:, kc, 0:T], kc == 0, kc == 7, reads=[b_wt, b_xnTc], writes=[b_ps[bu]])
                si = fc % 2
                P.emit("act", lambda e, si=si, bg=bg: e.activation(out=sgS[si][:, 0:T], in_=psb[bg][:, 0:T], func=AF.Silu),
                       reads=[b_ps[bg]], writes=[b_sgS[si]])
                P.emit("dve", lambda e, si=si, bu=bu, fc=fc: e.tensor_tensor(out=hT[:, fc, 0:T], in0=sgS[si][:, 0:T], in1=psb[bu][:, 0:T], op=ALU.mult),
                       reads=[b_sgS[si], b_ps[bu]], writes=[b_hT[fc]])
        for half in range(2):
            banks = [nbank() for _ in range(nt)]
            for k in range(3):
                wt, b_wt = w_get(U_FOUT[l][half * 3 + k])
                f0 = 8 * k
                nfc = min(8, NFC - f0)
                for fi in range(nfc):
                    fc = f0 + fi
                    for s in range(nt):
                        P.mm(psb[banks[s]][:, :], hT[:, fc, s * 128:(s + 1) * 128], wt[:, fi * 512:(fi + 1) * 512], fc == 0, fc == NFC - 1,
                             reads=[b_hT[fc], b_wt], writes=[b_ps[banks[s]]])
            for s in range(nt):
                xv = xr[sl][:, s, half * 512:(half + 1) * 512]
                bi = banks[s]
                P.emit("dve", lambda e, xv=xv, bi=bi: e.tensor_tensor(out=xv, in0=psb[bi][:, :], in1=xv, op=ALU.add),
                       reads=[b_ps[bi], b_xr[sl]], writes=[b_xr[sl]])

    def conv_mixer(sl, nt, halo_only):
        T = nt * 128
        c0, b_ms = do_norm(sl, nt)
        norm_T(lambda s: xr[sl][:, s, :], nt, rsC, c0, b_xr[sl], b_ms, xnTc, b_xnTc)
        for j in range(8):
            wt, b_wt = w_get(U_CIN[j])
            wv = wt[:, 0:3072].rearrange("p (g k f) -> p g k f", g=3, k=8)
            pb = {}
            for t3 in ([1, 2] if halo_only else [0, 1, 2]):
                bi = nbank()
                pb[t3] = bi
                for kc in range(8):
                    P.mm(psb[bi][:, 0:T], wv[:, t3, kc, :], xnTc[:, kc, 0:T], kc == 0, kc == 7, reads=[b_wt, b_xnTc], writes=[b_ps[bi]])
            ci = j % 2
            P.emit("act", lambda e, ci=ci, bi=pb[1]: e.activation(out=cS[ci][:, 0:T], in_=psb[bi][:, 0:T], func=AF.Copy),
                   reads=[b_ps[pb[1]]], writes=[b_cS[ci]])
            P.emit("dve", lambda e, ci=ci, bi=pb[2], j=j: e.tensor_tensor(out=uT[:, j, 2:2 + T], in0=cS[ci][:, 0:T], in1=psb[bi][:, 0:T], op=ALU.mult),
                   reads=[b_cS[ci], b_ps[pb[2]]], writes=[b_uT[j]])
            if halo_only:
                P.emit("dve", lambda e, j=j: e.tensor_scalar(out=uT[:, j, 0:2], in0=uT[:, j, T:T + 2], scalar1=flagt[:, 0:1], scalar2=None, op0=ALU.mult),
                       reads=[b_uT[j], b_flag], writes=[b_uT[j]])
                continue
            P.emit("pool", lambda e, j=j, ci=ci: e.tensor_scalar(out=t1[ci][:, 0:T], in0=uT[:, j, 0:T], scalar1=cwt[:, j, 0:1], scalar2=None, op0=ALU.mult),
                   reads=[b_uT[j], b_cwt], writes=[b_t1[ci]])
            P.emit("pool", lambda e, j=j, ci=ci: e.tensor_scalar(out=t2[ci][:, 0:T], in0=uT[:, j, 1:1 + T], scalar1=cwt[:, j, 1:2], scalar2=None, op0=ALU.mult),
                   reads=[b_uT[j], b_cwt], writes=[b_t2[ci]])
            P.emit("pool", lambda e, ci=ci: e.tensor_tensor(out=t1[ci][:, 0:T], in0=t1[ci][:, 0:T], in1=t2[ci][:, 0:T], op=ALU.add),
                   reads=[b_t1[ci], b_t2[ci]], writes=[b_t1[ci]])
            P.emit("pool", lambda e, j=j, ci=ci: e.tensor_scalar(out=t2[ci][:, 0:T], in0=uT[:, j, 2:2 + T], scalar1=cwt[:, j, 2:3], scalar2=None, op0=ALU.mult),
                   reads=[b_uT[j], b_cwt, b_t1[ci]], writes=[b_t2[ci]])
            P.emit("pool", lambda e, ci=ci: e.tensor_tensor(out=t1[ci][:, 0:T], in0=t1[ci][:, 0:T], in1=t2[ci][:, 0:T], op=ALU.add),
                   reads=[b_t1[ci], b_t2[ci]], writes=[b_t1[ci]])
            P.emit("dve", lambda e, j=j, ci=ci, bi=pb[0]: e.tensor_tensor(out=zT[:, j, 0:T], in0=t1[ci][:, 0:T], in1=psb[bi][:, 0:T], op=ALU.mult),
                   reads=[b_t1[ci], b_ps[pb[0]]], writes=[b_zT[j]])
            P.emit("pool", lambda e, j=j: e.tensor_copy(out=uT[:, j, 0:2], in_=uT[:, j, T:T + 2]), reads=[b_uT[j]], writes=[b_uT[j]])
        if halo_only:
            return
        proj_tokmajor(lambda k, s: zT[:, k, s * 128:(s + 1) * 128], b_zT, 8, U_COUT, sl, nt)

    def final_out(gi, sl, nt):
        c0, b_ms = do_norm(sl, nt)
        g = gi - 1
        for s in range(nt):
            oi = s % 2
            P.emit("dve", lambda e, s=s, oi=oi: e.scalar_tensor_tensor(out=ot[oi][:], in0=xr[sl][:, s, :], scalar=rsC[:, c0 + s:c0 + s + 1], in1=gfin[:],
                                                                       op0=ALU.mult, op1=ALU.mult),
                   reads=[b_xr[sl], b_ms, b_gfin], writes=[b_ot[oi]])
            bo = P.buf()
            r0 = g * 512 + s * 128
            P.dma(out[r0:r0 + 128, :], ot[oi][:], reads=[b_ot[oi]], writes=[bo], skey=f"ot{oi}", eng="pool")
            b_out.append(bo)

    C_load(0)
    for gi, (kind, tt0, nt, j0) in enumerate(groups):
        sl = gi % 2
        if gi + 1 < len(groups):
            C_load(gi + 1)
        proj_tokmajor(lambda k, s: AOt[sl][:, k, s * 128:(s + 1) * 128], [b_AOt[sl]], H, U_WO, sl, nt)
        ffn(0, sl, nt)
        conv_mixer(sl, nt, kind == "halo")
        if kind == "own":
            ffn(1, sl, nt)
            final_out(gi, sl, nt)
    P.wait_only("sp", b_out)
    P.wait_only("pool", b_out)
    P.finalize()
    return nc


def make_masks(NP, NO, half):
    NB = NP + NO
    NQT = 2 * NO + 1
    ind = np.zeros((NQT, NB), np.float32)
    for j in range(NQT):
        ob = NP - 1 + (j + 1) // 2
        lo = 0 if half == 1 else NP
        for n in range(NB):
            if lo <= n < ob:
                ind[j, n] = 1.0
    neg = np.where(ind > 0, 0.0, NEGBIG).astype(np.float32)
    ind_b = np.ascontiguousarray(np.broadcast_to(ind.reshape(1, -1), (128, NQT * NB)))
    neg_b = np.ascontiguousarray(np.broadcast_to(neg.reshape(1, -1), (128, NQT * NB)))
    return neg_b, ind_b


_PROG_CACHE = {}


def run_module(inputs, debug=False, stop_after=None):
    x = np.asarray(inputs["x"], np.float32)
    B, S, _ = x.shape
    half_t = S // 2
    NP = NO = half_t // 256
    ncores = 2 * B
    key = (NP, NO, debug, stop_after)
    if key not in _PROG_CACHE:
        _PROG_CACHE[key] = build_program(NP, NO, debug=debug, stop_after=stop_after)
    nc = _PROG_CACHE[key]
    f = lambda k: np.ascontiguousarray(np.asarray(inputs[k], np.float32))
    shared = {
        "wqkv": f("attn_w_qkv")[0], "wo": f("attn_w_o")[0], "cwin": f("conv_w_in")[0], "cw": f("conv_w")[0],
        "cwout": f("conv_w_out")[0], "fwin": f("ffn_w_in"), "fwout": f("ffn_w_out"),
        "n_attn": f("attn_norm")[0], "n_conv": f("conv_norm")[0], "n_ffn": f("ffn_norm"), "n_fin": f("final_norm"),
    }
    in_maps = []
    for c in range(ncores):
        b, half = c // 2, c % 2
        xl = np.zeros((2 * half_t, D), np.float32)
        if half == 1:
            xl[:half_t] = x[b, :half_t]
        xl[half_t:] = x[b, half * half_t:(half + 1) * half_t]
        neg, ind = make_masks(NP, NO, half)
        m = dict(shared)
        m.update({"xl": xl, "pastneg": neg, "pastind": ind, "flag": np.full((128, 1), float(half), np.float32)})
        in_maps.append(m)
    res = run_bass_kernel_spmd(nc, in_maps, core_ids=list(range(ncores)))
    out = np.zeros((B, S, D), np.float32)
    for c in range(ncores):
        b, half = c // 2, c % 2
        out[b, half * half_t:(half + 1) * half_t] = res.results[c]["out"]
    return out, res


def kernel(**inputs):
    out, _ = run_module(inputs)
    return out
```

```python
import contextlib
import numpy as np
import ml_dtypes
import concourse.bass as bass
import concourse.mybir as mybir
from concourse.bass_utils import run_bass_kernel_spmd

F32 = mybir.dt.float32
BF16 = mybir.dt.bfloat16
AF = mybir.ActivationFunctionType
ALU = mybir.AluOpType
AX = mybir.AxisListType

SEM_LIMIT = 30000


class Buf:
    __slots__ = ("name", "last_w", "readers", "dma_readers")

    def __init__(self, name):
        self.name = name
        self.last_w = None
        self.readers = {}
        self.dma_readers = []


class Op:
    __slots__ = ("eng", "fn", "waits", "signal", "sem", "val", "is_dma", "skey", "idx")

    def __init__(self, eng, fn, is_dma, skey):
        self.eng = eng
        self.fn = fn
        self.waits = []
        self.signal = False
        self.sem = None
        self.val = 0
        self.is_dma = is_dma
        self.skey = skey


class Prog:
    ENGS = ("pe", "act", "dve", "pool", "sp")

    def __init__(self, nc):
        self.nc = nc
        self.ops = {e: [] for e in self.ENGS}
        self.stack = contextlib.ExitStack()
        self.nbuf = 0
        self.sb_base = (nc.sbuf_base + 63) // 64 * 64
        self.sb_ptr = self.sb_base
        self.sb_top = nc.sbuf_top
        self.sb_n = 0
        self.sb_peak = 0
        self.last_comp = {}
        self.dma_since = {}

    def sbuf(self, name, shape, dtype):
        esz = 4 if dtype == F32 else 2
        size = esz
        for d in shape[1:]:
            size *= d
        size = (size + 63) // 64 * 64
        off = self.sb_ptr
        self.sb_ptr += size
        assert self.sb_ptr <= self.sb_top, (name, self.sb_ptr, self.sb_top)
        self.sb_peak = max(self.sb_peak, self.sb_ptr)
        self.sb_n += 1
        return self.nc.alloc_sbuf_tensor_at(f"{name}{self.sb_n}", list(shape), dtype, offset=off)

    def mark(self):
        return self.sb_ptr

    def release(self, mark):
        self.sb_ptr = mark

    def barrier(self):
        deps = list(self.last_comp.values()) + list(self.dma_since.values())
        for e in self.ENGS:
            op = Op(e, lambda eng: None, False, None)
            for w in deps:
                if w.eng == e and not w.is_dma:
                    continue
                w.signal = True
                op.waits.append(w)
            self.ops[e].append(op)
        self.dma_since = {}

    def psum(self, name, shape, dtype):
        return self.stack.enter_context(self.nc.psum_tensor(name, list(shape), dtype))

    def buf(self, name=None):
        self.nbuf += 1
        return Buf(name or f"b{self.nbuf}")

    def wait_only(self, eng, reads):
        op = Op(eng, lambda e: None, False, None)
        for b in reads:
            w = b.last_w
            if w is not None and w not in op.waits:
                w.signal = True
                op.waits.append(w)
        self.ops[eng].append(op)
        return op

    def emit(self, eng, fn, reads=(), writes=(), dma=False, skey=None):
        op = Op(eng, fn, dma, skey)
        waits = []
        for b in reads:
            if b.last_w is not None:
                waits.append(b.last_w)
        for b in writes:
            w = b.last_w
            if w is not None and (w.eng != eng or w.is_dma or dma):
                waits.append(w)
            for e, r in b.readers.items():
                if e != eng or dma:
                    waits.append(r)
            for r in b.dma_readers:
                waits.append(r)
        seen = set()
        for w in waits:
            if id(w) in seen or w is op:
                continue
            seen.add(id(w))
            w.signal = True
            op.waits.append(w)
        for b in reads:
            if dma:
                b.dma_readers.append(op)
            else:
                b.readers[eng] = op
        for b in writes:
            b.last_w = op
            b.readers = {}
            b.dma_readers = []
        if dma:
            op.signal = True
            self.dma_since[skey] = op
        else:
            self.last_comp[eng] = op
        self.ops[eng].append(op)
        return op

    def dma(self, out, in_, reads=(), writes=(), skey=None, eng="sp", **kw):
        assert skey is not None
        return self.emit(eng, lambda e: e.dma_start(out=out, in_=in_, **kw),
                         reads=reads, writes=writes, dma=True, skey=skey)

    def mm(self, out, lhsT, rhs, start, stop, reads=(), writes=()):
        return self.emit("pe", lambda e: e.matmul(out, lhsT, rhs, start=start, stop=stop),
                         reads=reads, writes=writes)

    def tr(self, out, in_, ident, reads=(), writes=()):
        return self.emit("pe", lambda e: e.transpose(out, in_, ident), reads=reads, writes=writes)

    def finalize(self):
        nc = self.nc
        sem_stack = self.stack
        nsem = [0]

        def new_sem(tag):
            nsem[0] += 1
            return sem_stack.enter_context(nc.semaphore(f"s_{tag}_{nsem[0]}"))

        for e in self.ENGS:
            cur = None
            cnt = 0
            for op in self.ops[e]:
                if op.is_dma or not op.signal:
                    continue
                if cur is None or cnt >= SEM_LIMIT:
                    cur = new_sem(e)
                    cnt = 0
                cnt += 1
                op.sem, op.val = cur, cnt
        keysem = {}
        for e in self.ENGS:
            for op in self.ops[e]:
                if not op.is_dma:
                    continue
                if op.skey not in keysem or keysem[op.skey][1] >= SEM_LIMIT:
                    keysem[op.skey] = [new_sem("d"), 0]
                ks = keysem[op.skey]
                ks[1] += 16
                op.sem, op.val = ks[0], ks[1]
        self.n_sems = nsem[0]

        with nc.Block() as block:
            def runner(ename):
                def run(eng):
                    seen = {}
                    for op in self.ops[ename]:
                        need = {}
                        for w in op.waits:
                            k = id(w.sem)
                            if seen.get(k, 0) >= w.val:
                                continue
                            if k not in need or need[k][1] < w.val:
                                need[k] = (w.sem, w.val)
                        for k, (s, v) in need.items():
                            eng.wait_ge(s, v)
                            seen[k] = v
                        ins = op.fn(eng)
                        if ins is None:
                            assert not op.signal
                            continue
                        if op.signal and op.sem is not None:
                            ins.then_inc(op.sem, 16 if op.is_dma else 1)
                return run

            block.tensor(runner("pe"))
            block.scalar(runner("act"))
            block.vector(runner("dve"))
            block.gpsimd(runner("pool"))
            block.sync(runner("sp"))
        self.stack.close()


D = 1024
H = 8
F = 2816
NFC = 22
EPS = 1e-6
QSCALE = 128.0 ** -0.5
NEGBIG = -30000.0


def build_program(NP, NO, debug=False, stop_after=None, conv_in_b=True):
    nc = bass.Bass("TRN2", target_bir_lowering=False)
    NB = NP + NO
    LT = NB * 256
    NT = LT // 128
    NG = LT // 512
    NQT = 2 * NO + 1
    GQ0 = NP // 2 - 1
    NQG = NG - GQ0
    QW = NQG * 512
    NAO = NQT * 128
    OUT_T = NO * 256

    def din(name, shape):
        return nc.dram_tensor(name, list(shape), F32, kind="ExternalInput").ap()

    xl = din("xl", [LT, D])
    wqkv = din("wqkv", [D, 3 * D])
    wo = din("wo", [D, D])
    cwin = din("cwin", [D, 3 * D])
    cw = din("cw", [3, D])
    cwout = din("cwout", [D, D])
    fwin = din("fwin", [2, D, 2 * F])
    fwout = din("fwout", [2, F, D])
    n_attn = din("n_attn", [D])
    n_conv = din("n_conv", [D])
    n_ffn = din("n_ffn", [2, D])
    n_fin = din("n_fin", [D])
    pastneg = din("pastneg", [128, NQT * NB])
    pastind = din("pastind", [128, NQT * NB])
    flag = din("flag", [128, 1])
    out = nc.dram_tensor("out", [OUT_T, D], F32, kind="ExternalOutput").ap()
    sk = "ExternalOutput" if debug else "Internal"
    KTs = nc.dram_tensor("KTs", [H, 128, LT], BF16, kind=sk).ap()
    Vs = nc.dram_tensor("Vs", [H, 128, NT, 130], BF16, kind=sk).ap()
    QTs = nc.dram_tensor("QTs", [H, 128, QW], BF16, kind=sk).ap()
    AOs = nc.dram_tensor("AOs", [H, 128, NAO], BF16, kind=sk).ap()
    NU = 46
    WU = nc.dram_tensor("WU", [NU, 128, 4096], BF16, kind="Internal").ap()
    U_WO = [0, 1]
    U_FIN = [[2 + l * 17 + u for u in range(11)] for l in range(2)]
    U_FOUT = [[2 + l * 17 + 11 + k for k in range(6)] for l in range(2)]
    U_CIN = [36 + j for j in range(8)]
    U_COUT = [44, 45]

    P = Prog(nc)
    psb = [P.psum(f"psb{i}", [128, 512], F32) for i in range(6)]
    b_ps = [P.buf(f"ps{i}") for i in range(6)]
    pT = [P.psum(f"pT{i}", [128, 8, 128], BF16) for i in range(2)]
    b_pT = [P.buf(f"pT{i}") for i in range(2)]
    psrr = [0]

    def nbank():
        i = psrr[0] % 6
        psrr[0] += 1
        return i

    identf = P.sbuf("identf", [128, 128], F32)
    ident = P.sbuf("ident", [128, 128], BF16)
    tri = P.sbuf("tri", [128, 128], BF16)
    gcols = P.sbuf("gcols", [128, 4, 8], F32)
    kms = P.sbuf("kms", [128, H, NB], F32)
    kmb = P.sbuf("kmb", [128, H, NB], BF16)
    flagt = P.sbuf("flagt", [128, 1], F32)
    b_idf, b_id, b_tri, b_g, b_kms, b_kmb, b_flag = [P.buf() for _ in range(7)]
    P.emit("pool", lambda e: e.memset(identf[:], 0.0), writes=[b_idf])
    P.emit("pool", lambda e: e.affine_select(out=identf[:], in_=identf[:], pattern=[[-1, 128]],
                                             compare_op=ALU.not_equal, fill=1.0, base=0, channel_multiplier=1),
           reads=[b_idf], writes=[b_idf])
    P.emit("dve", lambda e: e.tensor_copy(out=ident[:], in_=identf[:]), reads=[b_idf], writes=[b_id])
    P.emit("pool", lambda e: e.memset(identf[:], 1.0), reads=[b_idf], writes=[b_idf])
    P.emit("pool", lambda e: e.affine_select(out=identf[:], in_=identf[:], pattern=[[1, 128]],
                                             compare_op=ALU.is_ge, fill=0.0, base=0, channel_multiplier=-1),
           reads=[b_idf], writes=[b_idf])
    P.emit("dve", lambda e: e.tensor_copy(out=tri[:], in_=identf[:]), reads=[b_idf], writes=[b_tri])
    for i, src in enumerate([n_attn, n_conv, n_ffn[0], n_ffn[1]]):
        P.dma(gcols[:, i, :], src.rearrange("(kc p) -> p kc", p=128), writes=[b_g], skey=f"g{i}",
              allow_slow_non_contiguous=True)
    P.dma(flagt[:], flag, writes=[b_flag], skey="flag")
    gimg = P.sbuf("gimg", [128, 3, 8, 128], F32)
    b_gimg = P.buf()
    P.emit("pool", lambda e: e.memset(gimg[:], 1.0), writes=[b_gimg])
    for i in range(3):
        for kc in range(8):
            P.emit("dve", lambda e, i=i, kc=kc: e.tensor_scalar(out=gimg[:, i, kc, :], in0=gimg[:, i, kc, :], scalar1=gcols[:, i + 1, kc:kc + 1],
                                                                scalar2=None, op0=ALU.mult), reads=[b_gimg, b_g], writes=[b_gimg])
    P.emit("pool", lambda e: e.memset(kms[:], 0.0), writes=[b_kms])

    b_WU = [P.buf(f"WU{u}") for u in range(NU)]
    rr = [0]

    def make_conv_jobs(stg, b_stg, img, b_img):
        jobs = []
        cnt = [0]

        def conv_ops(slot, views, gidx, eng_list):
            for (iv, sv, G) in views:
                eng = eng_list[rr[0] % len(eng_list)]
                rr[0] += 1
                rd = [b for b in b_stg[slot]]
                if G is None:
                    if eng == "act":
                        P.emit("act", lambda e, iv=iv, sv=sv: e.activation(out=iv, in_=sv, func=AF.Copy),
                               reads=rd, writes=[b_img[slot]])
                    else:
                        P.emit(eng, lambda e, iv=iv, sv=sv: e.tensor_copy(out=iv, in_=sv), reads=rd, writes=[b_img[slot]])
                else:
                    if eng == "act":
                        eng = "pool"
                    gv = gimg[:, gidx - 1, :, :].unsqueeze(1).broadcast_to([128, G, 8, 128])
                    P.emit(eng, lambda e, iv=iv, sv=sv, gv=gv: e.tensor_tensor(out=iv, in0=sv, in1=gv, op=ALU.mult),
                           reads=rd + [b_gimg], writes=[b_img[slot]])

        def job(uidx, loads, views, gidx):
            def run(eng_list):
                slot = cnt[0] % 2
                cnt[0] += 1
                for i, (sv, src) in enumerate(loads):
                    P.dma(sv(stg[slot]), src, writes=[b_stg[slot][i]], skey=f"stg{slot}_{i}")
                conv_ops(slot, [(iv(img[slot]), sv(stg[slot]), kc) for (iv, sv, kc) in views], gidx, eng_list)
                P.dma(WU[uidx], img[slot][:], reads=[b_img[slot]], writes=[b_WU[uidx]], skey=f"img{slot}", eng="pool")
            return run

        def v3(a, b):
            return lambda t: t[:, 0:a * b].rearrange("p (a b) -> p a b", a=a)

        for srcw, units in ((wo, U_WO), (cwout, U_COUT)):
            for n in range(2):
                src = srcw.rearrange("(h p) c -> p h c", p=128)[:, :, n * 512:(n + 1) * 512]
                jobs.append(job(units[n], [(v3(8, 512), src)],
                                [(lambda t: t[:, :], lambda t: t[:, :], None)], None))
        for l in range(2):
            for u in range(11):
                loads = []
                for gu in range(2):
                    for c in range(2):
                        col0 = gu * F + (2 * u + c) * 128
                        src = fwin[l].rearrange("(kc p) n -> p kc n", p=128)[:, :, col0:col0 + 128]
                        o = (gu * 2 + c) * 1024
                        loads.append((lambda t, o=o: t[:, o:o + 1024].rearrange("p (a b) -> p a b", a=8), src))
                vw = lambda t: t[:, :].rearrange("p (g k f) -> p g k f", g=4, k=8)
                jobs.append(job(U_FIN[l][u], loads, [(vw, vw, 4)], 2 + l))
        for l in range(2):
            for half in range(2):
                for k in range(3):
                    f0 = 8 * k
                    nfc = min(8, NFC - f0)
                    src = fwout[l].rearrange("(fc p) c -> p fc c", p=128)[:, f0:f0 + nfc, half * 512:(half + 1) * 512]
                    jobs.append(job(U_FOUT[l][half * 3 + k], [(v3(nfc, 512), src)],
                                    [(lambda t, n=nfc: t[:, 0:n * 512], lambda t, n=nfc: t[:, 0:n * 512], None)], None))
        for j in range(8):
            loads = []
            for t3 in range(3):
                col0 = t3 * D + j * 128
                src = cwin.rearrange("(kc p) n -> p kc n", p=128)[:, :, col0:col0 + 128]
                o = t3 * 1024
                loads.append((lambda t, o=o: t[:, o:o + 1024].rearrange("p (a b) -> p a b", a=8), src))
            vw = lambda t: t[:, 0:3072].rearrange("p (g k f) -> p g k f", g=3, k=8)
            jobs.append(job(U_CIN[j], loads, [(vw, vw, 3)], 1))
        return jobs

    m_persist = P.mark()

    Wsb = P.sbuf("Wsb", [128, 8, 3 * D], BF16)
    b_Wp = [[P.buf(f"W{part}_{kc}") for kc in range(8)] for part in range(3)]
    stgA = [P.sbuf(f"stgA{i}", [128, D], F32) for i in range(4)]
    b_stgA = [P.buf() for _ in range(4)]
    wi = 0
    for part in (1, 2, 0):
        for kc in range(8):
            sl = wi % 4
            P.dma(stgA[sl][:], wqkv[kc * 128:(kc + 1) * 128, part * D:(part + 1) * D], writes=[b_stgA[sl]], skey=f"stgA{sl}")
            ov = Wsb[:, kc, part * D:(part + 1) * D]
            iv = stgA[sl][:]
            gs = gcols[:, 0, kc:kc + 1]
            if wi % 2 == 0:
                P.emit("act", lambda e, ov=ov, iv=iv, gs=gs: e.activation(out=ov, in_=iv, func=AF.Copy, scale=gs),
                       reads=[b_stgA[sl], b_g], writes=[b_Wp[part][kc]])
            else:
                P.emit("dve", lambda e, ov=ov, iv=iv, gs=gs: e.tensor_scalar(out=ov, in0=iv, scalar1=gs, scalar2=None, op0=ALU.mult),
                       reads=[b_stgA[sl], b_g], writes=[b_Wp[part][kc]])
            wi += 1

    xs = [P.sbuf(f"xs{i}", [128, 4, D], F32) for i in range(2)]
    b_xs = [P.buf() for _ in range(2)]
    junk = P.sbuf("junk", [128, D], BF16)
    b_junk = P.buf()
    msA = P.sbuf("msA", [128, NT], F32)
    sdA = P.sbuf("sdA", [128, NT], F32)
    rsA = P.sbuf("rsA", [128, NT], F32)
    b_msA = [P.buf() for _ in range(NG)]
    xn = [P.sbuf(f"xn{i}", [128, D], BF16) for i in range(2)]
    b_xn = [P.buf() for _ in range(2)]
    xnT = [P.sbuf(f"xnT{i}", [128, 8, 512], BF16) for i in range(2)]
    b_xnT = [P.buf() for _ in range(2)]
    Kst = [P.sbuf(f"Kst{i}", [128, H, 512], BF16) for i in range(2)]
    Qst = [P.sbuf(f"Qst{i}", [128, H, 512], BF16) for i in range(2)]
    Vst = [P.sbuf(f"Vst{i}", [128, H, 4, 130], BF16) for i in range(2)]
    b_Kst = [P.buf() for _ in range(2)]
    b_Qst = [P.buf() for _ in range(2)]
    b_Vst = [P.buf() for _ in range(2)]
    b_KT = [P.buf() for _ in range(NG)]
    b_V = [P.buf() for _ in range(NG)]
    b_Q = [P.buf() for _ in range(NG)]
    P.emit("pool", lambda e: e.memset(msA[:], 0.0), writes=b_msA)
    for i in range(2):
        P.emit("pool", lambda e, i=i: e.memset(Vst[i][:, :, :, 128:129], 1.0), writes=[b_Vst[i]])
        P.emit("pool", lambda e, i=i: e.memset(Vst[i][:, :, :, 129:130], 0.0), writes=[b_Vst[i]])

    def rms_stats(xtile_fn, nt, ms, sd, rs, c0, b_x, b_ms):
        for s in range(nt):
            P.emit("act", lambda e, s=s: e.activation(out=junk[:], in_=xtile_fn(s), func=AF.Square, scale=1.0 / 32,
                                                      accum_out=ms[:, c0 + s:c0 + s + 1]),
                   reads=[b_x], writes=[b_junk, b_ms])
        P.emit("dve", lambda e: e.tensor_scalar(out=sd[:, c0:c0 + nt], in0=ms[:, c0:c0 + nt], scalar1=EPS, scalar2=None, op0=ALU.add),
               reads=[b_ms], writes=[b_ms])
        P.emit("act", lambda e: e.activation(out=sd[:, c0:c0 + nt], in_=sd[:, c0:c0 + nt], func=AF.Sqrt),
               reads=[b_ms], writes=[b_ms])
        P.emit("dve", lambda e: e.reciprocal(out=rs[:, c0:c0 + nt], in_=sd[:, c0:c0 + nt]), reads=[b_ms], writes=[b_ms])

    def norm_T(xtile_fn, nt, rs, c0, b_x, b_ms, xnT_t, b_xnT_t):
        for s in range(nt):
            i = s % 2
            P.emit("act", lambda e, s=s, i=i: e.activation(out=xn[i][:], in_=xtile_fn(s), func=AF.Copy,
                                                           scale=rs[:, c0 + s:c0 + s + 1]),
                   reads=[b_x, b_ms], writes=[b_xn[i]])
            for kc in range(8):
                P.tr(pT[i][:, kc, :], xn[i][:, kc * 128:(kc + 1) * 128], ident[:], reads=[b_xn[i], b_id], writes=[b_pT[i]])
            P.emit("dve", lambda e, s=s, i=i: e.tensor_copy(out=xnT_t[:, :, s * 128:(s + 1) * 128], in_=pT[i][:]),
                   reads=[b_pT[i]], writes=[b_xnT_t])

    def A_front(g):
        sl = g % 2
        P.dma(xs[sl][:], xl[g * 512:(g + 1) * 512, :].rearrange("(s p) d -> p s d", p=128), writes=[b_xs[sl]], skey=f"xs{sl}")
        rms_stats(lambda s: xs[sl][:, s, :], 4, msA, sdA, rsA, g * 4, b_xs[sl], b_msA[g])
        norm_T(lambda s: xs[sl][:, s, :], 4, rsA, g * 4, b_xs[sl], b_msA[g], xnT[sl], b_xnT[sl])

    evr = [0]

    def A_back(g):
        sl = g % 2
        xt_ = xnT[sl]
        for h in range(H):
            bi = nbank()
            for kc in range(8):
                P.mm(psb[bi][:, :], Wsb[:, kc, D + h * 128:D + (h + 1) * 128], xt_[:, kc, :], kc == 0, kc == 7,
                     reads=[b_Wp[1][kc], b_xnT[sl]], writes=[b_ps[bi]])
            for blk in range(2):
                P.emit("act", lambda e, h=h, bi=bi, blk=blk: e.activation(
                    out=Kst[sl][:, h, blk * 256:(blk + 1) * 256], in_=psb[bi][:, blk * 256:(blk + 1) * 256], func=AF.Copy,
                    accum_out=kms[:, h, 2 * g + blk:2 * g + blk + 1]),
                    reads=[b_ps[bi]], writes=[b_Kst[sl], b_kms])
        P.dma(KTs.rearrange("h d t -> d h t")[:, :, g * 512:(g + 1) * 512], Kst[sl][:], reads=[b_Kst[sl]], writes=[b_KT[g]],
              skey=f"Kst{sl}", eng="pool")
        if g >= GQ0:
            for h in range(H):
                bi = nbank()
                for kc in range(8):
                    P.mm(psb[bi][:, :], Wsb[:, kc, h * 128:(h + 1) * 128], xt_[:, kc, :], kc == 0, kc == 7,
                         reads=[b_Wp[0][kc], b_xnT[sl]], writes=[b_ps[bi]])
                P.emit("dve", lambda e, h=h, bi=bi: e.tensor_scalar(out=Qst[sl][:, h, :], in0=psb[bi][:, :], scalar1=QSCALE,
                                                                   scalar2=None, op0=ALU.mult),
                       reads=[b_ps[bi]], writes=[b_Qst[sl]])
            gq = g - GQ0
            P.dma(QTs.rearrange("h d t -> d h t")[:, :, gq * 512:(gq + 1) * 512], Qst[sl][:], reads=[b_Qst[sl]], writes=[b_Q[g]],
                  skey=f"Qst{sl}", eng="pool")
        for s in range(4):
            for half in range(2):
                bi = nbank()
                for kc in range(8):
                    P.mm(psb[bi][:, :], xt_[:, kc, s * 128:(s + 1) * 128], Wsb[:, kc, 2 * D + half * 512:2 * D + (half + 1) * 512],
                         kc == 0, kc == 7, reads=[b_Wp[2][kc], b_xnT[sl]], writes=[b_ps[bi]])
                ov = Vst[sl][:, half * 4:(half + 1) * 4, s, 0:128]
                iv = psb[bi][:, :].rearrange("p (h d) -> p h d", h=4)
                evr[0] += 1
                if evr[0] % 2 == 0:
                    P.emit("act", lambda e, ov=ov, iv=iv: e.activation(out=ov, in_=iv, func=AF.Copy),
                           reads=[b_ps[bi]], writes=[b_Vst[sl]])
                else:
                    P.emit("dve", lambda e, ov=ov, iv=iv: e.tensor_copy(out=ov, in_=iv), reads=[b_ps[bi]], writes=[b_Vst[sl]])
        P.dma(Vs.rearrange("h p t c -> p h t c")[:, :, g * 4:(g + 1) * 4, :], Vst[sl][:], reads=[b_Vst[sl]], writes=[b_V[g]],
              skey=f"Vst{sl}", eng="pool")

    A_front(0)
    for g in range(NG):
        if g + 1 < NG:
            A_front(g + 1)
        A_back(g)
    P.emit("dve", lambda e: e.tensor_copy(out=kmb[:], in_=kms[:]), reads=[b_kms], writes=[b_kmb])
    P.barrier()
    P.release(m_persist)
    if stop_after == "A":
        P.finalize()
        return nc

    KT = [P.sbuf(f"KT{i}", [128, LT], BF16) for i in range(2)]
    Vb = [P.sbuf(f"Vb{i}", [128, NT, 130], BF16) for i in range(2)]
    QT = [P.sbuf(f"QT{i}", [128, QW], BF16) for i in range(2)]
    AOst = [P.sbuf(f"AOst{i}", [128, NAO], BF16) for i in range(2)]
    b_KTb = [P.buf() for _ in range(2)]
    b_Vb = [P.buf() for _ in range(2)]
    b_QTb = [P.buf() for _ in range(2)]
    b_AOst = [P.buf() for _ in range(2)]
    b_AO = [P.buf() for _ in range(H)]
    pneg = P.sbuf("pneg", [128, NQT * NB], F32)
    pind = P.sbuf("pind", [128, NQT * NB], F32)
    b_pm = P.buf()
    P.dma(pneg[:], pastneg, writes=[b_pm], skey="pneg")
    P.dma(pind[:], pastind, writes=[b_pm], skey="pind")
    gm = P.sbuf("gm", [128, NQT, NB], F32)
    top8 = P.sbuf("top8", [128, NQT, 8], F32)
    sel = [P.sbuf(f"sel{i}", [128, NQT, NB], F32) for i in range(2)]
    b_gm = P.buf()
    b_top8 = P.buf()
    b_sel = [P.buf() for _ in range(2)]
    PT = [P.sbuf(f"PT{i}", [128, 512], BF16) for i in range(3)]
    b_PT = [P.buf() for _ in range(3)]
    acc = [P.sbuf(f"acc{i}", [128, 2, 130], F32) for i in range(2)]
    b_acc = [[P.buf() for _ in range(2)] for _ in range(2)]
    rec = [P.sbuf(f"rec{i}", [128, 2], F32) for i in range(2)]
    obf = [P.sbuf(f"obf{i}", [128, 2, 128], BF16) for i in range(2)]
    b_obf = [P.buf() for _ in range(2)]
    b_rec = [P.buf() for _ in range(2)]
    stgB = [P.sbuf(f"stgB{i}", [128, 4096], F32) for i in range(2)]
    imgB = [P.sbuf(f"imgB{i}", [128, 4096], BF16) for i in range(2)]
    b_stgB = [[P.buf() for _ in range(4)] for _ in range(2)]
    b_imgB = [P.buf() for _ in range(2)]
    conv_jobs = make_conv_jobs(stgB, b_stgB, imgB, b_imgB)

    def B_load(h):
        hb = h % 2
        P.dma(KT[hb][:], KTs[h], reads=b_KT, writes=[b_KTb[hb]], skey=f"KT{hb}")
        P.dma(Vb[hb][:], Vs[h], reads=b_V, writes=[b_Vb[hb]], skey=f"Vb{hb}")
        P.dma(QT[hb][:], QTs[h], reads=[b for b in b_Q[GQ0:]], writes=[b_QTb[hb]], skey=f"QT{hb}")

    def B_gate(h):
        hb = h % 2
        j0 = 0
        while j0 < NQT:
            j1 = min(NQT, j0 + 512 // NB)
            for j in range(j0, j1):
                P.mm(psb[5][:, (j - j0) * NB:(j - j0 + 1) * NB], QT[hb][:, (j + 3) * 128:(j + 4) * 128], kmb[:, h, :], True, True,
                     reads=[b_QTb[hb], b_kmb], writes=[b_ps[5]])
            P.emit("dve", lambda e, j0=j0, j1=j1: e.tensor_tensor(
                out=gm[:, j0:j1, :], in0=psb[5][:, 0:(j1 - j0) * NB].rearrange("p (j n) -> p j n", n=NB),
                in1=pneg[:, j0 * NB:j1 * NB].rearrange("p (j n) -> p j n", n=NB), op=ALU.add),
                reads=[b_ps[5], b_pm], writes=[b_gm])
            j0 = j1
        for j in range(NQT):
            P.emit("dve", lambda e, j=j: e.max(out=top8[:, j, :], in_=gm[:, j, :]), reads=[b_gm], writes=[b_top8])
        for j in range(NQT):
            P.emit("dve", lambda e, j=j: e.scalar_tensor_tensor(
                out=sel[hb][:, j, :], in0=gm[:, j, :], scalar=top8[:, j, 2:3], in1=pind[:, j * NB:(j + 1) * NB],
                op0=ALU.is_ge, op1=ALU.mult), reads=[b_gm, b_top8, b_pm], writes=[b_sel[hb]])

    items = []
    for h in range(H):
        for m in range(NO + 1):
            ob = NP - 1 + m
            items.append((h, m, ob, True))
            for n in range(ob):
                items.append((h, m, n, False))
    nitems = len(items)
    ncj = len(conv_jobs)
    cj_every = max(1, (nitems - 40) // ncj)
    cj_next = [0]
    scount = [0]
    ocount = [0]
    item_state = {}

    def qinfo(m):
        if m == 0:
            return [0], 3 * 128, 128
        return [2 * m - 1, 2 * m], (2 * m + 2) * 128, 256

    def item_S(idx):
        h, m, n, own = items[idx]
        hb = h % 2
        jt, qc, W = qinfo(m)
        sb = scount[0] % 3
        scount[0] += 1
        item_state[idx] = sb
        for kt in range(2):
            P.mm(psb[sb][:, kt * W:(kt + 1) * W], KT[hb][:, (2 * n + kt) * 128:(2 * n + kt + 1) * 128], QT[hb][:, qc:qc + W],
                 True, True, reads=[b_KTb[hb], b_QTb[hb]], writes=[b_ps[sb]])
        P.emit("act", lambda e, sb=sb, W=W: e.activation(out=PT[sb][:, 0:2 * W], in_=psb[sb][:, 0:2 * W], func=AF.Exp),
               reads=[b_ps[sb]], writes=[b_PT[sb]])
        if own:
            if m == 0:
                P.emit("pool", lambda e, sb=sb, W=W: e.tensor_tensor(out=PT[sb][:, W:W + 128], in0=PT[sb][:, W:W + 128], in1=tri[:], op=ALU.mult),
                       reads=[b_PT[sb], b_tri], writes=[b_PT[sb]])
            else:
                P.emit("pool", lambda e, sb=sb: e.tensor_tensor(out=PT[sb][:, 0:128], in0=PT[sb][:, 0:128], in1=tri[:], op=ALU.mult),
                       reads=[b_PT[sb], b_tri], writes=[b_PT[sb]])
                P.emit("pool", lambda e, sb=sb, W=W: e.tensor_tensor(out=PT[sb][:, W + 128:W + 256], in0=PT[sb][:, W + 128:W + 256], in1=tri[:], op=ALU.mult),
                       reads=[b_PT[sb], b_tri], writes=[b_PT[sb]])

    def item_PV(idx):
        h, m, n, own = items[idx]
        hb = h % 2
        jt, qc, W = qinfo(m)
        sb = item_state.pop(idx)
        ob_ = 3 + (ocount[0] % 2)
        ocount[0] += 1
        ab = m % 2
        pso = psb[ob_][:, :].rearrange("p (q c) -> p q c", q=2)
        for qt in range(len(jt)):
            kts = [0, 1]
            if own and m > 0 and qt == 0:
                kts = [0]
            for kt in kts:
                P.mm(pso[:, qt, 0:130], PT[sb][:, kt * W + qt * 128:kt * W + (qt + 1) * 128], Vb[hb][:, 2 * n + kt, :],
                     kt == kts[0], kt == kts[-1], reads=[b_PT[sb], b_Vb[hb]], writes=[b_ps[ob_]])
        for qt in range(len(jt)):
            if own:
                P.emit("dve", lambda e, qt=qt, pso=pso, ab=ab: e.tensor_copy(out=acc[ab][:, qt, :], in_=pso[:, qt, 0:130]),
                       reads=[b_ps[ob_]], writes=[b_acc[ab][qt]])
            else:
                j = jt[qt]
                P.emit("dve", lambda e, qt=qt, pso=pso, ab=ab, j=j, n=n, hb=hb: e.scalar_tensor_tensor(
                    out=acc[ab][:, qt, :], in0=pso[:, qt, 0:130], scalar=sel[hb][:, j, n:n + 1], in1=acc[ab][:, qt, :],
                    op0=ALU.mult, op1=ALU.add), reads=[b_ps[ob_], b_sel[hb], b_acc[ab][qt]], writes=[b_acc[ab][qt]])
        last = (idx + 1 == nitems) or items[idx + 1][3]
        if last:
            nq = len(jt)
            P.emit("dve", lambda e, ab=ab, nq=nq: e.reciprocal(out=rec[ab][:, 0:nq], in_=acc[ab][:, 0:nq, 128]),
                   reads=b_acc[ab][0:nq], writes=[b_rec[ab]])
            for qt in range(nq):
                P.emit("dve", lambda e, ab=ab, qt=qt: e.tensor_scalar(out=obf[ab][:, qt, :], in0=acc[ab][:, qt, 0:128],
                                                                      scalar1=rec[ab][:, qt:qt + 1], scalar2=None, op0=ALU.mult),
                       reads=[b_acc[ab][qt], b_rec[ab]], writes=[b_obf[ab]])
            for qt in range(nq):
                P.tr(pT[ab][:, qt, :], obf[ab][:, qt, :], ident[:], reads=[b_obf[ab], b_id], writes=[b_pT[ab]])
            c0 = jt[0] * 128
            P.emit("act", lambda e, ab=ab, nq=nq, c0=c0, hb=hb: e.activation(
                out=AOst[hb][:, c0:c0 + nq * 128], in_=pT[ab][:, 0:nq, :], func=AF.Copy),
                reads=[b_pT[ab]], writes=[b_AOst[hb]])
            if m == NO:
                P.dma(AOs[h], AOst[hb][:], reads=[b_AOst[hb]], writes=[b_AO[h]], skey=f"AOst{hb}", eng="pool")

    B_load(0)
    B_gate(0)
    LOOK = 2
    for idx in range(nitems + LOOK):
        if idx < nitems:
            h, m, n, own = items[idx]
            if own and m == NO and h + 1 < H:
                B_gate(h + 1)
            item_S(idx)
        if idx - LOOK >= 0:
            item_PV(idx - LOOK)
            h2, m2, n2, own2 = items[idx - LOOK]
            if own2 and m2 == 0 and h2 + 1 < H:
                B_load(h2 + 1)
        if idx % cj_every == cj_every - 1 and cj_next[0] < ncj:
            conv_jobs[cj_next[0]](["pool"])
            cj_next[0] += 1
    while cj_next[0] < ncj:
        conv_jobs[cj_next[0]](["pool", "dve", "act"])
        cj_next[0] += 1
    P.barrier()
    P.release(m_persist)
    if stop_after == "B":
        P.finalize()
        return nc

    NS = 5
    wslot = [P.sbuf(f"wslot{i}", [128, 4096], BF16) for i in range(NS)]
    b_wslot = [P.buf() for _ in range(NS)]
    xr = [P.sbuf(f"xr{i}", [128, 4, D], F32) for i in range(2)]
    b_xr = [P.buf() for _ in range(2)]
    AOt = [P.sbuf(f"AOt{i}", [128, H, 512], BF16) for i in range(2)]
    b_AOt = [P.buf() for _ in range(2)]
    xnTc = P.sbuf("xnTc", [128, 8, 512], BF16)
    b_xnTc = P.buf()
    hT = P.sbuf("hT", [128, NFC, 512], BF16)
    b_hT = [P.buf() for _ in range(NFC)]
    sgS = [P.sbuf(f"sgS{i}", [128, 512], F32) for i in range(2)]
    b_sgS = [P.buf() for _ in range(2)]
    cS = [P.sbuf(f"cS{i}", [128, 512], F32) for i in range(2)]
    b_cS = [P.buf() for _ in range(2)]
    uT = P.sbuf("uT", [128, 8, 516], F32)
    b_uT = [P.buf() for _ in range(8)]
    t1 = [P.sbuf(f"t1_{i}", [128, 512], F32) for i in range(2)]
    t2 = [P.sbuf(f"t2_{i}", [128, 512], F32) for i in range(2)]
    b_t1 = [P.buf() for _ in range(2)]
    b_t2 = [P.buf() for _ in range(2)]
    zT = P.sbuf("zT", [128, 8, 512], BF16)
    b_zT = [P.buf() for _ in range(8)]
    cwt = P.sbuf("cwt", [128, 8, 3], F32)
    b_cwt = P.buf()
    gfin = P.sbuf("gfin", [128, D], F32)
    b_gfin = P.buf()
    ot = [P.sbuf(f"ot{i}", [128, D], F32) for i in range(2)]
    b_ot = [P.buf() for _ in range(2)]
    NMS = 5 * 4 * (NO // 2 + 1) + 8
    msC = P.sbuf("msC", [128, NMS], F32)
    sdC = P.sbuf("sdC", [128, NMS], F32)
    rsC = P.sbuf("rsC", [128, NMS], F32)
    msc = [0]
    b_out = []
    for k in range(3):
        P.dma(cwt[:, :, k], cw[k].rearrange("(j p) -> p j", p=128), writes=[b_cwt], skey=f"cwt{k}", allow_slow_non_contiguous=True)
    P.dma(gfin[:], n_fin.partition_broadcast(128), writes=[b_gfin], skey="gfin")
    b_msall = P.buf()
    P.emit("pool", lambda e: e.memset(msC[:], 0.0), writes=[b_msall])

    groups = [("halo", 2 * NP - 1, 1, 0)] + [("own", 2 * NP + 4 * g, 4, 1 + 4 * g) for g in range(NO // 2)]
    seq = []
    for (kind, tt0, nt, j0) in groups:
        seq += U_WO + U_FIN[0] + U_FOUT[0] + U_CIN
        if kind == "own":
            seq += U_COUT + U_FIN[1] + U_FOUT[1]
    wst = {"issued": 0, "cur": 0}

    def w_ensure(upto):
        while wst["issued"] <= min(upto, len(seq) - 1):
            i = wst["issued"]
            P.dma(wslot[i % NS][:], WU[seq[i]], reads=[b_WU[seq[i]]], writes=[b_wslot[i % NS]], skey=f"ws{i % NS}")
            wst["issued"] += 1

    def w_get(expect):
        i = wst["cur"]
        assert seq[i] == expect, (i, seq[i], expect)
        w_ensure(i + NS - 1)
        wst["cur"] += 1
        return wslot[i % NS], b_wslot[i % NS]

    def C_load(gi):
        kind, tt0, nt, j0 = groups[gi]
        sl = gi % 2
        T = nt * 128
        P.dma(xr[sl][:, 0:nt, :], xl[tt0 * 128:tt0 * 128 + T, :].rearrange("(s p) d -> p s d", p=128), writes=[b_xr[sl]], skey=f"xr{sl}")
        P.dma(AOt[sl][:, :, 0:T], AOs.rearrange("h d t -> d h t")[:, :, j0 * 128:j0 * 128 + T], reads=b_AO, writes=[b_AOt[sl]],
              skey=f"AOt{sl}")

    def proj_tokmajor(lhs_fn, lhs_reads, nk, units, sl, nt):
        for half in range(2):
            wt, b_wt = w_get(units[half])
            for s in range(nt):
                bi = nbank()
                for k in range(nk):
                    P.mm(psb[bi][:, :], lhs_fn(k, s), wt[:, k * 512:(k + 1) * 512], k == 0, k == nk - 1,
                         reads=lhs_reads + [b_wt], writes=[b_ps[bi]])
                xv = xr[sl][:, s, half * 512:(half + 1) * 512]
                P.emit("dve", lambda e, xv=xv, bi=bi: e.tensor_tensor(out=xv, in0=psb[bi][:, :], in1=xv, op=ALU.add),
                       reads=[b_ps[bi], b_xr[sl]], writes=[b_xr[sl]])

    def do_norm(sl, nt):
        c0 = msc[0]
        msc[0] += nt
        b_ms = P.buf()
        b_ms.last_w = b_msall.last_w
        rms_stats(lambda s: xr[sl][:, s, :], nt, msC, sdC, rsC, c0, b_xr[sl], b_ms)
        return c0, b_ms

    def ffn(l, sl, nt):
        T = nt * 128
        c0, b_ms = do_norm(sl, nt)
        norm_T(lambda s: xr[sl][:, s, :], nt, rsC, c0, b_xr[sl], b_ms, xnTc, b_xnTc)
        for u in range(11):
            wt, b_wt = w_get(U_FIN[l][u])
            wv = wt[:, :].rearrange("p (g c k f) -> p g c k f", g=2, c=2, k=8)
            for c in range(2):
                fc = 2 * u + c
                bg = nbank()
                for kc in range(8):
                    P.mm(psb[bg][:, 0:T], wv[:, 0, c, kc, :], xnTc[:, kc, 0:T], kc == 0, kc == 7, reads=[b_wt, b_xnTc], writes=[b_ps[bg]])
                bu = nbank()
                for kc in range(8):
                    P.mm(psb[bu][:, 0:T], wv[:, 1, c, kc, :], xnTc[:, kc, 0:T], kc == 0, kc == 7, reads=[b_wt, b_xnTc], writes=[b_ps[bu]])
                si = fc % 2
                P.emit("act", lambda e, si=si, bg=bg: e.activation(out=sgS[si][:, 0:T], in_=psb[bg][:, 0:T], func=AF.Silu),
                       reads=[b_ps[bg]], writes=[b_sgS[si]])
                P.emit("dve", lambda e, si=si, bu=bu, fc=fc: e.tensor_tensor(out=hT[:, fc, 0:T], in0=sgS[si][:, 0:T], in1=psb[bu][:, 0:T], op=ALU.mult),
                       reads=[b_sgS[si], b_ps[bu]], writes=[b_hT[fc]])
        for half in range(2):
            banks = [nbank() for _ in range(nt)]
            for k in range(3):
                wt, b_wt = w_get(U_FOUT[l][half * 3 + k])
                f0 = 8 * k
                nfc = min(8, NFC - f0)
                for fi in range(nfc):
                    fc = f0 + fi
                    for s in range(nt):
                        P.mm(psb[banks[s]][:, :], hT[:, fc, s * 128:(s + 1) * 128], wt[:, fi * 512:(fi + 1) * 512], fc == 0, fc == NFC - 1,
                             reads=[b_hT[fc], b_wt], writes=[b_ps[banks[s]]])
            for s in range(nt):
                xv = xr[sl][:, s, half * 512:(half + 1) * 512]
                bi = banks[s]
                P.emit("dve", lambda e, xv=xv, bi=bi: e.tensor_tensor(out=xv, in0=psb[bi][:, :], in1=xv, op=ALU.add),
                       reads=[b_ps[bi], b_xr[sl]], writes=[b_xr[sl]])

    def conv_mixer(sl, nt, halo_only):
        T = nt * 128
        c0, b_ms = do_norm(sl, nt)
        norm_T(lambda s: xr[sl][:, s, :], nt, rsC, c0, b_xr[sl], b_ms, xnTc, b_xnTc)
        for j in range(8):
            wt, b_wt = w_get(U_CIN[j])
            wv = wt[:, 0:3072].rearrange("p (g k f) -> p g k f", g=3, k=8)
            pb = {}
            for t3 in ([1, 2] if halo_only else [0, 1, 2]):
                bi = nbank()
                pb[t3] = bi
                for kc in range(8):
                    P.mm(psb[bi][:, 0:T], wv[:, t3, kc, :], xnTc[:, kc, 0:T], kc == 0, kc == 7, reads=[b_wt, b_xnTc], writes=[b_ps[bi]])
            ci = j % 2
            P.emit("act", lambda e, ci=ci, bi=pb[1]: e.activation(out=cS[ci][:, 0:T], in_=psb[bi][:, 0:T], func=AF.Copy),
                   reads=[b_ps[pb[1]]], writes=[b_cS[ci]])
            P.emit("dve", lambda e, ci=ci, bi=pb[2], j=j: e.tensor_tensor(out=uT[:, j, 2:2 + T], in0=cS[ci][:, 0:T], in1=psb[bi][:, 0:T], op=ALU.mult),
                   reads=[b_cS[ci], b_ps[pb[2]]], writes=[b_uT[j]])
            if halo_only:
                P.emit("dve", lambda e, j=j: e.tensor_scalar(out=uT[:, j, 0:2], in0=uT[:, j, T:T + 2], scalar1=flagt[:, 0:1], scalar2=None, op0=ALU.mult),
                       reads=[b_uT[j], b_flag], writes=[b_uT[j]])
                continue
            P.emit("act", lambda e, j=j, ci=ci: e.activation(out=t1[ci][:, 0:T], in_=uT[:, j, 0:T], func=AF.Copy, scale=cwt[:, j, 0:1]),
                   reads=[b_uT[j], b_cwt], writes=[b_t1[ci]])
            P.emit("dve", lambda e, j=j, ci=ci: e.scalar_tensor_tensor(out=t2[ci][:, 0:T], in0=uT[:, j, 1:1 + T], scalar=cwt[:, j, 1:2], in1=t1[ci][:, 0:T],
                                                                       op0=ALU.mult, op1=ALU.add),
                   reads=[b_uT[j], b_cwt, b_t1[ci]], writes=[b_t2[ci]])
            P.emit("dve", lambda e, j=j, ci=ci: e.scalar_tensor_tensor(out=t1[ci][:, 0:T], in0=uT[:, j, 2:2 + T], scalar=cwt[:, j, 2:3], in1=t2[ci][:, 0:T],
                                                                       op0=ALU.mult, op1=ALU.add),
                   reads=[b_uT[j], b_cwt, b_t2[ci]], writes=[b_t1[ci]])
            P.emit("dve", lambda e, j=j, ci=ci, bi=pb[0]: e.tensor_tensor(out=zT[:, j, 0:T], in0=t1[ci][:, 0:T], in1=psb[bi][:, 0:T], op=ALU.mult),
                   reads=[b_t1[ci], b_ps[pb[0]]], writes=[b_zT[j]])
            P.emit("dve", lambda e, j=j: e.tensor_copy(out=uT[:, j, 0:2], in_=uT[:, j, T:T + 2]), reads=[b_uT[j]], writes=[b_uT[j]])
        if halo_only:
            return
        proj_tokmajor(lambda k, s: zT[:, k, s * 128:(s + 1) * 128], b_zT, 8, U_COUT, sl, nt)

    def final_out(gi, sl, nt):
        c0, b_ms = do_norm(sl, nt)
        g = gi - 1
        for s in range(nt):
            oi = s % 2
            P.emit("dve", lambda e, s=s, oi=oi: e.scalar_tensor_tensor(out=ot[oi][:], in0=xr[sl][:, s, :], scalar=rsC[:, c0 + s:c0 + s + 1], in1=gfin[:],
                                                                       op0=ALU.mult, op1=ALU.mult),
                   reads=[b_xr[sl], b_ms, b_gfin], writes=[b_ot[oi]])
            bo = P.buf()
            r0 = g * 512 + s * 128
            P.dma(out[r0:r0 + 128, :], ot[oi][:], reads=[b_ot[oi]], writes=[bo], skey=f"ot{oi}", eng="pool")
            b_out.append(bo)

    C_load(0)
    for gi, (kind, tt0, nt, j0) in enumerate(groups):
        sl = gi % 2
        if gi + 1 < len(groups):
            C_load(gi + 1)
        proj_tokmajor(lambda k, s: AOt[sl][:, k, s * 128:(s + 1) * 128], [b_AOt[sl]], H, U_WO, sl, nt)
        ffn(0, sl, nt)
        conv_mixer(sl, nt, kind == "halo")
        if kind == "own":
            ffn(1, sl, nt)
            final_out(gi, sl, nt)
    P.wait_only("sp", b_out)
    P.wait_only("pool", b_out)
    P.finalize()
    return nc


def make_masks(NP, NO, half):
    NB = NP + NO
    NQT = 2 * NO + 1
    ind = np.zeros((NQT, NB), np.float32)
    for j in range(NQT):
        ob = NP - 1 + (j + 1) // 2
        lo = 0 if half == 1 else NP
        for n in range(NB):
            if lo <= n < ob:
                ind[j, n] = 1.0
    neg = np.where(ind > 0, 0.0, NEGBIG).astype(np.float32)
    ind_b = np.ascontiguousarray(np.broadcast_to(ind.reshape(1, -1), (128, NQT * NB)))
    neg_b = np.ascontiguousarray(np.broadcast_to(neg.reshape(1, -1), (128, NQT * NB)))
    return neg_b, ind_b


_PROG_CACHE = {}


def run_module(inputs, debug=False, stop_after=None):
    x = np.asarray(inputs["x"], np.float32)
    B, S, _ = x.shape
    half_t = S // 2
    NP = NO = half_t // 256
    ncores = 2 * B
    key = (NP, NO, debug, stop_after)
    if key not in _PROG_CACHE:
        _PROG_CACHE[key] = build_program(NP, NO, debug=debug, stop_after=stop_after)
    nc = _PROG_CACHE[key]
    f = lambda k: np.ascontiguousarray(np.asarray(inputs[k], np.float32))
    shared = {
        "wqkv": f("attn_w_qkv")[0], "wo": f("attn_w_o")[0], "cwin": f("conv_w_in")[0], "cw": f("conv_w")[0],
        "cwout": f("conv_w_out")[0], "fwin": f("ffn_w_in"), "fwout": f("ffn_w_out"),
        "n_attn": f("attn_norm")[0], "n_conv": f("conv_norm")[0], "n_ffn": f("ffn_norm"), "n_fin": f("final_norm"),
    }
    in_maps = []
    for c in range(ncores):
        b, half = c // 2, c % 2
        xl = np.zeros((2 * half_t, D), np.float32)
        if half == 1:
            xl[:half_t] = x[b, :half_t]
        xl[half_t:] = x[b, half * half_t:(half + 1) * half_t]
        neg, ind = make_masks(NP, NO, half)
        m = dict(shared)
        m.update({"xl": xl, "pastneg": neg, "pastind": ind, "flag": np.full((128, 1), float(half), np.float32)})
        in_maps.append(m)
    res = run_bass_kernel_spmd(nc, in_maps, core_ids=list(range(ncores)))
    out = np.zeros((B, S, D), np.float32)
    for c in range(ncores):
        b, half = c // 2, c % 2
        out[b, half * half_t:(half + 1) * half_t] = res.results[c]["out"]
    return out, res


def kernel(**inputs):
    out, _ = run_module(inputs)
    return out
```

```python
import contextlib
import numpy as np
import ml_dtypes
import concourse.bass as bass
import concourse.mybir as mybir
from concourse.bass_utils import run_bass_kernel_spmd

F32 = mybir.dt.float32
BF16 = mybir.dt.bfloat16
AF = mybir.ActivationFunctionType
ALU = mybir.AluOpType
AX = mybir.AxisListType

SEM_LIMIT = 30000


class Buf:
    __slots__ = ("name", "last_w", "readers", "dma_readers")

    def __init__(self, name):
        self.name = name
        self.last_w = None
        self.readers = {}
        self.dma_readers = []


class Op:
    __slots__ = ("eng", "fn", "waits", "signal", "sem", "val", "is_dma", "skey", "idx")

    def __init__(self, eng, fn, is_dma, skey):
        self.eng = eng
        self.fn = fn
        self.waits = []
        self.signal = False
        self.sem = None
        self.val = 0
        self.is_dma = is_dma
        self.skey = skey


class Prog:
    ENGS = ("pe", "act", "dve", "pool", "sp")

    def __init__(self, nc):
        self.nc = nc
        self.ops = {e: [] for e in self.ENGS}
        self.stack = contextlib.ExitStack()
        self.nbuf = 0
        self.sb_base = (nc.sbuf_base + 63) // 64 * 64
        self.sb_ptr = self.sb_base
        self.sb_top = nc.sbuf_top
        self.sb_n = 0
        self.sb_peak = 0
        self.last_comp = {}
        self.dma_since = {}

    def sbuf(self, name, shape, dtype):
        esz = 4 if dtype == F32 else 2
        size = esz
        for d in shape[1:]:
            size *= d
        size = (size + 63) // 64 * 64
        off = self.sb_ptr
        self.sb_ptr += size
        assert self.sb_ptr <= self.sb_top, (name, self.sb_ptr, self.sb_top)
        self.sb_peak = max(self.sb_peak, self.sb_ptr)
        self.sb_n += 1
        return self.nc.alloc_sbuf_tensor_at(f"{name}{self.sb_n}", list(shape), dtype, offset=off)

    def mark(self):
        return self.sb_ptr

    def release(self, mark):
        self.sb_ptr = mark

    def barrier(self):
        deps = list(self.last_comp.values()) + list(self.dma_since.values())
        for e in self.ENGS:
            op = Op(e, lambda eng: None, False, None)
            for w in deps:
                if w.eng == e and not w.is_dma:
                    continue
                w.signal = True
                op.waits.append(w)
            self.ops[e].append(op)
        self.dma_since = {}

    def psum(self, name, shape, dtype):
        return self.stack.enter_context(self.nc.psum_tensor(name, list(shape), dtype))

    def buf(self, name=None):
        self.nbuf += 1
        return Buf(name or f"b{self.nbuf}")

    def wait_only(self, eng, reads):
        op = Op(eng, lambda e: None, False, None)
        for b in reads:
            w = b.last_w
            if w is not None and w not in op.waits:
                w.signal = True
                op.waits.append(w)
        self.ops[eng].append(op)
        return op

    def emit(self, eng, fn, reads=(), writes=(), dma=False, skey=None):
        op = Op(eng, fn, dma, skey)
        waits = []
        for b in reads:
            if b.last_w is not None:
                waits.append(b.last_w)
        for b in writes:
            w = b.last_w
            if w is not None and (w.eng != eng or w.is_dma or dma):
                waits.append(w)
            for e, r in b.readers.items():
                if e != eng or dma:
                    waits.append(r)
            for r in b.dma_readers:
                waits.append(r)
        seen = set()
        for w in waits:
            if id(w) in seen or w is op:
                continue
            seen.add(id(w))
            w.signal = True
            op.waits.append(w)
        for b in reads:
            if dma:
                b.dma_readers.append(op)
            else:
                b.readers[eng] = op
        for b in writes:
            b.last_w = op
            b.readers = {}
            b.dma_readers = []
        if dma:
            op.signal = True
            self.dma_since[skey] = op
        else:
            self.last_comp[eng] = op
        self.ops[eng].append(op)
        return op

    def dma(self, out, in_, reads=(), writes=(), skey=None, eng="sp", **kw):
        assert skey is not None
        return self.emit(eng, lambda e: e.dma_start(out=out, in_=in_, **kw),
                         reads=reads, writes=writes, dma=True, skey=skey)

    def mm(self, out, lhsT, rhs, start, stop, reads=(), writes=()):
        return self.emit("pe", lambda e: e.matmul(out, lhsT, rhs, start=start, stop=stop),
                         reads=reads, writes=writes)

    def tr(self, out, in_, ident, reads=(), writes=()):
        return self.emit("pe", lambda e: e.transpose(out, in_, ident), reads=reads, writes=writes)

    def finalize(self):
        nc = self.nc
        sem_stack = self.stack
        nsem = [0]

        def new_sem(tag):
            nsem[0] += 1
            return sem_stack.enter_context(nc.semaphore(f"s_{tag}_{nsem[0]}"))

        for e in self.ENGS:
            cur = None
            cnt = 0
            for op in self.ops[e]:
                if op.is_dma or not op.signal:
                    continue
                if cur is None or cnt >= SEM_LIMIT:
                    cur = new_sem(e)
                    cnt = 0
                cnt += 1
                op.sem, op.val = cur, cnt
        keysem = {}
        for e in self.ENGS:
            for op in self.ops[e]:
                if not op.is_dma:
                    continue
                if op.skey not in keysem or keysem[op.skey][1] >= SEM_LIMIT:
                    keysem[op.skey] = [new_sem("d"), 0]
                ks = keysem[op.skey]
                ks[1] += 16
                op.sem, op.val = ks[0], ks[1]
        self.n_sems = nsem[0]

        with nc.Block() as block:
            def runner(ename):
                def run(eng):
                    seen = {}
                    for op in self.ops[ename]:
                        need = {}
                        for w in op.waits:
                            k = id(w.sem)
                            if seen.get(k, 0) >= w.val:
                                continue
                            if k not in need or need[k][1] < w.val:
                                need[k] = (w.sem, w.val)
                        for k, (s, v) in need.items():
                            eng.wait_ge(s, v)
                            seen[k] = v
                        ins = op.fn(eng)
                        if ins is None:
                            assert not op.signal
                            continue
                        if op.signal and op.sem is not None:
                            ins.then_inc(op.sem, 16 if op.is_dma else 1)
                return run

            block.tensor(runner("pe"))
            block.scalar(runner("act"))
            block.vector(runner("dve"))
            block.gpsimd(runner("pool"))
            block.sync(runner("sp"))
        self.stack.close()


D = 1024
H = 8
F = 2816
NFC = 22
EPS = 1e-6
QSCALE = 128.0 ** -0.5
NEGBIG = -30000.0


def build_program(NP, NO, debug=False, stop_after=None, conv_in_b=True):
    nc = bass.Bass("TRN2", target_bir_lowering=False)
    NB = NP + NO
    LT = NB * 256
    NT = LT // 128
    NG = LT // 512
    NQT = 2 * NO + 1
    GQ0 = NP // 2 - 1
    NQG = NG - GQ0
    QW = NQG * 512
    NAO = NQT * 128
    OUT_T = NO * 256

    def din(name, shape):
        return nc.dram_tensor(name, list(shape), F32, kind="ExternalInput").ap()

    xl = din("xl", [LT, D])
    wqkv = din("wqkv", [D, 3 * D])
    wo = din("wo", [D, D])
    cwin = din("cwin", [D, 3 * D])
    cw = din("cw", [3, D])
    cwout = din("cwout", [D, D])
    fwin = din("fwin", [2, D, 2 * F])
    fwout = din("fwout", [2, F, D])
    n_attn = din("n_attn", [D])
    n_conv = din("n_conv", [D])
    n_ffn = din("n_ffn", [2, D])
    n_fin = din("n_fin", [D])
    pastneg = din("pastneg", [128, NQT * NB])
    pastind = din("pastind", [128, NQT * NB])
    flag = din("flag", [128, 1])
    out = nc.dram_tensor("out", [OUT_T, D], F32, kind="ExternalOutput").ap()
    sk = "ExternalOutput" if debug else "Internal"
    KTs = nc.dram_tensor("KTs", [H, 128, LT], BF16, kind=sk).ap()
    Vs = nc.dram_tensor("Vs", [H, 128, NT, 130], BF16, kind=sk).ap()
    QTs = nc.dram_tensor("QTs", [H, 128, QW], BF16, kind=sk).ap()
    AOs = nc.dram_tensor("AOs", [H, 128, NAO], BF16, kind=sk).ap()
    NU = 46
    WU = nc.dram_tensor("WU", [NU, 128, 4096], BF16, kind="Internal").ap()
    U_WO = [0, 1]
    U_FIN = [[2 + l * 17 + u for u in range(11)] for l in range(2)]
    U_FOUT = [[2 + l * 17 + 11 + k for k in range(6)] for l in range(2)]
    U_CIN = [36 + j for j in range(8)]
    U_COUT = [44, 45]

    P = Prog(nc)
    psb = [P.psum(f"psb{i}", [128, 512], F32) for i in range(8)]
    b_ps = [P.buf(f"ps{i}") for i in range(8)]
    pT = [psb[6 + i][:, :].bitcast(BF16).rearrange("p (a b) -> p a b", a=8) for i in range(2)]
    b_pT = [b_ps[6], b_ps[7]]
    psrr = [0]

    def nbank():
        i = psrr[0] % 6
        psrr[0] += 1
        return i

    identf = P.sbuf("identf", [128, 128], F32)
    ident = P.sbuf("ident", [128, 128], BF16)
    tri = P.sbuf("tri", [128, 128], BF16)
    gcols = P.sbuf("gcols", [128, 4, 8], F32)
    kms = P.sbuf("kms", [128, H, NB], F32)
    kmb = P.sbuf("kmb", [128, H, NB], BF16)
    flagt = P.sbuf("flagt", [128, 1], F32)
    b_idf, b_id, b_tri, b_g, b_kms, b_kmb, b_flag = [P.buf() for _ in range(7)]
    P.emit("pool", lambda e: e.memset(identf[:], 0.0), writes=[b_idf])
    P.emit("pool", lambda e: e.affine_select(out=identf[:], in_=identf[:], pattern=[[-1, 128]],
                                             compare_op=ALU.not_equal, fill=1.0, base=0, channel_multiplier=1),
           reads=[b_idf], writes=[b_idf])
    P.emit("dve", lambda e: e.tensor_copy(out=ident[:], in_=identf[:]), reads=[b_idf], writes=[b_id])
    identk = P.sbuf("identk", [128, 128], F32)
    b_idk = P.buf()
    P.emit("dve", lambda e: e.tensor_copy(out=identk[:], in_=identf[:]), reads=[b_idf], writes=[b_idk])
    P.emit("pool", lambda e: e.memset(identf[:], 1.0), reads=[b_idf], writes=[b_idf])
    P.emit("pool", lambda e: e.affine_select(out=identf[:], in_=identf[:], pattern=[[1, 128]],
                                             compare_op=ALU.is_ge, fill=0.0, base=0, channel_multiplier=-1),
           reads=[b_idf], writes=[b_idf])
    P.emit("dve", lambda e: e.tensor_copy(out=tri[:], in_=identf[:]), reads=[b_idf], writes=[b_tri])
    for i, src in enumerate([n_attn, n_conv, n_ffn[0], n_ffn[1]]):
        P.dma(gcols[:, i, :], src.rearrange("(kc p) -> p kc", p=128), writes=[b_g], skey=f"g{i}",
              allow_slow_non_contiguous=True)
    P.dma(flagt[:], flag, writes=[b_flag], skey="flag")
    gimg = P.sbuf("gimg", [128, 3, 8, 128], F32)
    b_gimg = P.buf()
    P.emit("pool", lambda e: e.memset(gimg[:], 1.0), writes=[b_gimg])
    for i in range(3):
        for kc in range(8):
            P.emit("dve", lambda e, i=i, kc=kc: e.tensor_scalar(out=gimg[:, i, kc, :], in0=gimg[:, i, kc, :], scalar1=gcols[:, i + 1, kc:kc + 1],
                                                                scalar2=None, op0=ALU.mult), reads=[b_gimg, b_g], writes=[b_gimg])
    P.emit("pool", lambda e: e.memset(kms[:], 0.0), writes=[b_kms])

    b_WU = [P.buf(f"WU{u}") for u in range(NU)]
    rr = [0]

    def make_conv_jobs(stg, b_stg, img, b_img):
        jobs = []
        cnt = [0]

        def conv_ops(slot, views, gidx, eng_list):
            for (iv, sv, G) in views:
                eng = eng_list[rr[0] % len(eng_list)]
                rr[0] += 1
                rd = [b for b in b_stg[slot]]
                if G is None:
                    if eng == "act":
                        P.emit("act", lambda e, iv=iv, sv=sv: e.activation(out=iv, in_=sv, func=AF.Copy),
                               reads=rd, writes=[b_img[slot]])
                    else:
                        P.emit(eng, lambda e, iv=iv, sv=sv: e.tensor_copy(out=iv, in_=sv), reads=rd, writes=[b_img[slot]])
                else:
                    if eng == "act":
                        eng = "pool"
                    gv = gimg[:, gidx - 1, :, :].unsqueeze(1).broadcast_to([128, G, 8, 128])
                    P.emit(eng, lambda e, iv=iv, sv=sv, gv=gv: e.tensor_tensor(out=iv, in0=sv, in1=gv, op=ALU.mult),
                           reads=rd + [b_gimg], writes=[b_img[slot]])

        def job(uidx, loads, views, gidx):
            def run(eng_list):
                slot = cnt[0] % 2
                cnt[0] += 1
                for i, (sv, src) in enumerate(loads):
                    P.dma(sv(stg[slot]), src, writes=[b_stg[slot][i]], skey=f"stg{slot}_{i}")
                conv_ops(slot, [(iv(img[slot]), sv(stg[slot]), kc) for (iv, sv, kc) in views], gidx, eng_list)
                P.dma(WU[uidx], img[slot][:], reads=[b_img[slot]], writes=[b_WU[uidx]], skey=f"img{slot}", eng="pool")
            return run

        def v3(a, b):
            return lambda t: t[:, 0:a * b].rearrange("p (a b) -> p a b", a=a)

        for srcw, units in ((wo, U_WO), (cwout, U_COUT)):
            for n in range(2):
                src = srcw.rearrange("(h p) c -> p h c", p=128)[:, :, n * 512:(n + 1) * 512]
                jobs.append(job(units[n], [(v3(8, 512), src)],
                                [(lambda t: t[:, :], lambda t: t[:, :], None)], None))
        for l in range(2):
            for u in range(11):
                loads = []
                for gu in range(2):
                    for c in range(2):
                        col0 = gu * F + (2 * u + c) * 128
                        src = fwin[l].rearrange("(kc p) n -> p kc n", p=128)[:, :, col0:col0 + 128]
                        o = (gu * 2 + c) * 1024
                        loads.append((lambda t, o=o: t[:, o:o + 1024].rearrange("p (a b) -> p a b", a=8), src))
                vw = lambda t: t[:, :].rearrange("p (g k f) -> p g k f", g=4, k=8)
                jobs.append(job(U_FIN[l][u], loads, [(vw, vw, 4)], 2 + l))
        for l in range(2):
            for half in range(2):
                for k in range(3):
                    f0 = 8 * k
                    nfc = min(8, NFC - f0)
                    src = fwout[l].rearrange("(fc p) c -> p fc c", p=128)[:, f0:f0 + nfc, half * 512:(half + 1) * 512]
                    jobs.append(job(U_FOUT[l][half * 3 + k], [(v3(nfc, 512), src)],
                                    [(lambda t, n=nfc: t[:, 0:n * 512], lambda t, n=nfc: t[:, 0:n * 512], None)], None))
        for j in range(8):
            loads = []
            for t3 in range(3):
                col0 = t3 * D + j * 128
                src = cwin.rearrange("(kc p) n -> p kc n", p=128)[:, :, col0:col0 + 128]
                o = t3 * 1024
                loads.append((lambda t, o=o: t[:, o:o + 1024].rearrange("p (a b) -> p a b", a=8), src))
            vw = lambda t: t[:, 0:3072].rearrange("p (g k f) -> p g k f", g=3, k=8)
            jobs.append(job(U_CIN[j], loads, [(vw, vw, 3)], 1))
        return jobs

    m_persist = P.mark()

    Wsb = P.sbuf("Wsb", [128, 8, 3 * D], BF16)
    b_Wp = [[P.buf(f"W{part}_{kc}") for kc in range(8)] for part in range(3)]
    stgA = [P.sbuf(f"stgA{i}", [128, D], F32) for i in range(4)]
    b_stgA = [P.buf() for _ in range(4)]
    wi = 0
    for part in (1, 2, 0):
        for kc in range(8):
            sl = wi % 4
            P.dma(stgA[sl][:], wqkv[kc * 128:(kc + 1) * 128, part * D:(part + 1) * D], writes=[b_stgA[sl]], skey=f"stgA{sl}")
            ov = Wsb[:, kc, part * D:(part + 1) * D]
            iv = stgA[sl][:]
            gs = gcols[:, 0, kc:kc + 1]
            if wi % 2 == 0:
                P.emit("act", lambda e, ov=ov, iv=iv, gs=gs: e.activation(out=ov, in_=iv, func=AF.Copy, scale=gs),
                       reads=[b_stgA[sl], b_g], writes=[b_Wp[part][kc]])
            else:
                P.emit("dve", lambda e, ov=ov, iv=iv, gs=gs: e.tensor_scalar(out=ov, in0=iv, scalar1=gs, scalar2=None, op0=ALU.mult),
                       reads=[b_stgA[sl], b_g], writes=[b_Wp[part][kc]])
            wi += 1

    xs = [P.sbuf(f"xs{i}", [128, 4, D], F32) for i in range(2)]
    b_xs = [P.buf() for _ in range(2)]
    junk = P.sbuf("junk", [128, D], BF16)
    b_junk = P.buf()
    msA = P.sbuf("msA", [128, NT], F32)
    sdA = P.sbuf("sdA", [128, NT], F32)
    rsA = P.sbuf("rsA", [128, NT], F32)
    b_msA = [P.buf() for _ in range(NG)]
    xn = [P.sbuf(f"xn{i}", [128, D], BF16) for i in range(2)]
    b_xn = [P.buf() for _ in range(2)]
    xnT = [P.sbuf(f"xnT{i}", [128, 8, 512], BF16) for i in range(2)]
    b_xnT = [P.buf() for _ in range(2)]
    Kst = [P.sbuf(f"Kst{i}", [128, H, 512], BF16) for i in range(2)]
    Qst = [P.sbuf(f"Qst{i}", [128, H, 512], BF16) for i in range(2)]
    Vst = [P.sbuf(f"Vst{i}", [128, H, 4, 130], BF16) for i in range(2)]
    b_Kst = [P.buf() for _ in range(2)]
    b_Qst = [P.buf() for _ in range(2)]
    b_Vst = [P.buf() for _ in range(2)]
    b_KT = [P.buf() for _ in range(NG)]
    b_V = [P.buf() for _ in range(NG)]
    b_Q = [P.buf() for _ in range(NG)]
    P.emit("pool", lambda e: e.memset(msA[:], 0.0), writes=b_msA)
    for i in range(2):
        P.emit("pool", lambda e, i=i: e.memset(Vst[i][:, :, :, 128:129], 1.0), writes=[b_Vst[i]])
        P.emit("pool", lambda e, i=i: e.memset(Vst[i][:, :, :, 129:130], 0.0), writes=[b_Vst[i]])

    def rms_stats(xtile_fn, nt, ms, sd, rs, c0, b_x, b_ms):
        for s in range(nt):
            P.emit("act", lambda e, s=s: e.activation(out=junk[:], in_=xtile_fn(s), func=AF.Square, scale=1.0 / 32,
                                                      accum_out=ms[:, c0 + s:c0 + s + 1]),
                   reads=[b_x], writes=[b_junk, b_ms])
        P.emit("dve", lambda e: e.tensor_scalar(out=sd[:, c0:c0 + nt], in0=ms[:, c0:c0 + nt], scalar1=EPS, scalar2=None, op0=ALU.add),
               reads=[b_ms], writes=[b_ms])
        P.emit("act", lambda e: e.activation(out=sd[:, c0:c0 + nt], in_=sd[:, c0:c0 + nt], func=AF.Sqrt),
               reads=[b_ms], writes=[b_ms])
        P.emit("dve", lambda e: e.reciprocal(out=rs[:, c0:c0 + nt], in_=sd[:, c0:c0 + nt]), reads=[b_ms], writes=[b_ms])

    def norm_T(xtile_fn, nt, rs, c0, b_x, b_ms, xnT_t, b_xnT_t):
        for s in range(nt):
            i = s % 2
            P.emit("act", lambda e, s=s, i=i: e.activation(out=xn[i][:], in_=xtile_fn(s), func=AF.Copy,
                                                           scale=rs[:, c0 + s:c0 + s + 1]),
                   reads=[b_x, b_ms], writes=[b_xn[i]])
            for kc in range(8):
                P.tr(pT[i][:, kc, :], xn[i][:, kc * 128:(kc + 1) * 128], ident[:], reads=[b_xn[i], b_id], writes=[b_pT[i]])
            P.emit("dve", lambda e, s=s, i=i: e.tensor_copy(out=xnT_t[:, :, s * 128:(s + 1) * 128], in_=pT[i]),
                   reads=[b_pT[i]], writes=[b_xnT_t])

    def A_front(g):
        sl = g % 2
        P.dma(xs[sl][:], xl[g * 512:(g + 1) * 512, :].rearrange("(s p) d -> p s d", p=128), writes=[b_xs[sl]], skey=f"xs{sl}")
        rms_stats(lambda s: xs[sl][:, s, :], 4, msA, sdA, rsA, g * 4, b_xs[sl], b_msA[g])
        norm_T(lambda s: xs[sl][:, s, :], 4, rsA, g * 4, b_xs[sl], b_msA[g], xnT[sl], b_xnT[sl])

    evr = [0]

    def A_back(g):
        sl = g % 2
        xt_ = xnT[sl]
        for h in range(H):
            bi = nbank()
            for kc in range(8):
                P.mm(psb[bi][:, :], Wsb[:, kc, D + h * 128:D + (h + 1) * 128], xt_[:, kc, :], kc == 0, kc == 7,
                     reads=[b_Wp[1][kc], b_xnT[sl]], writes=[b_ps[bi]])
            for blk in range(2):
                P.emit("act", lambda e, h=h, bi=bi, blk=blk: e.activation(
                    out=Kst[sl][:, h, blk * 256:(blk + 1) * 256], in_=psb[bi][:, blk * 256:(blk + 1) * 256], func=AF.Copy,
                    accum_out=kms[:, h, 2 * g + blk:2 * g + blk + 1]),
                    reads=[b_ps[bi]], writes=[b_Kst[sl], b_kms])
        P.dma(KTs.rearrange("h d t -> d h t")[:, :, g * 512:(g + 1) * 512], Kst[sl][:], reads=[b_Kst[sl]], writes=[b_KT[g]],
              skey=f"Kst{sl}", eng="pool")
        if g >= GQ0:
            for h in range(H):
                bi = nbank()
                for kc in range(8):
                    P.mm(psb[bi][:, :], Wsb[:, kc, h * 128:(h + 1) * 128], xt_[:, kc, :], kc == 0, kc == 7,
                         reads=[b_Wp[0][kc], b_xnT[sl]], writes=[b_ps[bi]])
                P.emit("dve", lambda e, h=h, bi=bi: e.tensor_scalar(out=Qst[sl][:, h, :], in0=psb[bi][:, :], scalar1=QSCALE,
                                                                   scalar2=None, op0=ALU.mult),
                       reads=[b_ps[bi]], writes=[b_Qst[sl]])
            gq = g - GQ0
            P.dma(QTs.rearrange("h d t -> d h t")[:, :, gq * 512:(gq + 1) * 512], Qst[sl][:], reads=[b_Qst[sl]], writes=[b_Q[g]],
                  skey=f"Qst{sl}", eng="pool")
        for s in range(4):
            for half in range(2):
                bi = nbank()
                for kc in range(8):
                    P.mm(psb[bi][:, :], xt_[:, kc, s * 128:(s + 1) * 128], Wsb[:, kc, 2 * D + half * 512:2 * D + (half + 1) * 512],
                         kc == 0, kc == 7, reads=[b_Wp[2][kc], b_xnT[sl]], writes=[b_ps[bi]])
                ov = Vst[sl][:, half * 4:(half + 1) * 4, s, 0:128]
                iv = psb[bi][:, :].rearrange("p (h d) -> p h d", h=4)
                evr[0] += 1
                if evr[0] % 2 == 0:
                    P.emit("act", lambda e, ov=ov, iv=iv: e.activation(out=ov, in_=iv, func=AF.Copy),
                           reads=[b_ps[bi]], writes=[b_Vst[sl]])
                else:
                    P.emit("dve", lambda e, ov=ov, iv=iv: e.tensor_copy(out=ov, in_=iv), reads=[b_ps[bi]], writes=[b_Vst[sl]])
        P.dma(Vs.rearrange("h p t c -> p h t c")[:, :, g * 4:(g + 1) * 4, :], Vst[sl][:], reads=[b_Vst[sl]], writes=[b_V[g]],
              skey=f"Vst{sl}", eng="pool")

    A_front(0)
    for g in range(NG):
        if g + 1 < NG:
            A_front(g + 1)
        A_back(g)
    P.emit("dve", lambda e: e.tensor_copy(out=kmb[:], in_=kms[:]), reads=[b_kms], writes=[b_kmb])
    P.barrier()
    P.release(m_persist)
    if stop_after == "A":
        P.finalize()
        return nc

    KT = [P.sbuf(f"KT{i}", [128, LT], BF16) for i in range(2)]
    Vb = [P.sbuf(f"Vb{i}", [128, NT, 130], BF16) for i in range(2)]
    QT = [P.sbuf(f"QT{i}", [128, QW], BF16) for i in range(2)]
    AOst1 = P.sbuf("AOst", [128, NAO], BF16)
    AOst = [AOst1, AOst1]
    b_KTb = [P.buf() for _ in range(2)]
    b_Vb = [P.buf() for _ in range(2)]
    b_QTb = [P.buf() for _ in range(2)]
    b_AOst1 = P.buf()
    b_AOst = [b_AOst1, b_AOst1]
    b_AO = [P.buf() for _ in range(H)]
    pneg = P.sbuf("pneg", [128, NQT * NB], F32)
    b_pm = P.buf()
    P.dma(pneg[:], pastneg, writes=[b_pm], skey="pneg")
    onehot = P.sbuf("onehot", [128, NB, 128], BF16)
    b_oh = P.buf()
    P.emit("pool", lambda e: e.memset(onehot[0:NB, :, :], 1.0), writes=[b_oh])
    for n in range(NB):
        P.emit("dve", lambda e, n=n: e.tensor_scalar(out=onehot[0:NB, n, :], in0=onehot[0:NB, n, :], scalar1=identk[0:NB, n:n + 1],
                                                     scalar2=None, op0=ALU.mult), reads=[b_oh, b_idk], writes=[b_oh])
    selTneg = P.sbuf("selTneg", [128, NQT * 128], BF16)
    b_selT = P.buf()
    selb16 = P.sbuf("selb16", [128, NQT, NB], BF16)
    b_selb = P.buf()
    thr = P.sbuf("thr", [128, NQT], F32)
    b_thr = P.buf()
    gm = P.sbuf("gm", [128, NQT, NB], F32)
    top8 = P.sbuf("top8", [128, NQT, 8], F32)
    sel = [P.sbuf(f"sel{i}", [128, NQT, NB], F32) for i in range(2)]
    b_gm = P.buf()
    b_top8 = P.buf()
    b_sel = [P.buf() for _ in range(2)]
    PT = [P.sbuf(f"PT{i}", [128, 512], BF16) for i in range(3)]
    b_PT = [P.buf() for _ in range(3)]
    acc = [P.sbuf(f"acc{i}", [128, 2, 130], F32) for i in range(2)]
    b_acc = [[P.buf() for _ in range(2)] for _ in range(2)]
    rec = [P.sbuf(f"rec{i}", [128, 2], F32) for i in range(2)]
    obf = [P.sbuf(f"obf{i}", [128, 2, 128], BF16) for i in range(2)]
    b_obf = [P.buf() for _ in range(2)]
    b_rec = [P.buf() for _ in range(2)]
    stgB = [P.sbuf(f"stgB{i}", [128, 4096], F32) for i in range(2)]
    imgB = [P.sbuf(f"imgB{i}", [128, 4096], BF16) for i in range(2)]
    b_stgB = [[P.buf() for _ in range(4)] for _ in range(2)]
    b_imgB = [P.buf() for _ in range(2)]
    conv_jobs = make_conv_jobs(stgB, b_stgB, imgB, b_imgB)

    USE_MASKED = False

    def is_masked(m):
        return USE_MASKED and 1 <= m < NO and m % 3 == 2

    def B_load(h):
        hb = h % 2
        P.dma(KT[hb][:], KTs[h], reads=b_KT, writes=[b_KTb[hb]], skey=f"KT{hb}")
        P.dma(Vb[hb][:], Vs[h], reads=b_V, writes=[b_Vb[hb]], skey=f"Vb{hb}")
        P.dma(QT[hb][:], QTs[h], reads=[b for b in b_Q[GQ0:]], writes=[b_QTb[hb]], skey=f"QT{hb}")

    def B_gate(h):
        hb = h % 2
        j0 = 0
        while j0 < NQT:
            j1 = min(NQT, j0 + 512 // NB)
            for j in range(j0, j1):
                P.mm(psb[5][:, (j - j0) * NB:(j - j0 + 1) * NB], QT[hb][:, (j + 3) * 128:(j + 4) * 128], kmb[:, h, :], True, True,
                     reads=[b_QTb[hb], b_kmb], writes=[b_ps[5]])
            P.emit("dve", lambda e, j0=j0, j1=j1: e.tensor_tensor(
                out=gm[:, j0:j1, :], in0=psb[5][:, 0:(j1 - j0) * NB].rearrange("p (j n) -> p j n", n=NB),
                in1=pneg[:, j0 * NB:j1 * NB].rearrange("p (j n) -> p j n", n=NB), op=ALU.add),
                reads=[b_ps[5], b_pm], writes=[b_gm])
            j0 = j1
        for j in range(NQT):
            P.emit("dve", lambda e, j=j: e.max(out=top8[:, j, :], in_=gm[:, j, :]), reads=[b_gm], writes=[b_top8])
        P.emit("dve", lambda e: e.tensor_scalar(out=thr[:, :], in0=top8[:, :, 2], scalar1=-10000.0, scalar2=None, op0=ALU.max),
               reads=[b_top8], writes=[b_thr])
        thb = thr[:, :].unsqueeze(2).broadcast_to([128, NQT, NB])
        P.emit("dve", lambda e: e.tensor_tensor(out=sel[hb][:, :, :], in0=gm[:, :, :], in1=thb, op=ALU.is_ge),
               reads=[b_gm, b_thr], writes=[b_sel[hb]])
        if USE_MASKED:
            P.emit("dve", lambda e: e.tensor_tensor(out=selb16[:, :, :], in0=gm[:, :, :], in1=thb, op=ALU.is_ge),
                   reads=[b_gm, b_thr], writes=[b_selb])
        j0 = 0
        while j0 < NQT and any(is_masked(mm) for mm in range(NO + 1)):
            j1 = min(NQT, j0 + 8)
            for j in range(j0, j1):
                P.tr(pT[0][0:NB, j - j0, :], selb16[:, j, :], ident[:], reads=[b_selb, b_id], writes=[b_pT[0]])
            P.emit("dve", lambda e, j0=j0, j1=j1: e.tensor_scalar(
                out=selTneg[0:NB, j0 * 128:j1 * 128].rearrange("p (a b) -> p a b", b=128), in0=pT[0][0:NB, 0:j1 - j0, :],
                scalar1=1.0, scalar2=-NEGBIG, op0=ALU.subtract, op1=ALU.mult), reads=[b_pT[0]], writes=[b_selT])
            j0 = j1

    items = []
    for h in range(H):
        for m in range(NO + 1):
            ob = NP - 1 + m
            items.append((h, m, ob, True))
            for n in range(ob):
                items.append((h, m, n, False))
    nitems = len(items)
    ncj = len(conv_jobs)
    cj_every = max(1, (nitems - 40) // ncj)
    cj_next = [0]
    scount = [0]
    ocount = [0]
    item_state = {}

    def qinfo(m):
        if m == 0:
            return [0], 3 * 128, 128
        return [2 * m - 1, 2 * m], (2 * m + 2) * 128, 256

    def item_S(idx):
        h, m, n, own = items[idx]
        hb = h % 2
        jt, qc, W = qinfo(m)
        sb = scount[0] % 3
        scount[0] += 1
        item_state[idx] = sb
        mk = is_masked(m) and not own
        if mk:
            c0 = jt[0] * 128
            P.mm(psb[sb][:, 0:2 * W].rearrange("p (a b) -> p a b", a=2), onehot[0:NB, n, :],
                 selTneg[0:NB, c0:c0 + W].unsqueeze(1).broadcast_to([NB, 2, W]), True, False,
                 reads=[b_oh, b_selT], writes=[b_ps[sb]])
        for kt in range(2):
            P.mm(psb[sb][:, kt * W:(kt + 1) * W], KT[hb][:, (2 * n + kt) * 128:(2 * n + kt + 1) * 128], QT[hb][:, qc:qc + W],
                 not mk, True, reads=[b_KTb[hb], b_QTb[hb]], writes=[b_ps[sb]])
        P.emit("act", lambda e, sb=sb, W=W: e.activation(out=PT[sb][:, 0:2 * W], in_=psb[sb][:, 0:2 * W], func=AF.Exp),
               reads=[b_ps[sb]], writes=[b_PT[sb]])
        if own:
            if m == 0:
                P.emit("pool", lambda e, sb=sb, W=W: e.tensor_tensor(out=PT[sb][:, W:W + 128], in0=PT[sb][:, W:W + 128], in1=tri[:], op=ALU.mult),
                       reads=[b_PT[sb], b_tri], writes=[b_PT[sb]])
            else:
                P.emit("pool", lambda e, sb=sb: e.tensor_tensor(out=PT[sb][:, 0:128], in0=PT[sb][:, 0:128], in1=tri[:], op=ALU.mult),
                       reads=[b_PT[sb], b_tri], writes=[b_PT[sb]])
                P.emit("pool", lambda e, sb=sb, W=W: e.tensor_tensor(out=PT[sb][:, W + 128:W + 256], in0=PT[sb][:, W + 128:W + 256], in1=tri[:], op=ALU.mult),
                       reads=[b_PT[sb], b_tri], writes=[b_PT[sb]])

    def item_PV(idx):
        h, m, n, own = items[idx]
        hb = h % 2
        jt, qc, W = qinfo(m)
        sb = item_state.pop(idx)
        ab = m % 2
        last = (idx + 1 == nitems) or items[idx + 1][3]
        mq = is_masked(m)
        if mq:
            ob_ = 7
        else:
            ob_ = 3 + (ocount[0] % 2)
            ocount[0] += 1
        pso = psb[ob_][:, :].rearrange("p (q c) -> p q c", q=2)
        pso5 = psb[5][:, :].rearrange("p (q c) -> p q c", q=2)
        for qt in range(len(jt)):
            kts = [0, 1]
            if own and m > 0 and qt == 0:
                kts = [0]
            for kt in kts:
                if mq:
                    st, sp = (own and kt == kts[0]), (last and kt == kts[-1])
                    dst, bdst = (pso, b_ps[7]) if qt == 0 else (pso5, b_ps[5])
                else:
                    st, sp = kt == kts[0], kt == kts[-1]
                    dst, bdst = pso, b_ps[ob_]
                P.mm(dst[:, qt, 0:130], PT[sb][:, kt * W + qt * 128:kt * W + (qt + 1) * 128], Vb[hb][:, 2 * n + kt, :],
                     st, sp, reads=[b_PT[sb], b_Vb[hb]], writes=[bdst])
        for qt in range(len(jt)):
            if mq:
                continue
            if own:
                P.emit("act", lambda e, qt=qt, pso=pso, ab=ab: e.activation(out=acc[ab][:, qt, :], in_=pso[:, qt, 0:130], func=AF.Copy),
                       reads=[b_ps[ob_]], writes=[b_acc[ab][qt]])
            else:
                j = jt[qt]
                P.emit("dve", lambda e, qt=qt, pso=pso, ab=ab, j=j, n=n, hb=hb: e.scalar_tensor_tensor(
                    out=acc[ab][:, qt, :], in0=pso[:, qt, 0:130], scalar=sel[hb][:, j, n:n + 1], in1=acc[ab][:, qt, :],
                    op0=ALU.mult, op1=ALU.add), reads=[b_ps[ob_], b_sel[hb], b_acc[ab][qt]], writes=[b_acc[ab][qt]])
        if last:
            nq = len(jt)
            if mq:
                asrcs = [pso, pso5]
                a_rq = lambda qt: [b_ps[7] if qt == 0 else b_ps[5]]
            else:
                asrcs = [acc[ab], acc[ab]]
                a_rq = lambda qt: [b_acc[ab][qt]]
            for qt in range(nq):
                P.emit("dve", lambda e, ab=ab, qt=qt, asrc=asrcs[qt]: e.reciprocal(out=rec[ab][:, qt:qt + 1], in_=asrc[:, qt, 128:129]),
                       reads=a_rq(qt), writes=[b_rec[ab]])
            for qt in range(nq):
                P.emit("act", lambda e, ab=ab, qt=qt, asrc=asrcs[qt]: e.activation(out=obf[ab][:, qt, :], in_=asrc[:, qt, 0:128], func=AF.Copy,
                                                                                   scale=rec[ab][:, qt:qt + 1]),
                       reads=a_rq(qt) + [b_rec[ab]], writes=[b_obf[ab]])
            for qt in range(nq):
                P.tr(pT[0][:, qt, :], obf[ab][:, qt, :], ident[:], reads=[b_obf[ab], b_id], writes=[b_pT[0]])
            c0 = jt[0] * 128
            P.emit("act", lambda e, nq=nq, c0=c0, hb=hb: e.activation(
                out=AOst[hb][:, c0:c0 + nq * 128], in_=pT[0][:, 0:nq, :], func=AF.Copy),
                reads=[b_pT[0]], writes=[b_AOst[hb]])
            if m == NO:
                P.dma(AOs[h], AOst[hb][:], reads=[b_AOst[hb]], writes=[b_AO[h]], skey="AOst", eng="pool")

    B_load(0)
    B_gate(0)
    LOOK = 2
    for idx in range(nitems + LOOK):
        if idx < nitems:
            h, m, n, own = items[idx]
            if own and m == NO and h + 1 < H:
                B_gate(h + 1)
            item_S(idx)
        if idx - LOOK >= 0:
            item_PV(idx - LOOK)
            h2, m2, n2, own2 = items[idx - LOOK]
            if own2 and m2 == 0 and h2 + 1 < H:
                B_load(h2 + 1)
        if idx % cj_every == cj_every - 1 and cj_next[0] < ncj:
            conv_jobs[cj_next[0]](["pool"])
            cj_next[0] += 1
    while cj_next[0] < ncj:
        conv_jobs[cj_next[0]](["pool", "dve", "act"])
        cj_next[0] += 1
    P.barrier()
    P.release(m_persist)
    if stop_after == "B":
        P.finalize()
        return nc

    NS = 5
    wslot = [P.sbuf(f"wslot{i}", [128, 4096], BF16) for i in range(NS)]
    b_wslot = [P.buf() for _ in range(NS)]
    xr = [P.sbuf(f"xr{i}", [128, 4, D], F32) for i in range(2)]
    b_xr = [P.buf() for _ in range(2)]
    AOt = [P.sbuf(f"AOt{i}", [128, H, 512], BF16) for i in range(2)]
    b_AOt = [P.buf() for _ in range(2)]
    xnTc = P.sbuf("xnTc", [128, 8, 512], BF16)
    b_xnTc = P.buf()
    hT = P.sbuf("hT", [128, NFC, 512], BF16)
    b_hT = [P.buf() for _ in range(NFC)]
    sgS = [P.sbuf(f"sgS{i}", [128, 512], F32) for i in range(2)]
    b_sgS = [P.buf() for _ in range(2)]
    cS = [P.sbuf(f"cS{i}", [128, 512], F32) for i in range(2)]
    b_cS = [P.buf() for _ in range(2)]
    uT = P.sbuf("uT", [128, 8, 516], F32)
    b_uT = [P.buf() for _ in range(8)]
    t1 = [P.sbuf(f"t1_{i}", [128, 512], F32) for i in range(2)]
    t2 = [P.sbuf(f"t2_{i}", [128, 512], F32) for i in range(2)]
    b_t1 = [P.buf() for _ in range(2)]
    b_t2 = [P.buf() for _ in range(2)]
    zT = P.sbuf("zT", [128, 8, 512], BF16)
    b_zT = [P.buf() for _ in range(8)]
    cwt = P.sbuf("cwt", [128, 8, 3], F32)
    b_cwt = P.buf()
    gfin = P.sbuf("gfin", [128, D], F32)
    b_gfin = P.buf()
    ot = [P.sbuf(f"ot{i}", [128, D], F32) for i in range(2)]
    b_ot = [P.buf() for _ in range(2)]
    NMS = 5 * 4 * (NO // 2 + 1) + 8
    msC = P.sbuf("msC", [128, NMS], F32)
    sdC = P.sbuf("sdC", [128, NMS], F32)
    rsC = P.sbuf("rsC", [128, NMS], F32)
    msc = [0]
    b_out = []
    for k in range(3):
        P.dma(cwt[:, :, k], cw[k].rearrange("(j p) -> p j", p=128), writes=[b_cwt], skey=f"cwt{k}", allow_slow_non_contiguous=True)
    P.dma(gfin[:], n_fin.partition_broadcast(128), writes=[b_gfin], skey="gfin")
    b_msall = P.buf()
    P.emit("pool", lambda e: e.memset(msC[:], 0.0), writes=[b_msall])

    groups = [("halo", 2 * NP - 1, 1, 0)] + [("own", 2 * NP + 4 * g, 4, 1 + 4 * g) for g in range(NO // 2)]
    seq = []
    for (kind, tt0, nt, j0) in groups:
        seq += U_WO + U_FIN[0] + U_FOUT[0] + U_CIN
        if kind == "own":
            seq += U_COUT + U_FIN[1] + U_FOUT[1]
    wst = {"issued": 0, "cur": 0}

    def w_ensure(upto):
        while wst["issued"] <= min(upto, len(seq) - 1):
            i = wst["issued"]
            P.dma(wslot[i % NS][:], WU[seq[i]], reads=[b_WU[seq[i]]], writes=[b_wslot[i % NS]], skey=f"ws{i % NS}")
            wst["issued"] += 1

    def w_get(expect):
        i = wst["cur"]
        assert seq[i] == expect, (i, seq[i], expect)
        w_ensure(i + NS - 1)
        wst["cur"] += 1
        return wslot[i % NS], b_wslot[i % NS]

    def C_load(gi):
        kind, tt0, nt, j0 = groups[gi]
        sl = gi % 2
        T = nt * 128
        P.dma(xr[sl][:, 0:nt, :], xl[tt0 * 128:tt0 * 128 + T, :].rearrange("(s p) d -> p s d", p=128), writes=[b_xr[sl]], skey=f"xr{sl}")
        P.dma(AOt[sl][:, :, 0:T], AOs.rearrange("h d t -> d h t")[:, :, j0 * 128:j0 * 128 + T], reads=b_AO, writes=[b_AOt[sl]],
              skey=f"AOt{sl}")

    def proj_tokmajor(lhs_fn, lhs_reads, nk, units, sl, nt):
        for half in range(2):
            wt, b_wt = w_get(units[half])
            for s in range(nt):
                bi = nbank()
                for k in range(nk):
                    P.mm(psb[bi][:, :], lhs_fn(k, s), wt[:, k * 512:(k + 1) * 512], k == 0, k == nk - 1,
                         reads=lhs_reads + [b_wt], writes=[b_ps[bi]])
                xv = xr[sl][:, s, half * 512:(half + 1) * 512]
                P.emit("dve", lambda e, xv=xv, bi=bi: e.tensor_tensor(out=xv, in0=psb[bi][:, :], in1=xv, op=ALU.add),
                       reads=[b_ps[bi], b_xr[sl]], writes=[b_xr[sl]])

    def do_norm(sl, nt):
        c0 = msc[0]
        msc[0] += nt
        b_ms = P.buf()
        b_ms.last_w = b_msall.last_w
        rms_stats(lambda s: xr[sl][:, s, :], nt, msC, sdC, rsC, c0, b_xr[sl], b_ms)
        return c0, b_ms

    def ffn(l, sl, nt):
        T = nt * 128
        c0, b_ms = do_norm(sl, nt)
        norm_T(lambda s: xr[sl][:, s, :], nt, rsC, c0, b_xr[sl], b_ms, xnTc, b_xnTc)
        for u in range(11):
            wt, b_wt = w_get(U_FIN[l][u])
            wv = wt[:, :].rearrange("p (g c k f) -> p g c k f", g=2, c=2, k=8)
            for c in range(2):
                fc = 2 * u + c
                bg = nbank()
                for kc in range(8):
                    P.mm(psb[bg][:, 0:T], wv[:, 0, c, kc, :], xnTc[:, kc, 0:T], kc == 0, kc == 7, reads=[b_wt, b_xnTc], writes=[b_ps[bg]])
                bu = nbank()
                for kc in range(8):
                    P.mm(psb[bu][:, 0:T], wv[:, 1, c, kc, :], xnTc[:, kc, 0:T], kc == 0, kc == 7, reads=[b_wt, b_xnTc], writes=[b_ps[bu]])
                si = fc % 2
                P.emit("act", lambda e, si=si, bg=bg: e.activation(out=sgS[si][:, 0:T], in_=psb[bg][:, 0:T], func=AF.Silu),
                       reads=[b_ps[bg]], writes=[b_sgS[si]])
                P.emit("dve", lambda e, si=si, bu=bu, fc=fc: e.tensor_tensor(out=hT[:, fc, 0:T], in0=sgS[si][:, 0:T], in1=psb[bu][:, 0:T], op=ALU.mult),
                       reads=[b_sgS[si], b_ps[bu]], writes=[b_hT[fc]])
        for half in range(2):
            banks = [nbank() for _ in range(nt)]
            for k in range(3):
                wt, b_wt = w_get(U_FOUT[l][half * 3 + k])
                f0 = 8 * k
                nfc = min(8, NFC - f0)
                for fi in range(nfc):
                    fc = f0 + fi
                    for s in range(nt):
                        P.mm(psb[banks[s]][:, :], hT[:, fc, s * 128:(s + 1) * 128], wt[:, fi * 512:(fi + 1) * 512], fc == 0, fc == NFC - 1,
                             reads=[b_hT[fc], b_wt], writes=[b_ps[banks[s]]])
            for s in range(nt):
                xv = xr[sl][:, s, half * 512:(half + 1) * 512]
                bi = banks[s]
                P.emit("dve", lambda e, xv=xv, bi=bi: e.tensor_tensor(out=xv, in0=psb[bi][:, :], in1=xv, op=ALU.add),
                       reads=[b_ps[bi], b_xr[sl]], writes=[b_xr[sl]])

    def conv_mixer(sl, nt, halo_only):
        T = nt * 128
        c0, b_ms = do_norm(sl, nt)
        norm_T(lambda s: xr[sl][:, s, :], nt, rsC, c0, b_xr[sl], b_ms, xnTc, b_xnTc)
        for j in range(8):
            wt, b_wt = w_get(U_CIN[j])
            wv = wt[:, 0:3072].rearrange("p (g k f) -> p g k f", g=3, k=8)
            pb = {}
            for t3 in ([1, 2] if halo_only else [0, 1, 2]):
                bi = nbank()
                pb[t3] = bi
                for kc in range(8):
                    P.mm(psb[bi][:, 0:T], wv[:, t3, kc, :], xnTc[:, kc, 0:T], kc == 0, kc == 7, reads=[b_wt, b_xnTc], writes=[b_ps[bi]])
            ci = j % 2
            P.emit("act", lambda e, ci=ci, bi=pb[1]: e.activation(out=cS[ci][:, 0:T], in_=psb[bi][:, 0:T], func=AF.Copy),
                   reads=[b_ps[pb[1]]], writes=[b_cS[ci]])
            P.emit("dve", lambda e, ci=ci, bi=pb[2], j=j: e.tensor_tensor(out=uT[:, j, 2:2 + T], in0=cS[ci][:, 0:T], in1=psb[bi][:, 0:T], op=ALU.mult),
                   reads=[b_cS[ci], b_ps[pb[2]]], writes=[b_uT[j]])
            if halo_only:
                P.emit("dve", lambda e, j=j: e.tensor_scalar(out=uT[:, j, 0:2], in0=uT[:, j, T:T + 2], scalar1=flagt[:, 0:1], scalar2=None, op0=ALU.mult),
                       reads=[b_uT[j], b_flag], writes=[b_uT[j]])
                continue
            P.emit("act", lambda e, j=j, ci=ci: e.activation(out=t1[ci][:, 0:T], in_=uT[:, j, 0:T], func=AF.Copy, scale=cwt[:, j, 0:1]),
                   reads=[b_uT[j], b_cwt], writes=[b_t1[ci]])
            P.emit("dve", lambda e, j=j, ci=ci: e.scalar_tensor_tensor(out=t2[ci][:, 0:T], in0=uT[:, j, 1:1 + T], scalar=cwt[:, j, 1:2], in1=t1[ci][:, 0:T],
                                                                       op0=ALU.mult, op1=ALU.add),
                   reads=[b_uT[j], b_cwt, b_t1[ci]], writes=[b_t2[ci]])
            P.emit("dve", lambda e, j=j, ci=ci: e.scalar_tensor_tensor(out=t1[ci][:, 0:T], in0=uT[:, j, 2:2 + T], scalar=cwt[:, j, 2:3], in1=t2[ci][:, 0:T],
                                                                       op0=ALU.mult, op1=ALU.add),
                   reads=[b_uT[j], b_cwt, b_t2[ci]], writes=[b_t1[ci]])
            P.emit("dve", lambda e, j=j, ci=ci, bi=pb[0]: e.tensor_tensor(out=zT[:, j, 0:T], in0=t1[ci][:, 0:T], in1=psb[bi][:, 0:T], op=ALU.mult),
                   reads=[b_t1[ci], b_ps[pb[0]]], writes=[b_zT[j]])
            P.emit("dve", lambda e, j=j: e.tensor_copy(out=uT[:, j, 0:2], in_=uT[:, j, T:T + 2]), reads=[b_uT[j]], writes=[b_uT[j]])
        if halo_only:
            return
        proj_tokmajor(lambda k, s: zT[:, k, s * 128:(s + 1) * 128], b_zT, 8, U_COUT, sl, nt)

    def final_out(gi, sl, nt):
        c0, b_ms = do_norm(sl, nt)
        g = gi - 1
        for s in range(nt):
            oi = s % 2
            P.emit("dve", lambda e, s=s, oi=oi: e.scalar_tensor_tensor(out=ot[oi][:], in0=xr[sl][:, s, :], scalar=rsC[:, c0 + s:c0 + s + 1], in1=gfin[:],
                                                                       op0=ALU.mult, op1=ALU.mult),
                   reads=[b_xr[sl], b_ms, b_gfin], writes=[b_ot[oi]])
            bo = P.buf()
            r0 = g * 512 + s * 128
            P.dma(out[r0:r0 + 128, :], ot[oi][:], reads=[b_ot[oi]], writes=[bo], skey=f"ot{oi}", eng="pool")
            b_out.append(bo)

    C_load(0)
    for gi, (kind, tt0, nt, j0) in enumerate(groups):
        sl = gi % 2
        if gi + 1 < len(groups):
            C_load(gi + 1)
        proj_tokmajor(lambda k, s: AOt[sl][:, k, s * 128:(s + 1) * 128], [b_AOt[sl]], H, U_WO, sl, nt)
        ffn(0, sl, nt)
        conv_mixer(sl, nt, kind == "halo")
        if kind == "own":
            ffn(1, sl, nt)
            final_out(gi, sl, nt)
    P.wait_only("sp", b_out)
    P.wait_only("pool", b_out)
    P.finalize()
    return nc


def make_masks(NP, NO, half):
    NB = NP + NO
    NQT = 2 * NO + 1
    ind = np.zeros((NQT, NB), np.float32)
    for j in range(NQT):
        ob = NP - 1 + (j + 1) // 2
        lo = 0 if half == 1 else NP
        for n in range(NB):
            if lo <= n < ob:
                ind[j, n] = 1.0
    neg = np.where(ind > 0, 0.0, NEGBIG).astype(np.float32)
    ind_b = np.ascontiguousarray(np.broadcast_to(ind.reshape(1, -1), (128, NQT * NB)))
    neg_b = np.ascontiguousarray(np.broadcast_to(neg.reshape(1, -1), (128, NQT * NB)))
    return neg_b, ind_b


_PROG_CACHE = {}


def run_module(inputs, debug=False, stop_after=None):
    x = np.asarray(inputs["x"], np.float32)
    B, S, _ = x.shape
    half_t = S // 2
    NP = NO = half_t // 256
    ncores = 2 * B
    key = (NP, NO, debug, stop_after)
    if key not in _PROG_CACHE:
        _PROG_CACHE[key] = build_program(NP, NO, debug=debug, stop_after=stop_after)
    nc = _PROG_CACHE[key]
    f = lambda k: np.ascontiguousarray(np.asarray(inputs[k], np.float32))
    shared = {
        "wqkv": f("attn_w_qkv")[0], "wo": f("attn_w_o")[0], "cwin": f("conv_w_in")[0], "cw": f("conv_w")[0],
        "cwout": f("conv_w_out")[0], "fwin": f("ffn_w_in"), "fwout": f("ffn_w_out"),
        "n_attn": f("attn_norm")[0], "n_conv": f("conv_norm")[0], "n_ffn": f("ffn_norm"), "n_fin": f("final_norm"),
    }
    in_maps = []
    for c in range(ncores):
        b, half = c // 2, c % 2
        xl = np.zeros((2 * half_t, D), np.float32)
        if half == 1:
            xl[:half_t] = x[b, :half_t]
        xl[half_t:] = x[b, half * half_t:(half + 1) * half_t]
        neg, ind = make_masks(NP, NO, half)
        m = dict(shared)
        m.update({"xl": xl, "pastneg": neg, "pastind": ind, "flag": np.full((128, 1), float(half), np.float32)})
        in_maps.append(m)
    res = run_bass_kernel_spmd(nc, in_maps, core_ids=list(range(ncores)))
    out = np.zeros((B, S, D), np.float32)
    for c in range(ncores):
        b, half = c // 2, c % 2
        out[b, half * half_t:(half + 1) * half_t] = res.results[c]["out"]
    return out, res


def kernel(**inputs):
    out, _ = run_module(inputs)
    return out
```

```python
import contextlib
import numpy as np
import ml_dtypes
import concourse.bass as bass
import concourse.mybir as mybir
from concourse.bass_utils import run_bass_kernel_spmd

F32 = mybir.dt.float32
BF16 = mybir.dt.bfloat16
AF = mybir.ActivationFunctionType
ALU = mybir.AluOpType
AX = mybir.AxisListType

SEM_LIMIT = 30000


class Buf:
    __slots__ = ("name", "last_w", "readers", "dma_readers")

    def __init__(self, name):
        self.name = name
        self.last_w = None
        self.readers = {}
        self.dma_readers = []


class Op:
    __slots__ = ("eng", "fn", "waits", "signal", "sem", "val", "is_dma", "skey", "idx")

    def __init__(self, eng, fn, is_dma, skey):
        self.eng = eng
        self.fn = fn
        self.waits = []
        self.signal = False
        self.sem = None
        self.val = 0
        self.is_dma = is_dma
        self.skey = skey


class Prog:
    ENGS = ("pe", "act", "dve", "pool", "sp")

    def __init__(self, nc):
        self.nc = nc
        self.ops = {e: [] for e in self.ENGS}
        self.stack = contextlib.ExitStack()
        self.nbuf = 0
        self.sb_base = (nc.sbuf_base + 63) // 64 * 64
        self.sb_ptr = self.sb_base
        self.sb_top = nc.sbuf_top
        self.sb_n = 0
        self.sb_peak = 0
        self.last_comp = {}
        self.dma_since = {}

    def sbuf(self, name, shape, dtype):
        esz = 4 if dtype == F32 else 2
        size = esz
        for d in shape[1:]:
            size *= d
        size = (size + 63) // 64 * 64
        off = self.sb_ptr
        self.sb_ptr += size
        assert self.sb_ptr <= self.sb_top, (name, self.sb_ptr, self.sb_top)
        self.sb_peak = max(self.sb_peak, self.sb_ptr)
        self.sb_n += 1
        return self.nc.alloc_sbuf_tensor_at(f"{name}{self.sb_n}", list(shape), dtype, offset=off)

    def mark(self):
        return self.sb_ptr

    def release(self, mark):
        self.sb_ptr = mark

    def barrier(self):
        deps = list(self.last_comp.values()) + list(self.dma_since.values())
        for e in self.ENGS:
            op = Op(e, lambda eng: None, False, None)
            for w in deps:
                if w.eng == e and not w.is_dma:
                    continue
                w.signal = True
                op.waits.append(w)
            self.ops[e].append(op)
        self.dma_since = {}

    def psum(self, name, shape, dtype):
        return self.stack.enter_context(self.nc.psum_tensor(name, list(shape), dtype))

    def buf(self, name=None):
        self.nbuf += 1
        return Buf(name or f"b{self.nbuf}")

    def wait_only(self, eng, reads):
        op = Op(eng, lambda e: None, False, None)
        for b in reads:
            w = b.last_w
            if w is not None and w not in op.waits:
                w.signal = True
                op.waits.append(w)
        self.ops[eng].append(op)
        return op

    def emit(self, eng, fn, reads=(), writes=(), dma=False, skey=None):
        op = Op(eng, fn, dma, skey)
        waits = []
        for b in reads:
            if b.last_w is not None:
                waits.append(b.last_w)
        for b in writes:
            w = b.last_w
            if w is not None and (w.eng != eng or w.is_dma or dma):
                waits.append(w)
            for e, r in b.readers.items():
                if e != eng or dma:
                    waits.append(r)
            for r in b.dma_readers:
                waits.append(r)
        seen = set()
        for w in waits:
            if id(w) in seen or w is op:
                continue
            seen.add(id(w))
            w.signal = True
            op.waits.append(w)
        for b in reads:
            if dma:
                b.dma_readers.append(op)
            else:
                b.readers[eng] = op
        for b in writes:
            b.last_w = op
            b.readers = {}
            b.dma_readers = []
        if dma:
            op.signal = True
            self.dma_since[skey] = op
        else:
            self.last_comp[eng] = op
        self.ops[eng].append(op)
        return op

    def dma(self, out, in_, reads=(), writes=(), skey=None, eng="sp", **kw):
        assert skey is not None
        return self.emit(eng, lambda e: e.dma_start(out=out, in_=in_, **kw),
                         reads=reads, writes=writes, dma=True, skey=skey)

    def mm(self, out, lhsT, rhs, start, stop, reads=(), writes=()):
        return self.emit("pe", lambda e: e.matmul(out, lhsT, rhs, start=start, stop=stop),
                         reads=reads, writes=writes)

    def tr(self, out, in_, ident, reads=(), writes=()):
        return self.emit("pe", lambda e: e.transpose(out, in_, ident), reads=reads, writes=writes)

    def finalize(self):
        nc = self.nc
        sem_stack = self.stack
        nsem = [0]

        def new_sem(tag):
            nsem[0] += 1
            return sem_stack.enter_context(nc.semaphore(f"s_{tag}_{nsem[0]}"))

        for e in self.ENGS:
            cur = None
            cnt = 0
            for op in self.ops[e]:
                if op.is_dma or not op.signal:
                    continue
                if cur is None or cnt >= SEM_LIMIT:
                    cur = new_sem(e)
                    cnt = 0
                cnt += 1
                op.sem, op.val = cur, cnt
        keysem = {}
        for e in self.ENGS:
            for op in self.ops[e]:
                if not op.is_dma:
                    continue
                if op.skey not in keysem or keysem[op.skey][1] >= SEM_LIMIT:
                    keysem[op.skey] = [new_sem("d"), 0]
                ks = keysem[op.skey]
                ks[1] += 16
                op.sem, op.val = ks[0], ks[1]
        self.n_sems = nsem[0]

        with nc.Block() as block:
            def runner(ename):
                def run(eng):
                    seen = {}
                    for op in self.ops[ename]:
                        need = {}
                        for w in op.waits:
                            k = id(w.sem)
                            if seen.get(k, 0) >= w.val:
                                continue
                            if k not in need or need[k][1] < w.val:
                                need[k] = (w.sem, w.val)
                        for k, (s, v) in need.items():
                            eng.wait_ge(s, v)
                            seen[k] = v
                        ins = op.fn(eng)
                        if ins is None:
                            assert not op.signal
                            continue
                        if op.signal and op.sem is not None:
                            ins.then_inc(op.sem, 16 if op.is_dma else 1)
                return run

            block.tensor(runner("pe"))
            block.scalar(runner("act"))
            block.vector(runner("dve"))
            block.gpsimd(runner("pool"))
            block.sync(runner("sp"))
        self.stack.close()


D = 1024
H = 8
F = 2816
NFC = 22
EPS = 1e-6
QSCALE = 128.0 ** -0.5
NEGBIG = -30000.0


def build_program(NP, NO, debug=False, stop_after=None, conv_in_b=True):
    nc = bass.Bass("TRN2", target_bir_lowering=False)
    NB = NP + NO
    LT = NB * 256
    NT = LT // 128
    NG = LT // 512
    NQT = 2 * NO + 1
    GQ0 = NP // 2 - 1
    NQG = NG - GQ0
    QW = NQG * 512
    NAO = NQT * 128
    OUT_T = NO * 256

    def din(name, shape):
        return nc.dram_tensor(name, list(shape), F32, kind="ExternalInput").ap()

    xl = din("xl", [LT, D])
    wqkv = din("wqkv", [D, 3 * D])
    wo = din("wo", [D, D])
    cwin = din("cwin", [D, 3 * D])
    cw = din("cw", [3, D])
    cwout = din("cwout", [D, D])
    fwin = din("fwin", [2, D, 2 * F])
    fwout = din("fwout", [2, F, D])
    n_attn = din("n_attn", [D])
    n_conv = din("n_conv", [D])
    n_ffn = din("n_ffn", [2, D])
    n_fin = din("n_fin", [D])
    pastneg = din("pastneg", [128, NQT * NB])
    pastind = din("pastind", [128, NQT * NB])
    flag = din("flag", [128, 1])
    out = nc.dram_tensor("out", [OUT_T, D], F32, kind="ExternalOutput").ap()
    sk = "ExternalOutput" if debug else "Internal"
    KTs = nc.dram_tensor("KTs", [H, 128, LT], BF16, kind=sk).ap()
    Vs = nc.dram_tensor("Vs", [H, 128, NT, 130], BF16, kind=sk).ap()
    QTs = nc.dram_tensor("QTs", [H, 128, QW], BF16, kind=sk).ap()
    AOs = nc.dram_tensor("AOs", [H, 128, NAO], BF16, kind=sk).ap()
    NU = 46
    WU = nc.dram_tensor("WU", [NU, 128, 4096], BF16, kind="Internal").ap()
    U_WO = [0, 1]
    U_FIN = [[2 + l * 17 + u for u in range(11)] for l in range(2)]
    U_FOUT = [[2 + l * 17 + 11 + k for k in range(6)] for l in range(2)]
    U_CIN = [36 + j for j in range(8)]
    U_COUT = [44, 45]

    P = Prog(nc)
    psb = [P.psum(f"psb{i}", [128, 512], F32) for i in range(8)]
    b_ps = [P.buf(f"ps{i}") for i in range(8)]
    pT = [psb[6 + i][:, :].bitcast(BF16).rearrange("p (a b) -> p a b", a=8) for i in range(2)]
    b_pT = [b_ps[6], b_ps[7]]
    psrr = [0]

    def nbank():
        i = psrr[0] % 6
        psrr[0] += 1
        return i

    identf = P.sbuf("identf", [128, 128], F32)
    ident = P.sbuf("ident", [128, 128], BF16)
    tri = P.sbuf("tri", [128, 128], BF16)
    gcols = P.sbuf("gcols", [128, 4, 8], F32)
    kms = P.sbuf("kms", [128, H, NB], F32)
    kmb = P.sbuf("kmb", [128, H, NB], BF16)
    flagt = P.sbuf("flagt", [128, 1], F32)
    b_idf, b_id, b_tri, b_g, b_kms, b_kmb, b_flag = [P.buf() for _ in range(7)]
    P.emit("pool", lambda e: e.memset(identf[:], 0.0), writes=[b_idf])
    P.emit("pool", lambda e: e.affine_select(out=identf[:], in_=identf[:], pattern=[[-1, 128]],
                                             compare_op=ALU.not_equal, fill=1.0, base=0, channel_multiplier=1),
           reads=[b_idf], writes=[b_idf])
    P.emit("dve", lambda e: e.tensor_copy(out=ident[:], in_=identf[:]), reads=[b_idf], writes=[b_id])
    identk = P.sbuf("identk", [128, 128], F32)
    b_idk = P.buf()
    P.emit("dve", lambda e: e.tensor_copy(out=identk[:], in_=identf[:]), reads=[b_idf], writes=[b_idk])
    P.emit("pool", lambda e: e.memset(identf[:], 1.0), reads=[b_idf], writes=[b_idf])
    P.emit("pool", lambda e: e.affine_select(out=identf[:], in_=identf[:], pattern=[[1, 128]],
                                             compare_op=ALU.is_ge, fill=0.0, base=0, channel_multiplier=-1),
           reads=[b_idf], writes=[b_idf])
    P.emit("dve", lambda e: e.tensor_copy(out=tri[:], in_=identf[:]), reads=[b_idf], writes=[b_tri])
    for i, src in enumerate([n_attn, n_conv, n_ffn[0], n_ffn[1]]):
        P.dma(gcols[:, i, :], src.rearrange("(kc p) -> p kc", p=128), writes=[b_g], skey=f"g{i}",
              allow_slow_non_contiguous=True)
    P.dma(flagt[:], flag, writes=[b_flag], skey="flag")
    gimg = P.sbuf("gimg", [128, 3, 8, 128], F32)
    b_gimg = P.buf()
    P.emit("pool", lambda e: e.memset(gimg[:], 1.0), writes=[b_gimg])
    for i in range(3):
        for kc in range(8):
            P.emit("dve", lambda e, i=i, kc=kc: e.tensor_scalar(out=gimg[:, i, kc, :], in0=gimg[:, i, kc, :], scalar1=gcols[:, i + 1, kc:kc + 1],
                                                                scalar2=None, op0=ALU.mult), reads=[b_gimg, b_g], writes=[b_gimg])
    P.emit("pool", lambda e: e.memset(kms[:], 0.0), writes=[b_kms])

    b_WU = [P.buf(f"WU{u}") for u in range(NU)]
    rr = [0]

    def make_conv_jobs(stg, b_stg, img, b_img):
        jobs = []
        cnt = [0]

        def conv_ops(slot, views, gidx, eng_list):
            for (iv, sv, G) in views:
                eng = eng_list[rr[0] % len(eng_list)]
                rr[0] += 1
                rd = [b for b in b_stg[slot]]
                if G is None:
                    if eng == "act":
                        P.emit("act", lambda e, iv=iv, sv=sv: e.activation(out=iv, in_=sv, func=AF.Copy),
                               reads=rd, writes=[b_img[slot]])
                    else:
                        P.emit(eng, lambda e, iv=iv, sv=sv: e.tensor_copy(out=iv, in_=sv), reads=rd, writes=[b_img[slot]])
                else:
                    if eng == "act":
                        eng = "pool"
                    gv = gimg[:, gidx - 1, :, :].unsqueeze(1).broadcast_to([128, G, 8, 128])
                    P.emit(eng, lambda e, iv=iv, sv=sv, gv=gv: e.tensor_tensor(out=iv, in0=sv, in1=gv, op=ALU.mult),
                           reads=rd + [b_gimg], writes=[b_img[slot]])

        def job(uidx, loads, views, gidx):
            def run(eng_list):
                slot = cnt[0] % 2
                cnt[0] += 1
                for i, (sv, src) in enumerate(loads):
                    P.dma(sv(stg[slot]), src, writes=[b_stg[slot][i]], skey=f"stg{slot}_{i}")
                conv_ops(slot, [(iv(img[slot]), sv(stg[slot]), kc) for (iv, sv, kc) in views], gidx, eng_list)
                P.dma(WU[uidx], img[slot][:], reads=[b_img[slot]], writes=[b_WU[uidx]], skey=f"img{slot}", eng="pool")
            return run

        def v3(a, b):
            return lambda t: t[:, 0:a * b].rearrange("p (a b) -> p a b", a=a)

        for srcw, units in ((wo, U_WO), (cwout, U_COUT)):
            for n in range(2):
                src = srcw.rearrange("(h p) c -> p h c", p=128)[:, :, n * 512:(n + 1) * 512]
                jobs.append(job(units[n], [(v3(8, 512), src)],
                                [(lambda t: t[:, :], lambda t: t[:, :], None)], None))
        for l in range(2):
            for u in range(11):
                loads = []
                for gu in range(2):
                    for c in range(2):
                        col0 = gu * F + (2 * u + c) * 128
                        src = fwin[l].rearrange("(kc p) n -> p kc n", p=128)[:, :, col0:col0 + 128]
                        o = (gu * 2 + c) * 1024
                        loads.append((lambda t, o=o: t[:, o:o + 1024].rearrange("p (a b) -> p a b", a=8), src))
                vw = lambda t: t[:, :].rearrange("p (g k f) -> p g k f", g=4, k=8)
                jobs.append(job(U_FIN[l][u], loads, [(vw, vw, 4)], 2 + l))
        for l in range(2):
            for half in range(2):
                for k in range(3):
                    f0 = 8 * k
                    nfc = min(8, NFC - f0)
                    src = fwout[l].rearrange("(fc p) c -> p fc c", p=128)[:, f0:f0 + nfc, half * 512:(half + 1) * 512]
                    jobs.append(job(U_FOUT[l][half * 3 + k], [(v3(nfc, 512), src)],
                                    [(lambda t, n=nfc: t[:, 0:n * 512], lambda t, n=nfc: t[:, 0:n * 512], None)], None))
        for j in range(8):
            loads = []
            for t3 in range(3):
                col0 = t3 * D + j * 128
                src = cwin.rearrange("(kc p) n -> p kc n", p=128)[:, :, col0:col0 + 128]
                o = t3 * 1024
                loads.append((lambda t, o=o: t[:, o:o + 1024].rearrange("p (a b) -> p a b", a=8), src))
            vw = lambda t: t[:, 0:3072].rearrange("p (g k f) -> p g k f", g=3, k=8)
            jobs.append(job(U_CIN[j], loads, [(vw, vw, 3)], 1))
        return jobs

    m_persist = P.mark()

    Wsb = P.sbuf("Wsb", [128, 8, 3 * D], BF16)
    b_Wp = [[P.buf(f"W{part}_{kc}") for kc in range(8)] for part in range(3)]
    stgA = [P.sbuf(f"stgA{i}", [128, D], F32) for i in range(4)]
    b_stgA = [P.buf() for _ in range(4)]
    wi = 0
    for part in (1, 2, 0):
        for kc in range(8):
            sl = wi % 4
            P.dma(stgA[sl][:], wqkv[kc * 128:(kc + 1) * 128, part * D:(part + 1) * D], writes=[b_stgA[sl]], skey=f"stgA{sl}")
            ov = Wsb[:, kc, part * D:(part + 1) * D]
            iv = stgA[sl][:]
            gs = gcols[:, 0, kc:kc + 1]
            if wi % 2 == 0:
                P.emit("act", lambda e, ov=ov, iv=iv, gs=gs: e.activation(out=ov, in_=iv, func=AF.Copy, scale=gs),
                       reads=[b_stgA[sl], b_g], writes=[b_Wp[part][kc]])
            else:
                P.emit("dve", lambda e, ov=ov, iv=iv, gs=gs: e.tensor_scalar(out=ov, in0=iv, scalar1=gs, scalar2=None, op0=ALU.mult),
                       reads=[b_stgA[sl], b_g], writes=[b_Wp[part][kc]])
            wi += 1

    xs = [P.sbuf(f"xs{i}", [128, 4, D], F32) for i in range(2)]
    b_xs = [P.buf() for _ in range(2)]
    junk = P.sbuf("junk", [128, D], BF16)
    b_junk = P.buf()
    msA = P.sbuf("msA", [128, NT], F32)
    sdA = P.sbuf("sdA", [128, NT], F32)
    rsA = P.sbuf("rsA", [128, NT], F32)
    b_msA = [P.buf() for _ in range(NG)]
    xn = [P.sbuf(f"xn{i}", [128, D], BF16) for i in range(2)]
    b_xn = [P.buf() for _ in range(2)]
    xnT = [P.sbuf(f"xnT{i}", [128, 8, 512], BF16) for i in range(2)]
    b_xnT = [P.buf() for _ in range(2)]
    Kst = [P.sbuf(f"Kst{i}", [128, H, 512], BF16) for i in range(2)]
    Qst = [P.sbuf(f"Qst{i}", [128, H, 512], BF16) for i in range(2)]
    Vst = [P.sbuf(f"Vst{i}", [128, H, 4, 130], BF16) for i in range(2)]
    b_Kst = [P.buf() for _ in range(2)]
    b_Qst = [P.buf() for _ in range(2)]
    b_Vst = [P.buf() for _ in range(2)]
    b_KT = [P.buf() for _ in range(NG)]
    b_V = [P.buf() for _ in range(NG)]
    b_Q = [P.buf() for _ in range(NG)]
    P.emit("pool", lambda e: e.memset(msA[:], 0.0), writes=b_msA)
    for i in range(2):
        P.emit("pool", lambda e, i=i: e.memset(Vst[i][:, :, :, 128:129], 1.0), writes=[b_Vst[i]])
        P.emit("pool", lambda e, i=i: e.memset(Vst[i][:, :, :, 129:130], 0.0), writes=[b_Vst[i]])

    def rms_stats(xtile_fn, nt, ms, sd, rs, c0, b_x, b_ms):
        for s in range(nt):
            P.emit("act", lambda e, s=s: e.activation(out=junk[:], in_=xtile_fn(s), func=AF.Square, scale=1.0 / 32,
                                                      accum_out=ms[:, c0 + s:c0 + s + 1]),
                   reads=[b_x], writes=[b_junk, b_ms])
        P.emit("dve", lambda e: e.tensor_scalar(out=sd[:, c0:c0 + nt], in0=ms[:, c0:c0 + nt], scalar1=EPS, scalar2=None, op0=ALU.add),
               reads=[b_ms], writes=[b_ms])
        P.emit("act", lambda e: e.activation(out=sd[:, c0:c0 + nt], in_=sd[:, c0:c0 + nt], func=AF.Sqrt),
               reads=[b_ms], writes=[b_ms])
        P.emit("dve", lambda e: e.reciprocal(out=rs[:, c0:c0 + nt], in_=sd[:, c0:c0 + nt]), reads=[b_ms], writes=[b_ms])

    def norm_T(xtile_fn, nt, rs, c0, b_x, b_ms, xnT_t, b_xnT_t):
        for s in range(nt):
            i = s % 2
            P.emit("act", lambda e, s=s, i=i: e.activation(out=xn[i][:], in_=xtile_fn(s), func=AF.Copy,
                                                           scale=rs[:, c0 + s:c0 + s + 1]),
                   reads=[b_x, b_ms], writes=[b_xn[i]])
            for kc in range(8):
                P.tr(pT[i][:, kc, :], xn[i][:, kc * 128:(kc + 1) * 128], ident[:], reads=[b_xn[i], b_id], writes=[b_pT[i]])
            P.emit("dve", lambda e, s=s, i=i: e.tensor_copy(out=xnT_t[:, :, s * 128:(s + 1) * 128], in_=pT[i]),
                   reads=[b_pT[i]], writes=[b_xnT_t])

    def A_front(g):
        sl = g % 2
        P.dma(xs[sl][:], xl[g * 512:(g + 1) * 512, :].rearrange("(s p) d -> p s d", p=128), writes=[b_xs[sl]], skey=f"xs{sl}")
        rms_stats(lambda s: xs[sl][:, s, :], 4, msA, sdA, rsA, g * 4, b_xs[sl], b_msA[g])
        norm_T(lambda s: xs[sl][:, s, :], 4, rsA, g * 4, b_xs[sl], b_msA[g], xnT[sl], b_xnT[sl])

    evr = [0]

    def A_back(g):
        sl = g % 2
        xt_ = xnT[sl]
        for h in range(H):
            bi = nbank()
            for kc in range(8):
                P.mm(psb[bi][:, :], Wsb[:, kc, D + h * 128:D + (h + 1) * 128], xt_[:, kc, :], kc == 0, kc == 7,
                     reads=[b_Wp[1][kc], b_xnT[sl]], writes=[b_ps[bi]])
            for blk in range(2):
                P.emit("act", lambda e, h=h, bi=bi, blk=blk: e.activation(
                    out=Kst[sl][:, h, blk * 256:(blk + 1) * 256], in_=psb[bi][:, blk * 256:(blk + 1) * 256], func=AF.Copy,
                    accum_out=kms[:, h, 2 * g + blk:2 * g + blk + 1]),
                    reads=[b_ps[bi]], writes=[b_Kst[sl], b_kms])
        P.dma(KTs.rearrange("h d t -> d h t")[:, :, g * 512:(g + 1) * 512], Kst[sl][:], reads=[b_Kst[sl]], writes=[b_KT[g]],
              skey=f"Kst{sl}", eng="pool")
        if g >= GQ0:
            for h in range(H):
                bi = nbank()
                for kc in range(8):
                    P.mm(psb[bi][:, :], Wsb[:, kc, h * 128:(h + 1) * 128], xt_[:, kc, :], kc == 0, kc == 7,
                         reads=[b_Wp[0][kc], b_xnT[sl]], writes=[b_ps[bi]])
                P.emit("dve", lambda e, h=h, bi=bi: e.tensor_scalar(out=Qst[sl][:, h, :], in0=psb[bi][:, :], scalar1=QSCALE,
                                                                   scalar2=None, op0=ALU.mult),
                       reads=[b_ps[bi]], writes=[b_Qst[sl]])
            gq = g - GQ0
            P.dma(QTs.rearrange("h d t -> d h t")[:, :, gq * 512:(gq + 1) * 512], Qst[sl][:], reads=[b_Qst[sl]], writes=[b_Q[g]],
                  skey=f"Qst{sl}", eng="pool")
        for s in range(4):
            for half in range(2):
                bi = nbank()
                for kc in range(8):
                    P.mm(psb[bi][:, :], xt_[:, kc, s * 128:(s + 1) * 128], Wsb[:, kc, 2 * D + half * 512:2 * D + (half + 1) * 512],
                         kc == 0, kc == 7, reads=[b_Wp[2][kc], b_xnT[sl]], writes=[b_ps[bi]])
                ov = Vst[sl][:, half * 4:(half + 1) * 4, s, 0:128]
                iv = psb[bi][:, :].rearrange("p (h d) -> p h d", h=4)
                evr[0] += 1
                if evr[0] % 2 == 0:
                    P.emit("act", lambda e, ov=ov, iv=iv: e.activation(out=ov, in_=iv, func=AF.Copy),
                           reads=[b_ps[bi]], writes=[b_Vst[sl]])
                else:
                    P.emit("dve", lambda e, ov=ov, iv=iv: e.tensor_copy(out=ov, in_=iv), reads=[b_ps[bi]], writes=[b_Vst[sl]])
        P.dma(Vs.rearrange("h p t c -> p h t c")[:, :, g * 4:(g + 1) * 4, :], Vst[sl][:], reads=[b_Vst[sl]], writes=[b_V[g]],
              skey=f"Vst{sl}", eng="pool")

    A_front(0)
    for g in range(NG):
        if g + 1 < NG:
            A_front(g + 1)
        A_back(g)
    P.emit("dve", lambda e: e.tensor_copy(out=kmb[:], in_=kms[:]), reads=[b_kms], writes=[b_kmb])
    P.barrier()
    P.release(m_persist)
    if stop_after == "A":
        P.finalize()
        return nc

    KT = [P.sbuf(f"KT{i}", [128, LT], BF16) for i in range(2)]
    Vb = [P.sbuf(f"Vb{i}", [128, NT, 130], BF16) for i in range(2)]
    QT = [P.sbuf(f"QT{i}", [128, QW], BF16) for i in range(2)]
    AOst1 = P.sbuf("AOst", [128, NAO], BF16)
    AOst = [AOst1, AOst1]
    b_KTb = [P.buf() for _ in range(2)]
    b_Vb = [P.buf() for _ in range(2)]
    b_QTb = [P.buf() for _ in range(2)]
    b_AOst1 = P.buf()
    b_AOst = [b_AOst1, b_AOst1]
    b_AO = [P.buf() for _ in range(H)]
    pneg = P.sbuf("pneg", [128, NQT * NB], F32)
    b_pm = P.buf()
    P.dma(pneg[:], pastneg, writes=[b_pm], skey="pneg")
    onehot = P.sbuf("onehot", [128, NB, 128], BF16)
    b_oh = P.buf()
    P.emit("pool", lambda e: e.memset(onehot[0:NB, :, :], 1.0), writes=[b_oh])
    for n in range(NB):
        P.emit("dve", lambda e, n=n: e.tensor_scalar(out=onehot[0:NB, n, :], in0=onehot[0:NB, n, :], scalar1=identk[0:NB, n:n + 1],
                                                     scalar2=None, op0=ALU.mult), reads=[b_oh, b_idk], writes=[b_oh])
    selTneg2 = [P.sbuf(f"selTneg{i}", [128, NQT * 128], BF16) for i in range(2)]
    b_selT2 = [P.buf() for _ in range(2)]
    selb16 = P.sbuf("selb16", [128, NQT, NB], BF16)
    b_selb = P.buf()
    thr = P.sbuf("thr", [128, NQT], F32)
    b_thr = P.buf()
    gm = P.sbuf("gm", [128, NQT, NB], F32)
    top8 = P.sbuf("top8", [128, NQT, 8], F32)
    sel = [P.sbuf(f"sel{i}", [128, NQT, NB], F32) for i in range(2)]
    b_gm = P.buf()
    b_top8 = P.buf()
    b_sel = [P.buf() for _ in range(2)]
    PT = [P.sbuf(f"PT{i}", [128, 512], BF16) for i in range(3)]
    b_PT = [P.buf() for _ in range(3)]
    acc = [P.sbuf(f"acc{i}", [128, 2, 130], F32) for i in range(2)]
    b_acc = [[P.buf() for _ in range(2)] for _ in range(2)]
    rec = [P.sbuf(f"rec{i}", [128, 2], F32) for i in range(3)]
    obf = [P.sbuf(f"obf{i}", [128, 2, 128], BF16) for i in range(3)]
    b_obf = [P.buf() for _ in range(3)]
    b_rec = [P.buf() for _ in range(3)]
    stgB = [P.sbuf(f"stgB{i}", [128, 4096], F32) for i in range(2)]
    imgB = [P.sbuf(f"imgB{i}", [128, 4096], BF16) for i in range(2)]
    b_stgB = [[P.buf() for _ in range(4)] for _ in range(2)]
    b_imgB = [P.buf() for _ in range(2)]
    conv_jobs = make_conv_jobs(stgB, b_stgB, imgB, b_imgB)

    USE_MASKED = True

    def is_masked(m):
        return USE_MASKED and m >= 2 and (m - 2) % 3 == 0 and m + 2 <= NO

    def B_load(h):
        hb = h % 2
        P.dma(KT[hb][:], KTs[h], reads=b_KT, writes=[b_KTb[hb]], skey=f"KT{hb}")
        P.dma(Vb[hb][:], Vs[h], reads=b_V, writes=[b_Vb[hb]], skey=f"Vb{hb}")
        P.dma(QT[hb][:], QTs[h], reads=[b for b in b_Q[GQ0:]], writes=[b_QTb[hb]], skey=f"QT{hb}")

    def B_gate(h):
        hb = h % 2
        j0 = 0
        while j0 < NQT:
            j1 = min(NQT, j0 + 512 // NB)
            for j in range(j0, j1):
                P.mm(psb[6][:, (j - j0) * NB:(j - j0 + 1) * NB], QT[hb][:, (j + 3) * 128:(j + 4) * 128], kmb[:, h, :], True, True,
                     reads=[b_QTb[hb], b_kmb], writes=[b_ps[6]])
            P.emit("dve", lambda e, j0=j0, j1=j1: e.tensor_tensor(
                out=gm[:, j0:j1, :], in0=psb[6][:, 0:(j1 - j0) * NB].rearrange("p (j n) -> p j n", n=NB),
                in1=pneg[:, j0 * NB:j1 * NB].rearrange("p (j n) -> p j n", n=NB), op=ALU.add),
                reads=[b_ps[6], b_pm], writes=[b_gm])
            j0 = j1
        for j in range(NQT):
            P.emit("dve", lambda e, j=j: e.max(out=top8[:, j, :], in_=gm[:, j, :]), reads=[b_gm], writes=[b_top8])
        P.emit("dve", lambda e: e.tensor_scalar(out=thr[:, :], in0=top8[:, :, 2], scalar1=-10000.0, scalar2=None, op0=ALU.max),
               reads=[b_top8], writes=[b_thr])
        thb = thr[:, :].unsqueeze(2).broadcast_to([128, NQT, NB])
        P.emit("dve", lambda e: e.tensor_tensor(out=sel[hb][:, :, :], in0=gm[:, :, :], in1=thb, op=ALU.is_ge),
               reads=[b_gm, b_thr], writes=[b_sel[hb]])
        if USE_MASKED:
            P.emit("dve", lambda e: e.tensor_tensor(out=selb16[:, :, :], in0=gm[:, :, :], in1=thb, op=ALU.is_ge),
                   reads=[b_gm, b_thr], writes=[b_selb])
        j0 = 0
        while j0 < NQT and any(is_masked(mm) for mm in range(NO + 1)):
            j1 = min(NQT, j0 + 8)
            for j in range(j0, j1):
                P.tr(pT[0][0:NB, j - j0, :], selb16[:, j, :], ident[:], reads=[b_selb, b_id], writes=[b_pT[0]])
            P.emit("dve", lambda e, j0=j0, j1=j1: e.tensor_scalar(
                out=selTneg2[hb][0:NB, j0 * 128:j1 * 128].rearrange("p (a b) -> p a b", b=128), in0=pT[0][0:NB, 0:j1 - j0, :],
                scalar1=1.0, scalar2=-NEGBIG, op0=ALU.subtract, op1=ALU.mult), reads=[b_pT[0]], writes=[b_selT2[hb]])
            j0 = j1

    def qb_items(h, m):
        ob = NP - 1 + m
        return [(h, m, ob, True)] + [(h, m, n, False) for n in range(ob)]

    items = []
    for h in range(H):
        m = 0
        while m <= NO:
            if is_masked(m):
                A = qb_items(h, m)
                Bq = qb_items(h, m + 1) + qb_items(h, m + 2)
                ia = ib = 0
                while ia < len(A) or ib < len(Bq):
                    if ib >= len(Bq) or (ia < len(A) and ia * len(Bq) <= ib * len(A)):
                        items.append(A[ia])
                        ia += 1
                    else:
                        items.append(Bq[ib])
                        ib += 1
                m += 3
            else:
                items += qb_items(h, m)
                m += 1
    nitems = len(items)
    is_last = [(not own) and n == NP - 1 + m - 1 for (h, m, n, own) in items]
    fin_count = {}
    ncj = len(conv_jobs)
    cj_every = max(1, (nitems - 40) // ncj)
    cj_next = [0]
    scount = [0]
    ocount = [0]
    item_state = {}

    def qinfo(m):
        if m == 0:
            return [0], 3 * 128, 128
        return [2 * m - 1, 2 * m], (2 * m + 2) * 128, 256

    def item_S(idx):
        h, m, n, own = items[idx]
        hb = h % 2
        jt, qc, W = qinfo(m)
        sb = scount[0] % 3
        scount[0] += 1
        item_state[idx] = sb
        mk = is_masked(m) and not own
        if mk:
            c0 = jt[0] * 128
            P.mm(psb[sb][:, 0:2 * W].rearrange("p (a b) -> p a b", a=2), onehot[0:NB, n, :],
                 selTneg2[hb][0:NB, c0:c0 + W].unsqueeze(1).broadcast_to([NB, 2, W]), True, False,
                 reads=[b_oh, b_selT2[hb]], writes=[b_ps[sb]])
        for kt in range(2):
            P.mm(psb[sb][:, kt * W:(kt + 1) * W], KT[hb][:, (2 * n + kt) * 128:(2 * n + kt + 1) * 128], QT[hb][:, qc:qc + W],
                 not mk, True, reads=[b_KTb[hb], b_QTb[hb]], writes=[b_ps[sb]])
        P.emit("act", lambda e, sb=sb, W=W: e.activation(out=PT[sb][:, 0:2 * W], in_=psb[sb][:, 0:2 * W], func=AF.Exp),
               reads=[b_ps[sb]], writes=[b_PT[sb]])
        if own:
            if m == 0:
                P.emit("pool", lambda e, sb=sb, W=W: e.tensor_tensor(out=PT[sb][:, W:W + 128], in0=PT[sb][:, W:W + 128], in1=tri[:], op=ALU.mult),
                       reads=[b_PT[sb], b_tri], writes=[b_PT[sb]])
            else:
                P.emit("pool", lambda e, sb=sb: e.tensor_tensor(out=PT[sb][:, 0:128], in0=PT[sb][:, 0:128], in1=tri[:], op=ALU.mult),
                       reads=[b_PT[sb], b_tri], writes=[b_PT[sb]])
                P.emit("pool", lambda e, sb=sb, W=W: e.tensor_tensor(out=PT[sb][:, W + 128:W + 256], in0=PT[sb][:, W + 128:W + 256], in1=tri[:], op=ALU.mult),
                       reads=[b_PT[sb], b_tri], writes=[b_PT[sb]])

    def item_PV(idx):
        h, m, n, own = items[idx]
        hb = h % 2
        jt, qc, W = qinfo(m)
        sb = item_state.pop(idx)
        ab = m % 2
        last = is_last[idx]
        mq = is_masked(m)
        if mq:
            ab = 2
        if mq:
            ob_ = 7
        else:
            ob_ = 3 + (ocount[0] % 2)
            ocount[0] += 1
        pso = psb[ob_][:, :].rearrange("p (q c) -> p q c", q=2)
        pso5 = psb[5][:, :].rearrange("p (q c) -> p q c", q=2)
        for qt in range(len(jt)):
            kts = [0, 1]
            if own and m > 0 and qt == 0:
                kts = [0]
            for kt in kts:
                if mq:
                    st, sp = (own and kt == kts[0]), (last and kt == kts[-1])
                    dst, bdst = (pso, b_ps[7]) if qt == 0 else (pso5, b_ps[5])
                else:
                    st, sp = kt == kts[0], kt == kts[-1]
                    dst, bdst = pso, b_ps[ob_]
                P.mm(dst[:, qt, 0:130], PT[sb][:, kt * W + qt * 128:kt * W + (qt + 1) * 128], Vb[hb][:, 2 * n + kt, :],
                     st, sp, reads=[b_PT[sb], b_Vb[hb]], writes=[bdst])
        for qt in range(len(jt)):
            if mq:
                continue
            if own:
                P.emit("act", lambda e, qt=qt, pso=pso, ab=ab: e.activation(out=acc[ab][:, qt, :], in_=pso[:, qt, 0:130], func=AF.Copy),
                       reads=[b_ps[ob_]], writes=[b_acc[ab][qt]])
            else:
                j = jt[qt]
                P.emit("dve", lambda e, qt=qt, pso=pso, ab=ab, j=j, n=n, hb=hb: e.scalar_tensor_tensor(
                    out=acc[ab][:, qt, :], in0=pso[:, qt, 0:130], scalar=sel[hb][:, j, n:n + 1], in1=acc[ab][:, qt, :],
                    op0=ALU.mult, op1=ALU.add), reads=[b_ps[ob_], b_sel[hb], b_acc[ab][qt]], writes=[b_acc[ab][qt]])
        if last:
            nq = len(jt)
            if mq:
                asrcs = [pso, pso5]
                a_rq = lambda qt: [b_ps[7] if qt == 0 else b_ps[5]]
            else:
                asrcs = [acc[ab], acc[ab]]
                a_rq = lambda qt: [b_acc[ab][qt]]
            for qt in range(nq):
                P.emit("dve", lambda e, ab=ab, qt=qt, asrc=asrcs[qt]: e.reciprocal(out=rec[ab][:, qt:qt + 1], in_=asrc[:, qt, 128:129]),
                       reads=a_rq(qt), writes=[b_rec[ab]])
            for qt in range(nq):
                P.emit("act", lambda e, ab=ab, qt=qt, asrc=asrcs[qt]: e.activation(out=obf[ab][:, qt, :], in_=asrc[:, qt, 0:128], func=AF.Copy,
                                                                                   scale=rec[ab][:, qt:qt + 1]),
                       reads=a_rq(qt) + [b_rec[ab]], writes=[b_obf[ab]])
            for qt in range(nq):
                P.tr(pT[0][:, qt, :], obf[ab][:, qt, :], ident[:], reads=[b_obf[ab], b_id], writes=[b_pT[0]])
            c0 = jt[0] * 128
            P.emit("act", lambda e, nq=nq, c0=c0, hb=hb: e.activation(
                out=AOst[hb][:, c0:c0 + nq * 128], in_=pT[0][:, 0:nq, :], func=AF.Copy),
                reads=[b_pT[0]], writes=[b_AOst[hb]])
            fin_count[h] = fin_count.get(h, 0) + 1
            if fin_count[h] == NO + 1:
                P.dma(AOs[h], AOst[hb][:], reads=[b_AOst[hb]], writes=[b_AO[h]], skey="AOst", eng="pool")

    B_load(0)
    B_gate(0)
    LOOK = 2
    for idx in range(nitems + LOOK):
        if idx < nitems:
            h, m, n, own = items[idx]
            if own and m == NO and h + 1 < H:
                B_gate(h + 1)
            item_S(idx)
        if idx - LOOK >= 0:
            item_PV(idx - LOOK)
            h2, m2, n2, own2 = items[idx - LOOK]
            if own2 and m2 == 0 and h2 + 1 < H:
                B_load(h2 + 1)
        if idx % cj_every == cj_every - 1 and cj_next[0] < ncj:
            conv_jobs[cj_next[0]](["pool"])
            cj_next[0] += 1
    while cj_next[0] < ncj:
        conv_jobs[cj_next[0]](["pool", "dve", "act"])
        cj_next[0] += 1
    P.barrier()
    P.release(m_persist)
    if stop_after == "B":
        P.finalize()
        return nc

    NS = 5
    wslot = [P.sbuf(f"wslot{i}", [128, 4096], BF16) for i in range(NS)]
    b_wslot = [P.buf() for _ in range(NS)]
    xr = [P.sbuf(f"xr{i}", [128, 4, D], F32) for i in range(2)]
    b_xr = [P.buf() for _ in range(2)]
    AOt = [P.sbuf(f"AOt{i}", [128, H, 512], BF16) for i in range(2)]
    b_AOt = [P.buf() for _ in range(2)]
    xnTc = P.sbuf("xnTc", [128, 8, 512], BF16)
    b_xnTc = P.buf()
    hT = P.sbuf("hT", [128, NFC, 512], BF16)
    b_hT = [P.buf() for _ in range(NFC)]
    sgS = [P.sbuf(f"sgS{i}", [128, 512], F32) for i in range(2)]
    b_sgS = [P.buf() for _ in range(2)]
    cS = [P.sbuf(f"cS{i}", [128, 512], F32) for i in range(2)]
    b_cS = [P.buf() for _ in range(2)]
    uT = P.sbuf("uT", [128, 8, 516], F32)
    b_uT = [P.buf() for _ in range(8)]
    t1 = [P.sbuf(f"t1_{i}", [128, 512], F32) for i in range(2)]
    t2 = [P.sbuf(f"t2_{i}", [128, 512], F32) for i in range(2)]
    b_t1 = [P.buf() for _ in range(2)]
    b_t2 = [P.buf() for _ in range(2)]
    zT = P.sbuf("zT", [128, 8, 512], BF16)
    b_zT = [P.buf() for _ in range(8)]
    cwt = P.sbuf("cwt", [128, 8, 3], F32)
    b_cwt = P.buf()
    gfin = P.sbuf("gfin", [128, D], F32)
    b_gfin = P.buf()
    ot = [P.sbuf(f"ot{i}", [128, D], F32) for i in range(2)]
    b_ot = [P.buf() for _ in range(2)]
    NMS = 5 * 4 * (NO // 2 + 1) + 8
    msC = P.sbuf("msC", [128, NMS], F32)
    sdC = P.sbuf("sdC", [128, NMS], F32)
    rsC = P.sbuf("rsC", [128, NMS], F32)
    msc = [0]
    b_out = []
    for k in range(3):
        P.dma(cwt[:, :, k], cw[k].rearrange("(j p) -> p j", p=128), writes=[b_cwt], skey=f"cwt{k}", allow_slow_non_contiguous=True)
    P.dma(gfin[:], n_fin.partition_broadcast(128), writes=[b_gfin], skey="gfin")
    b_msall = P.buf()
    P.emit("pool", lambda e: e.memset(msC[:], 0.0), writes=[b_msall])

    groups = [("halo", 2 * NP - 1, 1, 0)] + [("own", 2 * NP + 4 * g, 4, 1 + 4 * g) for g in range(NO // 2)]
    seq = []
    for (kind, tt0, nt, j0) in groups:
        seq += U_WO + U_FIN[0] + U_FOUT[0] + U_CIN
        if kind == "own":
            seq += U_COUT + U_FIN[1] + U_FOUT[1]
    wst = {"issued": 0, "cur": 0}

    def w_ensure(upto):
        while wst["issued"] <= min(upto, len(seq) - 1):
            i = wst["issued"]
            P.dma(wslot[i % NS][:], WU[seq[i]], reads=[b_WU[seq[i]]], writes=[b_wslot[i % NS]], skey=f"ws{i % NS}")
            wst["issued"] += 1

    def w_get(expect):
        i = wst["cur"]
        assert seq[i] == expect, (i, seq[i], expect)
        w_ensure(i + NS - 1)
        wst["cur"] += 1
        return wslot[i % NS], b_wslot[i % NS]

    def C_load(gi):
        kind, tt0, nt, j0 = groups[gi]
        sl = gi % 2
        T = nt * 128
        P.dma(xr[sl][:, 0:nt, :], xl[tt0 * 128:tt0 * 128 + T, :].rearrange("(s p) d -> p s d", p=128), writes=[b_xr[sl]], skey=f"xr{sl}")
        P.dma(AOt[sl][:, :, 0:T], AOs.rearrange("h d t -> d h t")[:, :, j0 * 128:j0 * 128 + T], reads=b_AO, writes=[b_AOt[sl]],
              skey=f"AOt{sl}")

    def proj_tokmajor(lhs_fn, lhs_reads, nk, units, sl, nt):
        for half in range(2):
            wt, b_wt = w_get(units[half])
            for s in range(nt):
                bi = nbank()
                for k in range(nk):
                    P.mm(psb[bi][:, :], lhs_fn(k, s), wt[:, k * 512:(k + 1) * 512], k == 0, k == nk - 1,
                         reads=lhs_reads + [b_wt], writes=[b_ps[bi]])
                xv = xr[sl][:, s, half * 512:(half + 1) * 512]
                P.emit("dve", lambda e, xv=xv, bi=bi: e.tensor_tensor(out=xv, in0=psb[bi][:, :], in1=xv, op=ALU.add),
                       reads=[b_ps[bi], b_xr[sl]], writes=[b_xr[sl]])

    def do_norm(sl, nt):
        c0 = msc[0]
        msc[0] += nt
        b_ms = P.buf()
        b_ms.last_w = b_msall.last_w
        rms_stats(lambda s: xr[sl][:, s, :], nt, msC, sdC, rsC, c0, b_xr[sl], b_ms)
        return c0, b_ms

    def ffn(l, sl, nt):
        T = nt * 128
        c0, b_ms = do_norm(sl, nt)
        norm_T(lambda s: xr[sl][:, s, :], nt, rsC, c0, b_xr[sl], b_ms, xnTc, b_xnTc)
        for u in range(11):
            wt, b_wt = w_get(U_FIN[l][u])
            wv = wt[:, :].rearrange("p (g c k f) -> p g c k f", g=2, c=2, k=8)
            for c in range(2):
                fc = 2 * u + c
                bg = nbank()
                for kc in range(8):
                    P.mm(psb[bg][:, 0:T], wv[:, 0, c, kc, :], xnTc[:, kc, 0:T], kc == 0, kc == 7, reads=[b_wt, b_xnTc], writes=[b_ps[bg]])
                bu = nbank()
                for kc in range(8):
                    P.mm(psb[bu][:, 0:T], wv[:, 1, c, kc, :], xnTc[:, kc, 0:T], kc == 0, kc == 7, reads=[b_wt, b_xnTc], writes=[b_ps[bu]])
                si = fc % 2
                P.emit("act", lambda e, si=si, bg=bg: e.activation(out=sgS[si][:, 0:T], in_=psb[bg][:, 0:T], func=AF.Silu),
                       reads=[b_ps[bg]], writes=[b_sgS[si]])
                P.emit("dve", lambda e, si=si, bu=bu, fc=fc: e.tensor_tensor(out=hT[:, fc, 0:T], in0=sgS[si][:, 0:T], in1=psb[bu][:, 0:T], op=ALU.mult),
                       reads=[b_sgS[si], b_ps[bu]], writes=[b_hT[fc]])
        for half in range(2):
            banks = [nbank() for _ in range(nt)]
            for k in range(3):
                wt, b_wt = w_get(U_FOUT[l][half * 3 + k])
                f0 = 8 * k
                nfc = min(8, NFC - f0)
                for fi in range(nfc):
                    fc = f0 + fi
                    for s in range(nt):
                        P.mm(psb[banks[s]][:, :], hT[:, fc, s * 128:(s + 1) * 128], wt[:, fi * 512:(fi + 1) * 512], fc == 0, fc == NFC - 1,
                             reads=[b_hT[fc], b_wt], writes=[b_ps[banks[s]]])
            for s in range(nt):
                xv = xr[sl][:, s, half * 512:(half + 1) * 512]
                bi = banks[s]
                P.emit("dve", lambda e, xv=xv, bi=bi: e.tensor_tensor(out=xv, in0=psb[bi][:, :], in1=xv, op=ALU.add),
                       reads=[b_ps[bi], b_xr[sl]], writes=[b_xr[sl]])

    def conv_mixer(sl, nt, halo_only):
        T = nt * 128
        c0, b_ms = do_norm(sl, nt)
        norm_T(lambda s: xr[sl][:, s, :], nt, rsC, c0, b_xr[sl], b_ms, xnTc, b_xnTc)
        for j in range(8):
            wt, b_wt = w_get(U_CIN[j])
            wv = wt[:, 0:3072].rearrange("p (g k f) -> p g k f", g=3, k=8)
            pb = {}
            for t3 in ([1, 2] if halo_only else [0, 1, 2]):
                bi = nbank()
                pb[t3] = bi
                for kc in range(8):
                    P.mm(psb[bi][:, 0:T], wv[:, t3, kc, :], xnTc[:, kc, 0:T], kc == 0, kc == 7, reads=[b_wt, b_xnTc], writes=[b_ps[bi]])
            ci = j % 2
            P.emit("act", lambda e, ci=ci, bi=pb[1]: e.activation(out=cS[ci][:, 0:T], in_=psb[bi][:, 0:T], func=AF.Copy),
                   reads=[b_ps[pb[1]]], writes=[b_cS[ci]])
            P.emit("dve", lambda e, ci=ci, bi=pb[2], j=j: e.tensor_tensor(out=uT[:, j, 2:2 + T], in0=cS[ci][:, 0:T], in1=psb[bi][:, 0:T], op=ALU.mult),
                   reads=[b_cS[ci], b_ps[pb[2]]], writes=[b_uT[j]])
            if halo_only:
                P.emit("dve", lambda e, j=j: e.tensor_scalar(out=uT[:, j, 0:2], in0=uT[:, j, T:T + 2], scalar1=flagt[:, 0:1], scalar2=None, op0=ALU.mult),
                       reads=[b_uT[j], b_flag], writes=[b_uT[j]])
                continue
            P.emit("act", lambda e, j=j, ci=ci: e.activation(out=t1[ci][:, 0:T], in_=uT[:, j, 0:T], func=AF.Copy, scale=cwt[:, j, 0:1]),
                   reads=[b_uT[j], b_cwt], writes=[b_t1[ci]])
            P.emit("dve", lambda e, j=j, ci=ci: e.scalar_tensor_tensor(out=t2[ci][:, 0:T], in0=uT[:, j, 1:1 + T], scalar=cwt[:, j, 1:2], in1=t1[ci][:, 0:T],
                                                                       op0=ALU.mult, op1=ALU.add),
                   reads=[b_uT[j], b_cwt, b_t1[ci]], writes=[b_t2[ci]])
            P.emit("dve", lambda e, j=j, ci=ci: e.scalar_tensor_tensor(out=t1[ci][:, 0:T], in0=uT[:, j, 2:2 + T], scalar=cwt[:, j, 2:3], in1=t2[ci][:, 0:T],
                                                                       op0=ALU.mult, op1=ALU.add),
                   reads=[b_uT[j], b_cwt, b_t2[ci]], writes=[b_t1[ci]])
            P.emit("dve", lambda e, j=j, ci=ci, bi=pb[0]: e.tensor_tensor(out=zT[:, j, 0:T], in0=t1[ci][:, 0:T], in1=psb[bi][:, 0:T], op=ALU.mult),
                   reads=[b_t1[ci], b_ps[pb[0]]], writes=[b_zT[j]])
            P.emit("dve", lambda e, j=j: e.tensor_copy(out=uT[:, j, 0:2], in_=uT[:, j, T:T + 2]), reads=[b_uT[j]], writes=[b_uT[j]])
        if halo_only:
            return
        proj_tokmajor(lambda k, s: zT[:, k, s * 128:(s + 1) * 128], b_zT, 8, U_COUT, sl, nt)

    def final_out(gi, sl, nt):
        c0, b_ms = do_norm(sl, nt)
        g = gi - 1
        for s in range(nt):
            oi = s % 2
            P.emit("dve", lambda e, s=s, oi=oi: e.scalar_tensor_tensor(out=ot[oi][:], in0=xr[sl][:, s, :], scalar=rsC[:, c0 + s:c0 + s + 1], in1=gfin[:],
                                                                       op0=ALU.mult, op1=ALU.mult),
                   reads=[b_xr[sl], b_ms, b_gfin], writes=[b_ot[oi]])
            bo = P.buf()
            r0 = g * 512 + s * 128
            P.dma(out[r0:r0 + 128, :], ot[oi][:], reads=[b_ot[oi]], writes=[bo], skey=f"ot{oi}", eng="pool")
            b_out.append(bo)

    C_load(0)
    for gi, (kind, tt0, nt, j0) in enumerate(groups):
        sl = gi % 2
        if gi + 1 < len(groups):
            C_load(gi + 1)
        proj_tokmajor(lambda k, s: AOt[sl][:, k, s * 128:(s + 1) * 128], [b_AOt[sl]], H, U_WO, sl, nt)
        ffn(0, sl, nt)
        conv_mixer(sl, nt, kind == "halo")
        if kind == "own":
            ffn(1, sl, nt)
            final_out(gi, sl, nt)
    P.wait_only("sp", b_out)
    P.wait_only("pool", b_out)
    P.finalize()
    return nc


def make_masks(NP, NO, half):
    NB = NP + NO
    NQT = 2 * NO + 1
    ind = np.zeros((NQT, NB), np.float32)
    for j in range(NQT):
        ob = NP - 1 + (j + 1) // 2
        lo = 0 if half == 1 else NP
        for n in range(NB):
            if lo <= n < ob:
                ind[j, n] = 1.0
    neg = np.where(ind > 0, 0.0, NEGBIG).astype(np.float32)
    ind_b = np.ascontiguousarray(np.broadcast_to(ind.reshape(1, -1), (128, NQT * NB)))
    neg_b = np.ascontiguousarray(np.broadcast_to(neg.reshape(1, -1), (128, NQT * NB)))
    return neg_b, ind_b


_PROG_CACHE = {}


def run_module(inputs, debug=False, stop_after=None):
    x = np.asarray(inputs["x"], np.float32)
    B, S, _ = x.shape
    half_t = S // 2
    NP = NO = half_t // 256
    ncores = 2 * B
    key = (NP, NO, debug, stop_after)
    if key not in _PROG_CACHE:
        _PROG_CACHE[key] = build_program(NP, NO, debug=debug, stop_after=stop_after)
    nc = _PROG_CACHE[key]
    f = lambda k: np.ascontiguousarray(np.asarray(inputs[k], np.float32))
    shared = {
        "wqkv": f("attn_w_qkv")[0], "wo": f("attn_w_o")[0], "cwin": f("conv_w_in")[0], "cw": f("conv_w")[0],
        "cwout": f("conv_w_out")[0], "fwin": f("ffn_w_in"), "fwout": f("ffn_w_out"),
        "n_attn": f("attn_norm")[0], "n_conv": f("conv_norm")[0], "n_ffn": f("ffn_norm"), "n_fin": f("final_norm"),
    }
    in_maps = []
    for c in range(ncores):
        b, half = c // 2, c % 2
        xl = np.zeros((2 * half_t, D), np.float32)
        if half == 1:
            xl[:half_t] = x[b, :half_t]
        xl[half_t:] = x[b, half * half_t:(half + 1) * half_t]
        neg, ind = make_masks(NP, NO, half)
        m = dict(shared)
        m.update({"xl": xl, "pastneg": neg, "pastind": ind, "flag": np.full((128, 1), float(half), np.float32)})
        in_maps.append(m)
    res = run_bass_kernel_spmd(nc, in_maps, core_ids=list(range(ncores)))
    out = np.zeros((B, S, D), np.float32)
    for c in range(ncores):
        b, half = c // 2, c % 2
        out[b, half * half_t:(half + 1) * half_t] = res.results[c]["out"]
    return out, res


def kernel(**inputs):
    out, _ = run_module(inputs)
    return out
```
